# Optimizing a Trainium2 kernel written in Bass

```python
import math
import jax, jax.numpy as jnp
from jax import lax
import numpy as np

D_MODEL = 2048
BATCH = 8
SEQ = 4096
DEPTH = 1

CTX_LEN = 256
GRID_W = 64
POS_BASE = 10000.0
LN_EPS = 1e-5

SSD_D_INNER = 2048
SSD_HEAD_DIM = 64
SSD_HEADS = SSD_D_INNER // SSD_HEAD_DIM
SSD_GROUPS = 4
SSD_STATE = 128
SSD_CHUNK = 128
CONV_W = 5
XBC_DIM = SSD_D_INNER + 2 * SSD_GROUPS * SSD_STATE
SSD_COLS = XBC_DIM + 2 * SSD_HEADS

CMLP_WIDTH = 2048
CMLP_GROUPS = 8
CMLP_GROUP_DIM = CMLP_WIDTH // CMLP_GROUPS
CMLP_CHUNK = 128

IN_DIM = SSD_COLS + SSD_D_INNER + 2 * CMLP_WIDTH + 2 * D_MODEL

N_EXPERTS = 64
EXPERT_DIM = 512
SHARED_DIM = 512
TOP_K = 8
N_EXPERT_GROUPS = 8
TOPK_GROUPS = 4
ROUTED_SCALE = 2.5

DEEPNORM_ALPHA = (2.0 * DEPTH) ** 0.25
DEEPNORM_BETA = (8.0 * DEPTH) ** -0.25

kernel_name = "hybrid_ssd_chunkmlp_moe_prefix_dit"


def layer_norm(h, g, b):
    hf = h.astype(jnp.float32)
    mu = jnp.mean(hf, -1, keepdims=True)
    var = jnp.mean(jnp.square(hf - mu), -1, keepdims=True)
    return ((hf - mu) * lax.rsqrt(var + LN_EPS) * g + b).astype(h.dtype)


def rms_norm(h, g):
    hf = h.astype(jnp.float32)
    return hf * lax.rsqrt(jnp.mean(jnp.square(hf), -1, keepdims=True) + LN_EPS) * g


def modulate(h, shift, scale):
    return h * (1 + scale) + shift


def adaln(cvec, w, b):
    return (jax.nn.silu(cvec) @ w + b)[:, None, :]


def sincos_2d(rows, dim):
    quarter = dim // 4
    omega = 1.0 / POS_BASE ** (jnp.arange(quarter, dtype=jnp.float32) / quarter)
    r = jnp.broadcast_to(jnp.arange(rows, dtype=jnp.float32)[:, None], (rows, GRID_W)).reshape(-1)
    col = jnp.broadcast_to(jnp.arange(GRID_W, dtype=jnp.float32)[None, :], (rows, GRID_W)).reshape(-1)
    ar = r[:, None] * omega
    ac = col[:, None] * omega
    return jnp.concatenate([jnp.sin(ar), jnp.cos(ar), jnp.sin(ac), jnp.cos(ac)], -1)


def dwconv_centred(u, w, b):
    y = lax.conv_general_dilated(u, w[:, None, :].astype(u.dtype), window_strides=(1,),
                                 padding=[(CONV_W // 2, CONV_W // 2)],
                                 dimension_numbers=('NWC', 'WIO', 'NWC'),
                                 feature_group_count=u.shape[-1])
    return y + b


def segsum(a):
    n = a.shape[-1]
    cs = jnp.cumsum(a, axis=-1)
    diff = cs[..., :, None] - cs[..., None, :]
    return jnp.where(jnp.tril(jnp.ones((n, n), dtype=bool)), diff, -jnp.inf)


def ssd_scan(xh, dt, A, Bm, Cm, init_state, with_output):
    f32 = jnp.float32
    bsz, L, H, P = xh.shape
    G, N = Bm.shape[-2], Bm.shape[-1]
    R = H // G
    Q = SSD_CHUNK
    nc = L // Q
    x = (xh.astype(f32) * dt[..., None]).reshape(bsz, nc, Q, G, R, P)
    a = jnp.moveaxis((dt * A).reshape(bsz, nc, Q, G, R), 2, -1)
    a_cs = jnp.cumsum(a, axis=-1)
    Bc = Bm.astype(f32).reshape(bsz, nc, Q, G, N)
    decay_to_end = jnp.moveaxis(jnp.exp(a_cs[..., -1:] - a_cs), -1, 2)[..., None]
    local_states = jnp.einsum('bclgn,bclgrp->bcgrpn', Bc, x * decay_to_end)
    init = init_state.astype(f32).reshape(bsz, 1, G, R, P, N)
    states = jnp.concatenate([init, local_states], axis=1)
    chunk_a = jnp.pad(jnp.moveaxis(a_cs[..., -1], 1, -1), ((0, 0), (0, 0), (0, 0), (1, 0)))
    carried = jnp.einsum('bgrzc,bcgrpn->bzgrpn', jnp.exp(segsum(chunk_a)), states)
    final = carried[:, -1].reshape(bsz, H, P, N)
    if not with_output:
        return None, final
    Cc = Cm.astype(f32).reshape(bsz, nc, Q, G, N)
    scores = jnp.einsum('bclgn,bcsgn->bcgls', Cc, Bc)
    y_diag = jnp.einsum('bcgrls,bcsgrp->bclgrp', scores[:, :, :, None] * jnp.exp(segsum(a)), x)
    decay_from_start = jnp.moveaxis(jnp.exp(a_cs), -1, 2)[..., None]
    y_off = jnp.einsum('bclgn,bcgrpn->bclgrp', Cc, carried[:, :-1]) * decay_from_start
    return (y_diag + y_off).reshape(bsz, L, H, P), final


def ssd_prep(p_ssd, conv_w, conv_b, dt_bias):
    bsz, L, _ = p_ssd.shape
    xbc = jax.nn.silu(dwconv_centred(p_ssd[..., :XBC_DIM], conv_w, conv_b))
    gn = SSD_GROUPS * SSD_STATE
    xh = xbc[..., :SSD_D_INNER].reshape(bsz, L, SSD_HEADS, SSD_HEAD_DIM)
    Bm = xbc[..., SSD_D_INNER:SSD_D_INNER + gn].reshape(bsz, L, SSD_GROUPS, SSD_STATE)
    Cm = xbc[..., SSD_D_INNER + gn:].reshape(bsz, L, SSD_GROUPS, SSD_STATE)
    dt = jax.nn.softplus(p_ssd[..., XBC_DIM:].astype(jnp.float32).reshape(bsz, L, 2, SSD_HEADS)
                         + dt_bias.astype(jnp.float32))
    return xh, Bm, Cm, dt


def ssd_bidir(xh, Bm, Cm, dt, a_log, init_f, init_b, with_output):
    A = -jnp.exp(a_log.astype(jnp.float32))
    flip = lambda t: jnp.flip(t, axis=1)
    y_f, s_f = ssd_scan(xh, dt[:, :, 0], A[0], Bm, Cm, init_f, with_output)
    y_b, s_b = ssd_scan(flip(xh), flip(dt[:, :, 1]), A[1], flip(Bm), flip(Cm), init_b, with_output)
    y = y_f + flip(y_b) if with_output else None
    return y, s_f, s_b


def chunk_mlp(u, v, ln_g, ln_b, w_s, b_s):
    bsz, L, _ = v.shape
    v = layer_norm(jax.nn.gelu(v), ln_g, ln_b)
    vc = v.reshape(bsz, L // CMLP_CHUNK, CMLP_CHUNK, CMLP_GROUPS, CMLP_GROUP_DIM)
    mixed = jnp.einsum('gts,bnsgd->bntgd', w_s, vc) + b_s.T[None, None, :, :, None]
    return jax.nn.gelu(u) * mixed.reshape(bsz, L, CMLP_WIDTH)


def token_mixer(a, init_f, init_b, lp):
    bsz, L, _ = a.shape
    proj = a @ lp['w_in']
    xh, Bm, Cm, dt = ssd_prep(proj[..., :SSD_COLS], lp['conv_w'], lp['conv_b'], lp['dt_bias'])
    o = SSD_COLS
    z = proj[..., o:o + SSD_D_INNER]
    o += SSD_D_INNER
    u = proj[..., o:o + CMLP_WIDTH]
    o += CMLP_WIDTH
    v = proj[..., o:o + CMLP_WIDTH]
    o += CMLP_WIDTH
    gate_ssd = proj[..., o:o + D_MODEL]
    gate_cm = proj[..., o + D_MODEL:]
    y, s_f, s_b = ssd_bidir(xh, Bm, Cm, dt, lp['a_log'], init_f, init_b, True)
    y = (y + lp['d_skip'].astype(jnp.float32)[:, None] * xh.astype(jnp.float32)).reshape(bsz, L, SSD_D_INNER)
    y = rms_norm(y * jax.nn.silu(z.astype(jnp.float32)), lp['ssd_norm_w']).astype(a.dtype)
    y_ssd = y @ lp['w_ssd_br']
    y_cm = chunk_mlp(u, v, lp['cmlp_ln_g'], lp['cmlp_ln_b'], lp['cmlp_ws'], lp['cmlp_bs']) @ lp['w_cmlp_br']
    merged = jax.nn.sigmoid(gate_ssd) * y_ssd + jax.nn.sigmoid(gate_cm) * y_cm
    return merged @ lp['w_o'], s_f, s_b


def context_ssd_states(a, lp):
    bsz = a.shape[0]
    proj = a @ lp['w_in'][:, :SSD_COLS]
    xh, Bm, Cm, dt = ssd_prep(proj, lp['conv_w'], lp['conv_b'], lp['dt_bias'])
    zero = jnp.zeros((bsz, SSD_HEADS, SSD_HEAD_DIM, SSD_STATE), jnp.float32)
    _, s_f, s_b = ssd_bidir(xh, Bm, Cm, dt, lp['a_log'], zero, zero, False)
    return s_f, s_b


def moe_ffn(h, w_router, router_bias, w_e_gate, w_e_up, w_e_down, w_sh_gate, w_sh_up, w_sh_down):
    shape = h.shape
    t = h.reshape(-1, shape[-1])
    n_tok = t.shape[0]
    scores = jax.nn.sigmoid(t.astype(jnp.float32) @ w_router.astype(jnp.float32))
    biased = scores + router_bias.astype(jnp.float32)
    per_group = N_EXPERTS // N_EXPERT_GROUPS
    group_score = lax.top_k(biased.reshape(n_tok, N_EXPERT_GROUPS, per_group), 2)[0].sum(-1)
    _, top_groups = lax.top_k(group_score, TOPK_GROUPS)
    group_mask = jax.nn.one_hot(top_groups, N_EXPERT_GROUPS, dtype=jnp.float32).sum(1) > 0
    expert_mask = jnp.repeat(group_mask, per_group, axis=1)
    _, top_experts = lax.top_k(jnp.where(expert_mask, biased, -jnp.inf), TOP_K)
    w = jnp.take_along_axis(scores, top_experts, axis=1)
    w = ROUTED_SCALE * w / jnp.sum(w, -1, keepdims=True)
    gates = jnp.zeros((n_tok, N_EXPERTS), jnp.float32).at[jnp.arange(n_tok)[:, None], top_experts].set(w)

    def add_expert(acc, params):
        wg, wu, wd, g = params
        hid = jax.nn.silu(t @ wg) * (t @ wu)
        return acc + g[:, None] * (hid @ wd).astype(jnp.float32), None

    routed, _ = lax.scan(add_expert, jnp.zeros((n_tok, shape[-1]), jnp.float32),
                         (w_e_gate, w_e_up, w_e_down, gates.T))
    shared = (jax.nn.silu(t @ w_sh_gate) * (t @ w_sh_up)) @ w_sh_down
    return (routed + shared.astype(jnp.float32)).astype(h.dtype).reshape(shape)


def setup_inputs(seed: int = 0) -> dict:
    key = jax.random.key(seed)
    ks = jax.random.split(key, 40)
    f32 = jnp.float32
    L = DEPTH

    def nrm(k, shape, s):
        return jax.random.normal(k, shape, f32) * s

    dt0 = jnp.exp(jax.random.uniform(ks[10], (L, 2, SSD_HEADS), f32, math.log(1e-3), math.log(1e-1)))
    return {
        'x': nrm(ks[0], (BATCH, SEQ, D_MODEL), 1.0),
        'c': nrm(ks[1], (BATCH, D_MODEL), 1.0),
        'ctx': nrm(ks[2], (BATCH, CTX_LEN, D_MODEL), 1.0),
        'c_ctx': nrm(ks[3], (D_MODEL,), 1.0),
        'ln_in_g': 1.0 + nrm(ks[4], (D_MODEL,), 0.02),
        'ln_in_b': nrm(ks[5], (D_MODEL,), 0.02),
        'w_ada': nrm(ks[6], (L, D_MODEL, 6 * D_MODEL), 0.5 * D_MODEL ** -0.5),
        'b_ada': nrm(ks[7], (L, 6 * D_MODEL), 0.02),
        'w_in': nrm(ks[8], (L, D_MODEL, IN_DIM), D_MODEL ** -0.5),
        'conv_w': nrm(ks[9], (L, CONV_W, XBC_DIM), CONV_W ** -0.5),
        'conv_b': nrm(ks[11], (L, XBC_DIM), 0.02),
        'dt_bias': dt0 + jnp.log(-jnp.expm1(-dt0)),
        'a_log': jnp.log(jax.random.uniform(ks[12], (L, 2, SSD_HEADS), f32, 1.0, 16.0)),
        'd_skip': 1.0 + nrm(ks[13], (L, SSD_HEADS), 0.1),
        'ssd_norm_w': 1.0 + nrm(ks[14], (L, SSD_D_INNER), 0.02),
        'w_ssd_br': nrm(ks[15], (L, SSD_D_INNER, D_MODEL), SSD_D_INNER ** -0.5),
        'cmlp_ln_g': 1.0 + nrm(ks[16], (L, CMLP_WIDTH), 0.02),
        'cmlp_ln_b': nrm(ks[17], (L, CMLP_WIDTH), 0.02),
        'cmlp_ws': nrm(ks[18], (L, CMLP_GROUPS, CMLP_CHUNK, CMLP_CHUNK), 0.5 * CMLP_CHUNK ** -0.5),
        'cmlp_bs': 1.0 + nrm(ks[19], (L, CMLP_GROUPS, CMLP_CHUNK), 0.1),
        'w_cmlp_br': nrm(ks[20], (L, CMLP_WIDTH, D_MODEL), CMLP_WIDTH ** -0.5),
        'w_o': nrm(ks[21], (L, D_MODEL, D_MODEL), DEEPNORM_BETA * D_MODEL ** -0.5),
        'ln1_g': 1.0 + nrm(ks[22], (L, D_MODEL), 0.02),
        'ln1_b': nrm(ks[23], (L, D_MODEL), 0.02),
        'w_router': nrm(ks[24], (L, D_MODEL, N_EXPERTS), D_MODEL ** -0.5),
        'router_bias': nrm(ks[25], (L, N_EXPERTS), 0.01),
        'w_e_gate': nrm(ks[26], (L, N_EXPERTS, D_MODEL, EXPERT_DIM), D_MODEL ** -0.5),
        'w_e_up': nrm(ks[27], (L, N_EXPERTS, D_MODEL, EXPERT_DIM), D_MODEL ** -0.5),
        'w_e_down': nrm(ks[28], (L, N_EXPERTS, EXPERT_DIM, D_MODEL), DEEPNORM_BETA * EXPERT_DIM ** -0.5),
        'w_sh_gate': nrm(ks[29], (L, D_MODEL, SHARED_DIM), D_MODEL ** -0.5),
        'w_sh_up': nrm(ks[30], (L, D_MODEL, SHARED_DIM), D_MODEL ** -0.5),
        'w_sh_down': nrm(ks[31], (L, SHARED_DIM, D_MODEL), DEEPNORM_BETA * SHARED_DIM ** -0.5),
        'ln2_g': 1.0 + nrm(ks[32], (L, D_MODEL), 0.02),
        'ln2_b': nrm(ks[33], (L, D_MODEL), 0.02),
    }


def reference(x, c, ctx, c_ctx, ln_in_g, ln_in_b, w_ada, b_ada, w_in, conv_w, conv_b, dt_bias, a_log,
              d_skip, ssd_norm_w, w_ssd_br, cmlp_ln_g, cmlp_ln_b, cmlp_ws, cmlp_bs, w_cmlp_br, w_o,
              ln1_g, ln1_b, w_router, router_bias, w_e_gate, w_e_up, w_e_down, w_sh_gate, w_sh_up,
              w_sh_down, ln2_g, ln2_b):
    bsz, n_lat, _ = x.shape
    rows = n_lat // GRID_W
    pos = sincos_2d(rows, D_MODEL).astype(x.dtype)
    h_lat = layer_norm(x + pos, ln_in_g, ln_in_b)
    h_ctx = layer_norm(ctx, ln_in_g, ln_in_b)
    zero_state = jnp.zeros((bsz, SSD_HEADS, SSD_HEAD_DIM, SSD_STATE), jnp.float32)
    for i in range(DEPTH):
        lp = {'w_in': w_in[i], 'conv_w': conv_w[i], 'conv_b': conv_b[i], 'dt_bias': dt_bias[i],
              'a_log': a_log[i], 'd_skip': d_skip[i], 'ssd_norm_w': ssd_norm_w[i], 'w_ssd_br': w_ssd_br[i],
              'cmlp_ln_g': cmlp_ln_g[i], 'cmlp_ln_b': cmlp_ln_b[i], 'cmlp_ws': cmlp_ws[i],
              'cmlp_bs': cmlp_bs[i], 'w_cmlp_br': w_cmlp_br[i], 'w_o': w_o[i]}
        moe_p = (w_router[i], router_bias[i], w_e_gate[i], w_e_up[i], w_e_down[i],
                 w_sh_gate[i], w_sh_up[i], w_sh_down[i])
        last = i == DEPTH - 1
        sh1, sc1, g1, sh2, sc2, g2 = jnp.split(adaln(c, w_ada[i], b_ada[i]), 6, axis=-1)
        csh1, csc1, cg1, csh2, csc2, cg2 = jnp.split(adaln(c_ctx[None], w_ada[i], b_ada[i]), 6, axis=-1)
        a_ctx = modulate(h_ctx, csh1, csc1)
        if last:
            s_f, s_b = context_ssd_states(a_ctx, lp)
        else:
            m_ctx, s_f, s_b = token_mixer(a_ctx, zero_state, zero_state, lp)
        m_lat, _, _ = token_mixer(modulate(h_lat, sh1, sc1), s_f, s_b, lp)
        h_lat = layer_norm(DEEPNORM_ALPHA * h_lat + g1 * m_lat, ln1_g[i], ln1_b[i])
        a2_lat = modulate(h_lat, sh2, sc2)
        if last:
            f_lat = moe_ffn(a2_lat, *moe_p)
        else:
            h_ctx = layer_norm(DEEPNORM_ALPHA * h_ctx + cg1 * m_ctx, ln1_g[i], ln1_b[i])
            a2_ctx = modulate(h_ctx, csh2, csc2)
            f_all = moe_ffn(jnp.concatenate([a2_lat, a2_ctx], axis=1), *moe_p)
            f_lat, f_ctx = f_all[:, :n_lat], f_all[:, n_lat:]
            h_ctx = layer_norm(DEEPNORM_ALPHA * h_ctx + cg2 * f_ctx, ln2_g[i], ln2_b[i])
        h_lat = layer_norm(DEEPNORM_ALPHA * h_lat + g2 * f_lat, ln2_g[i], ln2_b[i])
    return h_lat
```

```python
import math
from contextlib import ExitStack

import numpy as np
import concourse.bass as bass
import concourse.mybir as mybir
from concourse.bass_utils import run_bass_kernel_spmd

F32 = mybir.dt.float32
BF16 = mybir.dt.bfloat16
I32 = mybir.dt.int32
AF = mybir.ActivationFunctionType
ALU = mybir.AluOpType

T = 4096
CTX = 256
D = 2048
KD = 16
NCH = T // 128
IN_DIM = 13376
XBC = 3072
NE = 64
ALPHA = 2.0 ** 0.25
EPS = 1e-5

DEBUG = None


class KB:
    def __init__(self):
        self.nc = bass.Bass("TRN2", target_bir_lowering=False)
        self.es = ExitStack()
        nc = self.nc
        self.eng = {"pe": nc.tensor, "dve": nc.vector, "act": nc.scalar, "pool": nc.gpsimd, "sp": nc.sync}
        self.sems = {}
        self.cnt = {}
        for e in self.eng:
            self.sems["s_" + e] = self.es.enter_context(nc.semaphore("s_" + e))
            self.cnt["s_" + e] = 0
        self.waited = {e: {} for e in self.eng}
        self.lastw = {}
        self.readers = {}
        self.same_sync = {"pe": False, "dve": True, "act": True, "pool": True, "sp": False}
        self.nps = 0

    def sb(self, es, name, shape, dt):
        self.nps += 1
        return es.enter_context(self.nc.sbuf_tensor("%s_%d" % (name, self.nps), shape, dt))

    def sem(self, name):
        if name not in self.sems:
            self.sems[name] = self.es.enter_context(self.nc.semaphore(name))
            self.cnt[name] = 0
        return self.sems[name]

    def _deps(self, e, reads, writes):
        req = {}

        def add(d):
            for sk, v in d.items():
                if req.get(sk, 0) < v:
                    req[sk] = v

        for k in reads:
            add(self.lastw.get(k, {}))
        for k in writes:
            add(self.lastw.get(k, {}))
            add(self.readers.get(k, {}))
        w = self.waited[e]
        for sk, v in req.items():
            if sk == "s_" + e and not self.same_sync[e]:
                continue
            if w.get(sk, 0) >= v:
                continue
            self.eng[e].wait_ge(self.sems[sk], v)
            w[sk] = v

    def _record(self, sk, v, reads, writes):
        for k in reads:
            d = self.readers.setdefault(k, {})
            if d.get(sk, 0) < v:
                d[sk] = v
        for k in writes:
            if k.startswith("D:"):
                d = self.lastw.setdefault(k, {})
                if d.get(sk, 0) < v:
                    d[sk] = v
            else:
                self.lastw[k] = {sk: v}
                self.readers[k] = {}

    def op(self, e, fn, reads=(), writes=()):
        self._deps(e, reads, writes)
        ins = fn(self.eng[e])
        sk = "s_" + e
        self.cnt[sk] += 1
        ins.then_inc(self.sems[sk], 1)
        self._record(sk, self.cnt[sk], reads, writes)
        return ins

    def dve(self, fn, reads=(), writes=()):
        return self.op("dve", fn, reads, writes)

    def act(self, fn, reads=(), writes=()):
        return self.op("act", fn, reads, writes)

    def pool(self, fn, reads=(), writes=()):
        return self.op("pool", fn, reads, writes)

    def mm(self, out, pairs, reads=(), writes=(), first_start=True):
        self._deps("pe", reads, writes)
        n = len(pairs)
        ins = None
        for i, (l, r) in enumerate(pairs):
            ins = self.nc.tensor.matmul(out, lhsT=l, rhs=r, start=(i == 0 and first_start), stop=(i == n - 1))
        self.cnt["s_pe"] += 1
        ins.then_inc(self.sems["s_pe"], 1)
        self._record("s_pe", self.cnt["s_pe"], reads, writes)

    def tr(self, out, in_, ident, reads=(), writes=()):
        self._deps("pe", reads, writes)
        ins = self.nc.tensor.transpose(out=out, in_=in_, identity=ident)
        self.cnt["s_pe"] += 1
        ins.then_inc(self.sems["s_pe"], 1)
        self._record("s_pe", self.cnt["s_pe"], reads, writes)

    def dma(self, q, out, in_, reads=(), writes=(), sem="dm", **kw):
        self._deps(q, reads, writes)
        s = self.sem(sem)
        ins = self.eng[q].dma_start(out=out, in_=in_, **kw)
        self.cnt[sem] += 16
        ins.then_inc(s, 16)
        self._record(sem, self.cnt[sem], reads, writes)

    def barrier(self):
        for e in self.eng:
            w = self.waited[e]
            for sk, v in self.cnt.items():
                if v == 0 or sk == "s_" + e or w.get(sk, 0) >= v:
                    continue
                self.eng[e].wait_ge(self.sems[sk], v)
                w[sk] = v

    def finish(self, keys):
        self._deps("sp", keys, ())
        self._deps("act", keys, ())


def build_program(dbg=None):
    kb = KB()
    nc = kb.nc
    dbg = dbg or {}
    dump = set(dbg.get("dump", []))
    stop = dbg.get("stop")

    def din(name, shape):
        return nc.dram_tensor(name, list(shape), F32, kind="ExternalInput").ap()

    def dscr(name, shape, dt=F32):
        kind = "ExternalOutput" if name in dump else "Internal"
        return nc.dram_tensor(name, list(shape), dt, kind=kind).ap()

    x = din("x", [T, D])
    ctx = din("ctx", [CTX, D])
    cvec = din("c", [D])
    c_ctx = din("c_ctx", [D])
    ln_in_g = din("ln_in_g", [D])
    ln_in_b = din("ln_in_b", [D])
    w_ada = din("w_ada", [D, 6 * D])
    b_ada = din("b_ada", [6 * D])
    w_in = din("w_in", [D, IN_DIM])
    conv_w = din("conv_w", [5, XBC])
    conv_b = din("conv_b", [XBC])
    dt_bias = din("dt_bias", [64])
    a_log = din("a_log", [64])
    d_skip = din("d_skip", [32])
    ssd_norm_w = din("ssd_norm_w", [D])
    w_ssd_br = din("w_ssd_br", [D, D])
    cmlp_ln_g = din("cmlp_ln_g", [D])
    cmlp_ln_b = din("cmlp_ln_b", [D])
    cmlp_ws = din("cmlp_ws", [8, 128, 128])
    cmlp_bs = din("cmlp_bs", [8, 128])
    w_cmlp_br = din("w_cmlp_br", [D, D])
    w_o = din("w_o", [D, D])
    ln1_g = din("ln1_g", [D])
    ln1_b = din("ln1_b", [D])
    w_router = din("w_router", [D, NE])
    router_bias = din("router_bias", [NE])
    w_e_gate = din("w_e_gate", [NE, D, 512])
    w_e_up = din("w_e_up", [NE, D, 512])
    w_e_down = din("w_e_down", [NE, 512, D])
    w_sh_gate = din("w_sh_gate", [D, 512])
    w_sh_up = din("w_sh_up", [D, 512])
    w_sh_down = din("w_sh_down", [512, D])
    ln2_g = din("ln2_g", [D])
    ln2_b = din("ln2_b", [D])
    cmask = din("cmask", [128, 6 * 128])
    y = nc.dram_tensor("y", [T, D], F32, kind="ExternalOutput").ap()

    RPOS = dscr("RPOS", [64, 1024])
    ADAROW = dscr("ADAROW", [96, 128])
    XN = dscr("XN", [T, D])
    XBC_T = dscr("XBC_T", [XBC, T + 4], BF16)
    XBC_C = dscr("XBC_C", [XBC, CTX + 4], BF16)
    ZS = dscr("ZS", [T, D])
    U_T = dscr("U_T", [D, T])
    VG = dscr("VG", [T, D])
    GS_T = dscr("GS_T", [D, T])
    GC_T = dscr("GC_T", [D, T])
    SBW = dscr("SBW", [NCH, 128, D], BF16)
    YN_T = dscr("YN_T", [D, T], BF16)
    CM_T = dscr("CM_T", [D, T], BF16)
    PART_T = dscr("PART_T", [D, T])
    MG_T = dscr("MG_T", [D, T], BF16)
    H1 = dscr("H1", [T, D])
    A2_T = dscr("A2_T", [D, T], BF16)
    FOUT = dscr("FOUT", [T, D])
    DBG_A = dscr("DBG_A", [128, 4096]) if "DBG_A" in dump else None

    es = kb.es

    def stop_here(name):
        if stop != name:
            return False
        kb.barrier()
        return True

    PS = [es.enter_context(nc.psum_tensor("ps%d" % i, [128, 512], F32)) for i in range(8)]
    PK = ["ps%d" % i for i in range(8)]

    cm = kb.sb(es, "cm", [128, 6, 128], F32)
    IDN, MU, MSL, ML, MSU, ONES = 0, 1, 2, 3, 4, 5
    dt_all = kb.sb(es, "dt_all", [128, NCH, 64], F32)
    dt_ctx = kb.sb(es, "dt_ctx", [128, 2, 64], F32)
    gates = kb.sb(es, "gates", [128, NCH, NE + 1], F32)
    cols = kb.sb(es, "cols", [128, 40, 16], F32)
    adac = kb.sb(es, "adac", [128, 96, 2], F32)
    convw = kb.sb(es, "convw", [128, 5, 24], F32)
    convb = kb.sb(es, "convb", [128, 24], F32)
    rows64 = kb.sb(es, "rows64", [128, 4, 64], F32)
    stats = kb.sb(es, "stats", [128, 64], F32)
    tmpr = kb.sb(es, "tmpr", [128, 128], F32)
    C_LNG, C_LNB, C_A1L, C_B1L, C_A1C, C_B1C, C_A2, C_B2, C_CMG, C_T0, C_T1 = range(11)

    kb.dma("sp", cm[:].rearrange("p a b -> p (a b)"), cmask[:, :], writes=["cm"], sem="k_cm")
    ident = cm[:, IDN, :]

    def col_load(dst, src1d, n, key):
        kb.dma("sp", tmpr[0:n, :], src1d.rearrange("(j p) -> j p", p=128), writes=["tmpr"], sem="k_tmpr")
        kb.tr(PS[0][:, 0:n], tmpr[0:n, :], cm[0:n, IDN, 0:n], reads=["tmpr", "cm"], writes=[PK[0]])
        kb.dve(lambda v: v.tensor_copy(out=dst, in_=PS[0][:, 0:n]), reads=[PK[0]], writes=[key])

    def row_load(dst, src1d, key, q="sp", sem=None):
        kb.dma(q, dst, src1d.partition_broadcast(128), writes=[key], sem="k_" + key)

    col_load(cols[:, C_LNG, :], ln_in_g, 16, "cols")
    col_load(cols[:, C_LNB, :], ln_in_b, 16, "cols")
    for k in range(5):
        col_load(convw[:, k, :], conv_w[k, :], 24, "convw")
    col_load(convb[:, :], conv_b, 24, "convb")
    row_load(rows64[:, 0, :], a_log, "rows64")
    row_load(rows64[:, 1, :], dt_bias, "rows64")
    row_load(rows64[:, 2, :], router_bias, "rows64")
    row_load(rows64[:, 3, 0:32], d_skip, "rows64")
    kb.act(lambda a: a.activation(out=rows64[:, 0, :], in_=rows64[:, 0, :], func=AF.Exp), reads=["rows64"], writes=["rows64"])
    kb.dve(lambda v: v.tensor_scalar(out=rows64[:, 0, :], in0=rows64[:, 0, :], scalar1=-1.0, scalar2=None, op0=ALU.mult),
           reads=["rows64"], writes=["rows64"])

    with ExitStack() as pes:
        ccol = kb.sb(pes, "ccol", [128, 16, 2], F32)
        craw = kb.sb(pes, "craw", [128, 32], F32)
        bcol = kb.sb(pes, "bcol", [128, 96], F32)
        wts = [kb.sb(pes, "wada%d" % i, [128, 16, 512], F32) for i in range(2)]
        col_load(craw[:, 0:16], cvec, 16, "craw")
        col_load(craw[:, 16:32], c_ctx, 16, "craw")
        col_load(bcol[:, :], b_ada, 96, "bcol")
        kb.act(lambda a: a.activation(out=ccol[:, :, 0], in_=craw[:, 0:16], func=AF.Silu), reads=["craw"], writes=["ccol"])
        kb.act(lambda a: a.activation(out=ccol[:, :, 1], in_=craw[:, 16:32], func=AF.Silu), reads=["craw"], writes=["ccol"])
        wav = w_ada.rearrange("(kc p) n -> p kc n", p=128)
        NT_A = 24

        def load_wada(ct):
            kb.dma("sp", wts[ct % 2][:], wav[:, :, ct * 512:(ct + 1) * 512], writes=["wada%d" % (ct % 2)], sem="wada%d" % (ct % 2))

        load_wada(0)
        for ct in range(NT_A):
            if ct + 1 < NT_A:
                load_wada(ct + 1)
            wt = wts[ct % 2]
            bank = 1 + (ct % 2)
            for cb in range(4):
                kb.mm(PS[bank][:, cb * 2:cb * 2 + 2],
                      [(wt[:, kc, cb * 128:(cb + 1) * 128], ccol[:, kc, :]) for kc in range(16)],
                      reads=["wada%d" % (ct % 2), "ccol"], writes=[PK[bank]])
            j0 = ct * 4
            kb.dve(lambda v: v.tensor_tensor(out=adac[:, j0:j0 + 4, :],
                                             in0=PS[bank][:, 0:8].rearrange("p (a b) -> p a b", b=2),
                                             in1=bcol[:, j0:j0 + 4].unsqueeze(2).to_broadcast([128, 4, 2]), op=ALU.add),
                   reads=[PK[bank], "bcol"], writes=["adac"])
        for w, (ca, cbb) in enumerate(((C_A1L, C_B1L), (C_A1C, C_B1C))):
            kb.dve(lambda v: v.tensor_scalar(out=cols[:, C_T0, :], in0=adac[:, 16:32, w], scalar1=1.0, scalar2=None, op0=ALU.add),
                   reads=["adac"], writes=["colsT"])
            kb.dve(lambda v: v.tensor_tensor(out=cols[:, ca, :], in0=cols[:, C_LNG, :], in1=cols[:, C_T0, :], op=ALU.mult),
                   reads=["colsT", "cols"], writes=["colsA%d" % w])
            kb.dve(lambda v: v.tensor_tensor(out=cols[:, C_T1, :], in0=cols[:, C_LNB, :], in1=cols[:, C_T0, :], op=ALU.mult),
                   reads=["colsT", "cols"], writes=["colsT1"])
            kb.dve(lambda v: v.tensor_tensor(out=cols[:, cbb, :], in0=cols[:, C_T1, :], in1=adac[:, 0:16, w], op=ALU.add),
                   reads=["colsT1", "adac"], writes=["colsB%d" % w])
        kb.dve(lambda v: v.tensor_scalar(out=cols[:, C_A2, :], in0=adac[:, 64:80, 0], scalar1=1.0, scalar2=None, op0=ALU.add),
               reads=["adac"], writes=["colsA2"])
        kb.dve(lambda v: v.tensor_copy(out=cols[:, C_B2, :], in_=adac[:, 48:64, 0]), reads=["adac"], writes=["colsB2"])
        kb.dve(lambda v: v.tensor_copy(out=bcol[:, :], in_=adac[:, :, 0]), reads=["adac"], writes=["bcol"])
        kb.tr(PS[0][0:96, 0:128], bcol[:, 0:96], ident, reads=["bcol", "cm"], writes=[PK[0]])
        kb.dve(lambda v: v.tensor_copy(out=tmpr[0:96, :], in_=PS[0][0:96, 0:128]), reads=[PK[0]], writes=["tmpr"])
        kb.dma("sp", ADAROW[:, :], tmpr[0:96, :], reads=["tmpr"], writes=["D:ADAROW"], sem="st0")
        kb.barrier()
    ADAFLAT = ADAROW.rearrange("a b -> (a b)")

    PC = kb.sb(es, "PC", [128, 1024], F32)
    with ExitStack() as pes:
        ji = kb.sb(pes, "ji", [128, 512], I32)
        om = kb.sb(pes, "om", [128, 512], F32)
        pi_ = kb.sb(pes, "pi_", [128, 1], I32)
        pf = kb.sb(pes, "pf", [128, 2], F32)
        kf = kb.sb(pes, "kf", [128, 512], F32)
        ki = kb.sb(pes, "ki", [128, 512], I32)
        kb.pool(lambda g: g.iota(out=ji[:], pattern=[[1, 512]], base=0, channel_multiplier=0), writes=["ji"])
        kb.pool(lambda g: g.iota(out=pi_[:], pattern=[[1, 1]], base=0, channel_multiplier=1), writes=["pi_"])
        kb.dve(lambda v: v.tensor_copy(out=om[:], in_=ji[:]), reads=["ji"], writes=["om"])
        kb.dve(lambda v: v.tensor_copy(out=pf[:, 0:1], in_=pi_[:]), reads=["pi_"], writes=["pf"])
        kb.dve(lambda v: v.tensor_scalar(out=pf[:, 1:2], in0=pf[:, 0:1], scalar1=63.5, scalar2=-64.0, op0=ALU.is_gt, op1=ALU.mult),
               reads=["pf"], writes=["pf1"])
        kb.dve(lambda v: v.tensor_tensor(out=pf[:, 0:1], in0=pf[:, 0:1], in1=pf[:, 1:2], op=ALU.add), reads=["pf", "pf1"], writes=["pf"])
        kb.act(lambda a: a.activation(out=om[:], in_=om[:], func=AF.Exp, scale=-math.log(10000.0) / 512.0), reads=["om"], writes=["om"])
        kb.dve(lambda v: v.tensor_scalar(out=om[:], in0=om[:], scalar1=pf[:, 0:1], scalar2=None, op0=ALU.mult), reads=["om", "pf"], writes=["om"])
        for half, shift in ((0, 0.0), (1, math.pi / 2)):
            dst = PC[:, half * 512:(half + 1) * 512]
            key = "PC%d" % half
            kb.dve(lambda v: v.tensor_scalar(out=dst, in0=om[:], scalar1=shift, scalar2=None, op0=ALU.add), reads=["om"], writes=[key])
            kb.dve(lambda v: v.tensor_scalar(out=kf[:], in0=dst, scalar1=1.0 / (2 * math.pi), scalar2=None, op0=ALU.mult), reads=[key], writes=["kf"])
            kb.dve(lambda v: v.tensor_copy(out=ki[:], in_=kf[:]), reads=["kf"], writes=["ki"])
            kb.dve(lambda v: v.tensor_copy(out=kf[:], in_=ki[:]), reads=["ki"], writes=["kf"])
            kb.dve(lambda v: v.scalar_tensor_tensor(out=dst, in0=kf[:], scalar=-2 * math.pi, in1=dst, op0=ALU.mult, op1=ALU.add),
                   reads=["kf", key], writes=[key])
            kb.dve(lambda v: v.tensor_scalar(out=kf[:], in0=dst, scalar1=math.pi, scalar2=-2 * math.pi, op0=ALU.is_gt, op1=ALU.mult), reads=[key], writes=["kf"])
            kb.dve(lambda v: v.tensor_tensor(out=dst, in0=dst, in1=kf[:], op=ALU.add), reads=["kf", key], writes=[key])
            kb.dve(lambda v: v.tensor_scalar(out=kf[:], in0=dst, scalar1=-math.pi, scalar2=2 * math.pi, op0=ALU.is_lt, op1=ALU.mult), reads=[key], writes=["kf"])
            kb.dve(lambda v: v.tensor_tensor(out=dst, in0=dst, in1=kf[:], op=ALU.add), reads=["kf", key], writes=[key])
            kb.act(lambda a: a.activation(out=dst, in_=dst, func=AF.Sin), reads=[key], writes=[key])
        kb.dma("sp", RPOS[:, :], PC[0:64, :], reads=["PC0", "PC1"], writes=["D:RPOS"], sem="st0")
        kb.barrier()

    wiv = w_in.rearrange("(kc p) n -> p kc n", p=128)

    def softplus_dt(ps_ap, pskey, dst, dkey):
        kb.dve(lambda v: v.tensor_tensor(out=stats[:, 0:64], in0=ps_ap, in1=rows64[:, 1, :], op=ALU.add), reads=["rows64", pskey], writes=["sp_x"])
        kb.dve(lambda v: v.scalar_tensor_tensor(out=dst, in0=stats[:, 0:64], scalar=-1.0, in1=stats[:, 0:64], op0=ALU.mult, op1=ALU.max), reads=["sp_x"], writes=[dkey])
        kb.act(lambda a: a.activation(out=dst, in_=dst, func=AF.Exp, scale=-1.0), reads=[dkey], writes=[dkey])
        kb.act(lambda a: a.activation(out=dst, in_=dst, func=AF.Ln, bias=1.0), reads=[dkey], writes=[dkey])
        kb.dve(lambda v: v.scalar_tensor_tensor(out=dst, in0=stats[:, 0:64], scalar=0.0, in1=dst, op0=ALU.max, op1=ALU.add),
               reads=["sp_x", dkey], writes=[dkey])

    def proj_phase(src, ntok, a_col, b_col, with_pos, xbc_dst, dt_dst, full, tag):
        NT = min(1024, ntok)
        nsup = ntok // NT
        TW = min(512, NT)
        with ExitStack() as pes:
            aT = kb.sb(pes, "aT" + tag, [128, KD, NT], BF16)
            xts = [kb.sb(pes, "xt%d%s" % (i, tag), [128, D], F32) for i in range(2)]
            xns = [kb.sb(pes, "xn%d%s" % (i, tag), [128, D], F32) for i in range(2)]
            pts = [kb.sb(pes, "pt%d%s" % (i, tag), [128, 1024], F32) for i in range(2)] if with_pos else None
            bst = kb.sb(pes, "bst" + tag, [128, 4, 6], F32)
            if full:
                lngr = kb.sb(pes, "lngr", [128, D], F32)
                lnbr = kb.sb(pes, "lnbr", [128, D], F32)
                hls = [kb.sb(pes, "hl%d" % i, [128, D], F32) for i in range(2)]
                row_load(lngr[:], ln_in_g, "lngr")
                row_load(lnbr[:], ln_in_b, "lnbr")
            mv = kb.sb(pes, "mv" + tag, [128, 4], F32)
            wbs = [kb.sb(pes, "wb%d%s" % (i, tag), [128, KD, 512], BF16) for i in range(2)]
            NSTG = 4
            stg = [kb.sb(pes, "stg%d%s" % (i, tag), [128, 512], F32) for i in range(NSTG)]
            stgb = [kb.sb(pes, "stgb%d%s" % (i, tag), [128, 512], BF16) for i in range(2)]
            segs = [("xbc", 0, XBC, "F"), ("dt", XBC, 64, "T")]
            if full:
                segs += [("z", 3136, D, "T"), ("u", 5184, D, "F"), ("v", 7232, D, "T"), ("gs", 9280, D, "F"), ("gc", 11328, D, "F")]
            wtiles = []
            for (nm, c0, ncol, lay) in segs:
                o = 0
                while o < ncol:
                    w = min(512, ncol - o)
                    wtiles.append((nm, c0 + o, o, w, lay))
                    o += w
            sti = [0]
            for s in range(nsup):
                for ci in range(NT // 128):
                    c = s * (NT // 128) + ci
                    xt = xts[c % 2]; xk = "xt%d%s" % (c % 2, tag)
                    xn = xns[c % 2]; nk = "xn%d%s" % (c % 2, tag)
                    kb.dma("sp", xt[:], src[c * 128:(c + 1) * 128, :], writes=[xk], sem="ldx%d" % (c % 2))
                    if with_pos:
                        pt = pts[c % 2]; pk = "pt%d%s" % (c % 2, tag)
                        kb.dma("sp", pt[0:64, :], RPOS[2 * c, :].partition_broadcast(64), reads=["D:RPOS"], writes=[pk], sem="ldp%d" % (c % 2))
                        kb.dma("sp", pt[64:128, :], RPOS[2 * c + 1, :].partition_broadcast(64), reads=["D:RPOS"], writes=[pk], sem="ldp%d" % (c % 2))
                        kb.dve(lambda v: v.tensor_tensor(out=xt[:, 0:1024], in0=xt[:, 0:1024], in1=pt[:], op=ALU.add), reads=[xk, pk], writes=[xk])
                        kb.dve(lambda g: g.tensor_tensor(out=xt[:, 1024:2048], in0=xt[:, 1024:2048], in1=PC[:], op=ALU.add),
                                reads=[xk, "PC0", "PC1"], writes=[xk + "h"])
                    for q in range(4):
                        kb.dve(lambda v: v.bn_stats(out=bst[:, q, :], in_=xt[:, q * 512:(q + 1) * 512]), reads=[xk, xk + "h"], writes=["bst" + tag])
                    kb.dve(lambda v: v.bn_aggr(out=mv[:, 0:2], in_=bst[:].rearrange("p a b -> p (a b)")), reads=["bst" + tag], writes=["mv" + tag])
                    kb.act(lambda a: a.activation(out=mv[:, 2:3], in_=mv[:, 1:2], func=AF.Sqrt, bias=EPS), reads=["mv" + tag], writes=["mv2" + tag])
                    kb.dve(lambda v: v.reciprocal(out=mv[:, 2:3], in_=mv[:, 2:3]), reads=["mv2" + tag], writes=["mv2" + tag])
                    kb.dve(lambda v: v.scalar_tensor_tensor(out=mv[:, 3:4], in0=mv[:, 0:1], scalar=-1.0, in1=mv[:, 2:3], op0=ALU.mult, op1=ALU.mult),
                           reads=["mv" + tag, "mv2" + tag], writes=["mv3" + tag])
                    kb.act(lambda a: a.activation(out=xn[:], in_=xt[:], func=AF.Identity, scale=mv[:, 2:3], bias=mv[:, 3:4]),
                           reads=[xk, xk + "h", "mv2" + tag, "mv3" + tag], writes=[nk])
                    if full:
                        hl = hls[c % 2]; hk = "hl%d" % (c % 2)
                        kb.dve(lambda v: v.tensor_tensor(out=hl[:], in0=xn[:], in1=lngr[:], op=ALU.mult), reads=[nk, "lngr"], writes=[hk])
                        kb.dve(lambda g: g.tensor_tensor(out=hl[:], in0=hl[:], in1=lnbr[:], op=ALU.add), reads=[hk, "lnbr"], writes=[hk])
                        kb.dma("sp", XN[c * 128:(c + 1) * 128, :], hl[:], reads=[hk], writes=["D:XN"], sem="stn%d" % (c % 2))
                    for qd in range(4):
                        bank = qd % 4
                        for j in range(4):
                            dk = qd * 4 + j
                            kb.tr(PS[bank][:, j * 128:(j + 1) * 128], xn[:, dk * 128:(dk + 1) * 128], ident, reads=[nk, "cm"], writes=[PK[bank]])
                        for j in range(4):
                            dk = qd * 4 + j
                            kb.act(lambda a: a.activation(out=aT[:, dk, ci * 128:(ci + 1) * 128], in_=PS[bank][:, j * 128:(j + 1) * 128],
                                                          func=AF.Identity, scale=a_col[:, dk:dk + 1], bias=b_col[:, dk:dk + 1]),
                                   reads=[PK[bank], "colsA0", "colsA1", "colsB0", "colsB1"], writes=["aT%s_%d" % (tag, ci // 4)])
                aTkeys = ["aT%s_%d" % (tag, i) for i in range(max(1, NT // 512))]

                def load_w(i):
                    nm, cabs, o, w, lay = wtiles[i]
                    kb.dma("pool", wbs[i % 2][:, :, 0:w], wiv[:, :, cabs:cabs + w], writes=["wb%d%s" % (i % 2, tag)], sem="ldw%d" % (i % 2))

                load_w(0)
                for i, (nm, cabs, o, w, lay) in enumerate(wtiles):
                    if i + 1 < len(wtiles):
                        load_w(i + 1)
                    wb = wbs[i % 2]; wk = "wb%d%s" % (i % 2, tag)
                    if lay == "F":
                        for cb in range(w // 128):
                            for tt in range(NT // TW):
                                bank = 4 + (sti[0] % 4)
                                kb.mm(PS[bank][:, 0:TW], [(wb[:, k, cb * 128:(cb + 1) * 128], aT[:, k, tt * TW:(tt + 1) * TW]) for k in range(KD)],
                                      reads=[wk] + aTkeys, writes=[PK[bank]])
                                row0 = o + cb * 128
                                tok0 = s * NT + tt * TW
                                if nm == "xbc":
                                    sg = stgb[sti[0] % 2]; sk = "stgb%d%s" % (sti[0] % 2, tag)
                                    kb.dve(lambda v: v.tensor_copy(out=sg[:, 0:TW], in_=PS[bank][:, 0:TW]), reads=[PK[bank]], writes=[sk])
                                    kb.dma("sp", xbc_dst[row0:row0 + 128, 2 + tok0:2 + tok0 + TW], sg[:, 0:TW], reads=[sk], writes=["D:XBC" + tag],
                                           sem="stb%d" % (sti[0] % 2))
                                else:
                                    sg = stg[sti[0] % NSTG]; sk = "stg%d%s" % (sti[0] % NSTG, tag)
                                    fn = AF.Gelu_apprx_tanh if nm == "u" else AF.Sigmoid
                                    dst = {"u": U_T, "gs": GS_T, "gc": GC_T}[nm]
                                    kb.act(lambda a: a.activation(out=sg[:, 0:TW], in_=PS[bank][:, 0:TW], func=fn), reads=[PK[bank]], writes=[sk])
                                    kb.dma("sp", dst[row0:row0 + 128, tok0:tok0 + TW], sg[:, 0:TW], reads=[sk], writes=["D:" + nm], sem="stf%d" % (sti[0] % NSTG))
                                sti[0] += 1
                    else:
                        for tc in range(NT // 128):
                            bank = 4 + (sti[0] % 4)
                            kb.mm(PS[bank][:, 0:w], [(aT[:, k, tc * 128:(tc + 1) * 128], wb[:, k, 0:w]) for k in range(KD)],
                                  reads=[wk] + aTkeys, writes=[PK[bank]])
                            c = s * (NT // 128) + tc
                            if nm == "dt":
                                softplus_dt(PS[bank][:, 0:64], PK[bank], dt_dst[:, c, :], "dt" + tag)
                            else:
                                sg = stg[sti[0] % NSTG]; sk = "stg%d%s" % (sti[0] % NSTG, tag)
                                fn = AF.Silu if nm == "z" else AF.Gelu_apprx_tanh
                                dst = ZS if nm == "z" else VG
                                kb.act(lambda a: a.activation(out=sg[:, 0:w], in_=PS[bank][:, 0:w], func=fn), reads=[PK[bank]], writes=[sk])
                                kb.dma("sp", dst[c * 128:(c + 1) * 128, o:o + w], sg[:, 0:w], reads=[sk], writes=["D:" + nm], sem="stf%d" % (sti[0] % NSTG))
                            sti[0] += 1
            kb.barrier()

    zt = kb.sb(es, "zt", [128, 2], BF16)
    kb.dve(lambda v: v.memset(zt[:], 0.0), writes=["zt"])
    for dst_, n_, tg in ((XBC_T, T, "L"), (XBC_C, CTX, "C")):
        for b in range(24):
            kb.dma("sp", dst_[b * 128:(b + 1) * 128, 0:2], zt[:], reads=["zt"], writes=["D:XBC" + tg], sem="st0")
            kb.dma("sp", dst_[b * 128:(b + 1) * 128, n_ + 2:n_ + 4], zt[:], reads=["zt"], writes=["D:XBC" + tg], sem="st0")

    SKIP = dbg.get("skip", False)
    if not SKIP:
        proj_phase(ctx, CTX, cols[:, C_A1C, :], cols[:, C_B1C, :], False, XBC_C, dt_ctx, False, "C")
        proj_phase(x, T, cols[:, C_A1L, :], cols[:, C_B1L, :], True, XBC_T, dt_all, True, "L")

    if stop == "proj":
        if DBG_A is not None:
            kb.dma("sp", DBG_A[:, 0:2048], dt_all[:].rearrange("p a b -> p (a b)"), reads=["dtL"], writes=["D:DBG_A"], sem="st0")
            kb.dma("sp", DBG_A[:, 2048:2048 + 192], adac[:].rearrange("p a b -> p (a b)"), reads=["adac"], writes=["D:DBG_A"], sem="st0")
            kb.dma("sp", DBG_A[:, 2304:2304 + 128], dt_ctx[:].rearrange("p a b -> p (a b)"), reads=["dtC"], writes=["D:DBG_A"], sem="st0")
            kb.dma("sp", DBG_A[:, 2560:2560 + 1024], PC[:], reads=["PC0", "PC1"], writes=["D:DBG_A"], sem="st0")
        kb.finish([k for k in kb.lastw if k.startswith("D:")])
        return nc

    ssd_es = ExitStack()
    dg = kb.sb(ssd_es, "dg", [128, 120, 128], BF16)
    S_init = kb.sb(ssd_es, "S_init", [128, 2, D], F32)
    for k in range(5):
        for b in range(24):
            kb.dve(lambda v: v.tensor_scalar(out=dg[:, k * 24 + b, :], in0=ident, scalar1=convw[:, k, b:b + 1], scalar2=None, op0=ALU.mult),
                   reads=["cm", "convw"], writes=["dg"])
    wrk = [0]

    def wbank():
        wrk[0] += 1
        return wrk[0] % 2

    def ssd_phase(XSRC, xkey, dtsrc, nch, mode, S_in, S_out_slot):
        with ExitStack() as pes:
            xins = [kb.sb(pes, "xin%d" % i, [128, 24, 132], BF16) for i in range(2)]
            xcT = kb.sb(pes, "xcT", [128, 16, 128], F32)
            BTf = kb.sb(pes, "BTf", [128, 4, 128], F32)
            BT = kb.sb(pes, "BT", [128, 4, 128], BF16)
            CT = kb.sb(pes, "CT", [128, 4, 128], BF16)
            Btok = kb.sb(pes, "Btok", [128, 4, 128], BF16)
            avec = kb.sb(pes, "avec", [128, 64], F32)
            Edec = kb.sb(pes, "Edec", [128, 5, 64], F32)
            w1 = kb.sb(pes, "w1", [128, 64], F32)
            xst = kb.sb(pes, "xst", [128, D], BF16)
            Sf = kb.sb(pes, "Sf", [128, D], F32)
            Sfb = kb.sb(pes, "Sfb", [128, D], BF16)
            xv = XSRC.rearrange("(b p) t -> p b t", p=128)
            if mode == "M":
                Sbb = [kb.sb(pes, "Sbb%d" % i, [128, D], BF16) for i in range(2)]
                zss = [kb.sb(pes, "zs%d" % i, [128, D], F32) for i in range(1)]
                xdt = [kb.sb(pes, "xdt%d" % i, [128, D], BF16) for i in range(2)]
                scm = [kb.sb(pes, "scm%d" % i, [128, 128], BF16) for i in range(2)]
                rhsD = [kb.sb(pes, "rhsD%d" % i, [128, 4, 128], F32) for i in range(2)]
                Ex = [kb.sb(pes, "Ex%d" % i, [128, 512], BF16) for i in range(2)]
                MT = [kb.sb(pes, "MT%d" % i, [128, 4, 128], BF16) for i in range(2)]
                t1 = kb.sb(pes, "t1", [128, 512], F32)
                t2 = kb.sb(pes, "t2", [128, 512], F32)
                ysb = kb.sb(pes, "ysb", [128, D], F32)
                xsk = kb.sb(pes, "xsk", [128, D], F32)
                nwr = kb.sb(pes, "nwr", [128, D], F32)
                ynT = [kb.sb(pes, "ynT%d" % i, [128, KD, 512], BF16) for i in range(1)]
                ss = kb.sb(pes, "ss", [128, 2], F32)
                row_load(nwr[:], ssd_norm_w, "nwr")
            if S_in is None:
                kb.dve(lambda v: v.memset(Sf[:], 0.0), writes=["Sf%d" % g for g in range(4)])
            else:
                kb.dve(lambda v: v.tensor_copy(out=Sf[:], in_=S_in), reads=["S_init"], writes=["Sf%d" % g for g in range(4)])
            kb.act(lambda a: a.copy(out=Sfb[:], in_=Sf[:]), reads=["Sf%d" % g for g in range(4)], writes=["Sfb%d" % g for g in range(4)])
            order = list(range(nch)) if mode != "B" else list(range(nch - 1, -1, -1))

            def load_x(i):
                c = order[i]
                kb.dma("sp", xins[i % 2][:], xv[:, :, c * 128:c * 128 + 132], reads=[xkey], writes=["xin%d" % (i % 2)], sem="ldxi%d" % (i % 2))

            load_x(0)
            for i, c in enumerate(order):
                if i + 1 < nch:
                    load_x(i + 1)
                xin = xins[i % 2]; xik = "xin%d" % (i % 2)
                if mode == "M":
                    kb.dma("sp", Sbb[i % 2][:], SBW[c, :, :], reads=["D:SBW"], writes=["Sbb%d" % (i % 2)], sem="ldsb%d" % (i % 2))
                    kb.dma("sp", zss[0][:], ZS[c * 128:(c + 1) * 128, :], reads=["D:z"], writes=["zs0"], sem="ldz0")
                for q in range(6):
                    bank = wbank()
                    for j in range(4):
                        b = q * 4 + j
                        kb.mm(PS[bank][:, j * 128:(j + 1) * 128], [(dg[:, k * 24 + b, :], xin[:, b, k:k + 128]) for k in range(5)],
                              reads=[xik, "dg"], writes=[PK[bank]])
                    for j in range(4):
                        b = q * 4 + j
                        if b < 16:
                            dst, key = xcT[:, b, :], "xcT"
                        elif b < 20:
                            dst, key = BTf[:, b - 16, :], "BTf"
                        else:
                            dst, key = CT[:, b - 20, :], "CT"
                        kb.act(lambda a: a.activation(out=dst, in_=PS[bank][:, j * 128:(j + 1) * 128], func=AF.Silu, bias=convb[:, b:b + 1]),
                               reads=[PK[bank], "convb"], writes=[key + str(q)])
                xcTk = ["xcT%d" % q for q in range(4)]
                kb.dve(lambda g_: g_.tensor_copy(out=BT[:], in_=BTf[:]), reads=["BTf4"], writes=["BT"])
                dtc = dtsrc[:, c, :]
                kb.dve(lambda v: v.tensor_tensor(out=avec[:], in0=dtc, in1=rows64[:, 0, :], op=ALU.mult), reads=["dtL", "dtC", "rows64"], writes=["avec"])
                bank = wbank()
                for j, mi in enumerate((MU, MSL, ML, MSU, ONES)):
                    kb.mm(PS[bank][:, j * 64:(j + 1) * 64], [(cm[:, mi, :], avec[:])], reads=["avec", "cm"], writes=[PK[bank]])
                kb.act(lambda a: a.activation(out=Edec[:].rearrange("p a b -> p (a b)"), in_=PS[bank][:, 0:320], func=AF.Exp), reads=[PK[bank]], writes=["Edec"])
                d_state = 1 if mode == "B" else 0
                ei = 3 if d_state == 1 else 1
                kb.dve(lambda v: v.tensor_tensor(out=w1[:, 0:32], in0=dtc[:, d_state * 32:(d_state + 1) * 32], in1=Edec[:, ei, d_state * 32:(d_state + 1) * 32], op=ALU.mult),
                       reads=["dtL", "dtC", "Edec"], writes=["w1"])
                for q in range(4):
                    bank = wbank()
                    for j in range(4):
                        kb.tr(PS[bank][:, j * 128:(j + 1) * 128], xcT[:, q * 4 + j, :], ident, reads=["xcT%d" % q, "cm"], writes=[PK[bank]])
                    qs = slice(q * 512, (q + 1) * 512)
                    pv = PS[bank][:, :].rearrange("p (h q) -> p h q", q=64)

                    def prod(dst, sc, rkeys, wkey):
                        kb.dve(lambda v: v.tensor_tensor(out=dst[:, qs].rearrange("p (h q) -> p h q", q=64), in0=pv,
                                                         in1=sc.unsqueeze(2).to_broadcast([128, 8, 64]), op=ALU.mult),
                               reads=[PK[bank]] + rkeys, writes=[wkey])
                    prod(xst, w1[:, q * 8:(q + 1) * 8], ["w1"], "xst%d" % q)
                    if mode == "M":
                        for d in range(2):
                            prod(xdt[d], dtc[:, d * 32 + q * 8:d * 32 + (q + 1) * 8], ["dtL"], "xdt%d_%d" % (d, q))
                        prod(xsk, rows64[:, 3, q * 8:(q + 1) * 8], ["rows64"], "xsk%d" % q)
                bank = wbank()
                for g in range(4):
                    kb.tr(PS[bank][:, g * 128:(g + 1) * 128], BTf[:, g, :], ident, reads=["BTf4", "cm"], writes=[PK[bank]])
                kb.act(lambda a: a.copy(out=Btok[:].rearrange("p a b -> p (a b)"), in_=PS[bank][:, :]), reads=[PK[bank]], writes=["Btok"])
                if mode == "B" and nch == NCH:
                    kb.dma("sp", SBW[c, :, :], Sfb[:], reads=["Sfb%d" % g for g in range(4)], writes=["D:SBW"], sem="stsb")
                if mode == "M":
                    Sb = Sbb[i % 2]; sbk = "Sbb%d" % (i % 2)
                    for g in range(4):
                        gs = slice(g * 512, (g + 1) * 512)
                        kb.mm(PS[2][:, 0:128], [(BT[:, g, :], CT[:, g, :])], reads=["BT", "CT5"], writes=[PK[2]])
                        kb.dve(lambda v: v.tensor_tensor(out=scm[0][:], in0=PS[2][:, 0:128], in1=cm[:, MU, :], op=ALU.mult), reads=[PK[2], "cm"], writes=["scm0"])
                        kb.dve(lambda v: v.tensor_tensor(out=scm[1][:], in0=PS[2][:, 0:128], in1=cm[:, ML, :], op=ALU.mult), reads=[PK[2], "cm"], writes=["scm1"])
                        kb.mm(PS[3][:, :], [(CT[:, g, :], Sfb[:, gs])], reads=["CT5", "Sfb%d" % g], writes=[PK[3]])
                        kb.mm(PS[4][:, :], [(CT[:, g, :], Sb[:, gs])], reads=["CT5", sbk], writes=[PK[4]])
                        for hq in range(2):
                            h0 = g * 8 + hq * 4
                            for d in range(2):
                                mrhs = MU if d == 0 else ML
                                mlhs = MSL if d == 0 else MSU
                                for i4 in range(4):
                                    kb.act(lambda a: a.activation(out=rhsD[d][:, i4, :], in_=cm[:, mrhs, :], func=AF.Identity,
                                                                  scale=avec[:, d * 32 + h0 + i4:d * 32 + h0 + i4 + 1]),
                                           reads=["avec", "cm"], writes=["rhsD%d_%d" % (d, i4)])
                                bd = 6 + d
                                kb.mm(PS[bd][:, :], [(cm[:, mlhs, :], rhsD[d][:].rearrange("p a b -> p (a b)"))], reads=["rhsD%d_%d" % (d, i4) for i4 in range(4)] + ["cm"], writes=[PK[bd]])
                                kb.act(lambda a: a.activation(out=Ex[d][:], in_=PS[bd][:, :], func=AF.Exp), reads=[PK[bd]], writes=["Ex%d" % d])
                                kb.dve(lambda v: v.tensor_tensor(out=MT[d][:], in0=Ex[d][:].rearrange("p (a b) -> p a b", b=128),
                                                                 in1=scm[d][:].unsqueeze(1).to_broadcast([128, 4, 128]), op=ALU.mult),
                                       reads=["Ex%d" % d, "scm%d" % d], writes=["MT%d" % d])
                            for ii in range(4):
                                h = h0 + ii
                                col = (hq * 4 + ii) * 64
                                kb.mm(PS[5][:, col:col + 64],
                                      [(MT[0][:, ii, :], xdt[0][:, h * 64:(h + 1) * 64]), (MT[1][:, ii, :], xdt[1][:, h * 64:(h + 1) * 64])],
                                      reads=["MT0", "MT1", "xdt0_%d" % g, "xdt1_%d" % g], writes=[PK[5]])
                        kb.dve(lambda v: v.tensor_tensor(out=t1[:].rearrange("p (h q) -> p h q", q=64), in0=PS[3][:, :].rearrange("p (h q) -> p h q", q=64),
                                                         in1=Edec[:, 0, g * 8:g * 8 + 8].unsqueeze(2).to_broadcast([128, 8, 64]), op=ALU.mult),
                               reads=[PK[3], "Edec"], writes=["t1"])
                        kb.dve(lambda v: v.tensor_tensor(out=t2[:].rearrange("p (h q) -> p h q", q=64), in0=PS[4][:, :].rearrange("p (h q) -> p h q", q=64),
                                                         in1=Edec[:, 2, 32 + g * 8:32 + g * 8 + 8].unsqueeze(2).to_broadcast([128, 8, 64]), op=ALU.mult),
                               reads=[PK[4], "Edec"], writes=["t2"])
                        kb.dve(lambda g_: g_.tensor_tensor(out=t1[:], in0=t1[:], in1=t2[:], op=ALU.add), reads=["t1", "t2"], writes=["t1"])
                        kb.dve(lambda v: v.tensor_tensor(out=ysb[:, gs], in0=PS[5][:, :], in1=t1[:], op=ALU.add), reads=[PK[5], "t1"], writes=["ysb%d" % g])
                for g in range(4):
                    gs = slice(g * 512, (g + 1) * 512)
                    bank = wbank()
                    kb.mm(PS[bank][:, :], [(Btok[:, g, :], xst[:, gs])], reads=["Btok", "xst%d" % g], writes=[PK[bank]])
                    kb.dve(lambda v: v.tensor_tensor(out=Sf[:, gs].rearrange("p (h q) -> p h q", q=64), in0=Sf[:, gs].rearrange("p (h q) -> p h q", q=64),
                                                     in1=Edec[:, 4, d_state * 32 + g * 8:d_state * 32 + g * 8 + 8].unsqueeze(2).to_broadcast([128, 8, 64]), op=ALU.mult),
                           reads=["Edec", "Sf%d" % g], writes=["Sf%d" % g])
                    kb.dve(lambda v: v.tensor_tensor(out=Sf[:, gs], in0=Sf[:, gs], in1=PS[bank][:, :], op=ALU.add), reads=[PK[bank], "Sf%d" % g], writes=["Sf%d" % g])
                    kb.act(lambda a: a.copy(out=Sfb[:, gs], in_=Sf[:, gs]), reads=["Sf%d" % g], writes=["Sfb%d" % g])
                if mode == "M":
                    ysk = ["ysb%d" % g for g in range(4)]
                    kb.dve(lambda v: v.tensor_tensor(out=ysb[:], in0=ysb[:], in1=xsk[:], op=ALU.add), reads=ysk + ["xsk%d" % q for q in range(4)], writes=["ysbA"])
                    kb.dve(lambda v: v.tensor_tensor(out=ysb[:], in0=ysb[:], in1=zss[0][:], op=ALU.mult), reads=["ysbA", "zs0"], writes=["ysbA"])
                    kb.act(lambda a: a.activation(out=xsk[:], in_=ysb[:], func=AF.Square, accum_out=ss[:, 0:1]), reads=["ysbA"] + ["xsk%d" % q for q in range(4)], writes=["ss"] + ["xsk%d" % q for q in range(4)])
                    kb.act(lambda a: a.activation(out=ss[:, 1:2], in_=ss[:, 0:1], func=AF.Sqrt, scale=1.0 / D, bias=EPS), reads=["ss"], writes=["ss1"])
                    kb.dve(lambda v: v.reciprocal(out=ss[:, 1:2], in_=ss[:, 1:2]), reads=["ss1"], writes=["ss1"])
                    kb.dve(lambda v: v.scalar_tensor_tensor(out=ysb[:], in0=ysb[:], scalar=ss[:, 1:2], in1=nwr[:], op0=ALU.mult, op1=ALU.mult),
                           reads=["ysbA", "ss1", "nwr"], writes=["ysbA"] + ysk)
                    yt = ynT[0]; ytk = "ynT0"
                    for q in range(4):
                        bank = wbank()
                        for j in range(4):
                            kb.tr(PS[bank][:, j * 128:(j + 1) * 128], ysb[:, (q * 4 + j) * 128:(q * 4 + j + 1) * 128], ident, reads=["ysbA", "cm"], writes=[PK[bank]])
                        o_ap = yt[:, q * 4:q * 4 + 4, (c % 4) * 128:(c % 4 + 1) * 128]
                        i_ap = PS[bank][:, :].rearrange("p (a b) -> p a b", b=128)
                        if q % 2 == 0:
                            kb.act(lambda a: a.copy(out=o_ap, in_=i_ap), reads=[PK[bank]], writes=[ytk])
                        else:
                            kb.dve(lambda v: v.tensor_copy(out=o_ap, in_=i_ap), reads=[PK[bank]], writes=[ytk])
                    if c % 4 == 3:
                        tl = c // 4
                        kb.dma("sp", YN_T.rearrange("(k p) t -> p k t", p=128)[:, :, tl * 512:(tl + 1) * 512], yt[:], reads=[ytk], writes=["D:YN_T"], sem="styn%d" % (tl % 2))
            if S_out_slot is not None and mode != "M":
                kb.dve(lambda v: v.tensor_copy(out=S_out_slot, in_=Sf[:]), reads=["Sf%d" % g for g in range(4)], writes=["S_init"])
            kb.barrier()

    if not SKIP:
        ssd_phase(XBC_C, "D:XBCC", dt_ctx, 2, "F", None, S_init[:, 0, :])
        ssd_phase(XBC_C, "D:XBCC", dt_ctx, 2, "B", None, S_init[:, 1, :])
    if stop == "ctxssd":
        if DBG_A is not None:
            kb.dma("sp", DBG_A[:, 0:4096], S_init[:].rearrange("p a b -> p (a b)"), reads=["S_init"], writes=["D:DBG_A"], sem="st0")
        kb.barrier()
        return nc
    if not SKIP:
        ssd_phase(XBC_T, "D:XBCL", dt_all, NCH, "B", S_init[:, 1, :], None)
    if stop_here("ssdB"):
        return nc
    if not SKIP:
        ssd_phase(XBC_T, "D:XBCL", dt_all, NCH, "M", S_init[:, 0, :], None)
    ssd_es.close()
    if stop_here("ssdM"):
        return nc

    with ExitStack() as pes:
      if not SKIP:
            wsr = kb.sb(pes, "wsr", [128, 8, 128], F32)
            wsTf = kb.sb(pes, "wsTf", [128, 8, 128], F32)
            wsTb = kb.sb(pes, "wsTb", [128, 8, 128], BF16)
            Bbc = kb.sb(pes, "Bbc", [128, D], F32)
            bsr = kb.sb(pes, "bsr", [1, 8, 128], F32)
            Rc = kb.sb(pes, "Rc", [128, 16, 128], F32)
            vgs = [kb.sb(pes, "vg%d" % i, [128, D], F32) for i in range(2)]
            vh = [kb.sb(pes, "vh%d" % i, [128, D], BF16) for i in range(4)]
            uT = [kb.sb(pes, "uT%d" % i, [128, KD, 512], F32) for i in range(1)]
            cmT = [kb.sb(pes, "cmT%d" % i, [128, KD, 512], BF16) for i in range(1)]
            tmpc = [kb.sb(pes, "tmpc%d" % i, [128, 512], F32) for i in range(2)]
            bst = kb.sb(pes, "bstc", [128, 4, 6], F32)
            mv = kb.sb(pes, "mvc", [128, 4], F32)
            col_load(cols[:, C_CMG, :], cmlp_ln_g, 16, "colsCMG")
            kb.dma("sp", wsr[:], cmlp_ws.rearrange("g t s -> t g s"), writes=["wsr"], sem="k_wsr")
            row_load(Bbc[:], cmlp_ln_b, "Bbc")
            kb.dma("sp", bsr[0:1, :, :], cmlp_bs.rearrange("g t -> (g t)").rearrange("(o g t) -> o g t", o=1, g=8), writes=["bsr"], sem="k_bsr")
            for g in range(8):
                kb.tr(PS[0][:, g % 4 * 128:(g % 4 + 1) * 128], wsr[:, g, :], ident, reads=["wsr", "cm"], writes=[PK[0]])
                kb.dve(lambda v: v.tensor_copy(out=wsTf[:, g, :], in_=PS[0][:, g % 4 * 128:(g % 4 + 1) * 128]), reads=[PK[0]], writes=["wsTf"])
            kb.dve(lambda v: v.tensor_copy(out=wsTb[:], in_=wsTf[:]), reads=["wsTf"], writes=["wsTb"])
            for blk in range(16):
                g = blk // 2
                kb.mm(PS[1][:, 0:128], [(Bbc[:, blk * 128:(blk + 1) * 128], wsTf[:, g, :]), (cm[0:1, ONES, :], bsr[0:1, g, :])],
                      reads=["Bbc", "wsTf", "bsr", "cm"], writes=[PK[1]])
                kb.dve(lambda v: v.tensor_copy(out=Rc[:, blk, :], in_=PS[1][:, 0:128]), reads=[PK[1]], writes=["Rc"])
            utv = U_T.rearrange("(k p) t -> p k t", p=128)
            for tl in range(T // 512):
                kb.dma("sp", uT[0][:], utv[:, :, tl * 512:(tl + 1) * 512], reads=["D:u"], writes=["uT0"], sem="ldu0")
                for ci in range(4):
                    c = tl * 4 + ci
                    vg = vgs[c % 2]; vk = "vg%d" % (c % 2)
                    kb.dma("sp", vg[:], VG[c * 128:(c + 1) * 128, :], reads=["D:v"], writes=[vk], sem="ldv%d" % (c % 2))
                    for q in range(4):
                        kb.dve(lambda v: v.bn_stats(out=bst[:, q, :], in_=vg[:, q * 512:(q + 1) * 512]), reads=[vk], writes=["bstc"])
                    kb.dve(lambda v: v.bn_aggr(out=mv[:, 0:2], in_=bst[:].rearrange("p a b -> p (a b)")), reads=["bstc"], writes=["mvc"])
                    kb.act(lambda a: a.activation(out=mv[:, 2:3], in_=mv[:, 1:2], func=AF.Sqrt, bias=EPS), reads=["mvc"], writes=["mvc2"])
                    kb.dve(lambda v: v.reciprocal(out=mv[:, 2:3], in_=mv[:, 2:3]), reads=["mvc2"], writes=["mvc2"])
                    kb.dve(lambda v: v.scalar_tensor_tensor(out=mv[:, 3:4], in0=mv[:, 0:1], scalar=-1.0, in1=mv[:, 2:3], op0=ALU.mult, op1=ALU.mult),
                           reads=["mvc", "mvc2"], writes=["mvc3"])
                    kb.act(lambda a: a.activation(out=vh[ci][:], in_=vg[:], func=AF.Identity, scale=mv[:, 2:3], bias=mv[:, 3:4]),
                           reads=[vk, "mvc2", "mvc3"], writes=["vh%d" % ci])
                ct = cmT[0]; ck = "cmT0"
                for blk in range(16):
                    g = blk // 2
                    bank = 2 + blk % 4
                    for ci in range(4):
                        kb.mm(PS[bank][:, ci * 128:(ci + 1) * 128], [(vh[ci][:, blk * 128:(blk + 1) * 128], wsTb[:, g, :])],
                              reads=["vh%d" % ci, "wsTb"], writes=[PK[bank]])
                    tp = tmpc[blk % 2]; tk = "tmpc%d" % (blk % 2)
                    kb.dve(lambda v: v.scalar_tensor_tensor(out=tp[:].rearrange("p (a b) -> p a b", b=128), in0=PS[bank][:, :].rearrange("p (a b) -> p a b", b=128),
                                                            scalar=cols[:, C_CMG, blk:blk + 1], in1=Rc[:, blk, :].unsqueeze(1).to_broadcast([128, 4, 128]),
                                                            op0=ALU.mult, op1=ALU.add),
                           reads=[PK[bank], "colsCMG", "Rc"], writes=[tk])
                    kb.dve(lambda g_: g_.tensor_tensor(out=ct[:, blk, :], in0=tp[:], in1=uT[0][:, blk, :], op=ALU.mult),
                            reads=[tk, "uT0"], writes=[ck])
                kb.dma("sp", CM_T.rearrange("(k p) t -> p k t", p=128)[:, :, tl * 512:(tl + 1) * 512], ct[:], reads=[ck], writes=["D:CM_T"], sem="stcm%d" % (tl % 2))
            if stop == "cmlp" and DBG_A is not None:
                kb.dma("sp", DBG_A[:, 0:2048], Rc[:].rearrange("p a b -> p (a b)"), reads=["Rc"], writes=["D:DBG_A"], sem="st0")
                kb.dma("sp", DBG_A[:, 2048:3072], wsTf[:].rearrange("p a b -> p (a b)"), reads=["wsTf"], writes=["D:DBG_A"], sem="st0")
                kb.dma("sp", DBG_A[:, 3072:3088], cols[:, C_CMG, :], reads=["colsCMG"], writes=["D:DBG_A"], sem="st0")
            kb.barrier()

    if stop_here("cmlp"):
        return nc
    def load_w2048(wres, wsrc, key):
        wv = wsrc.rearrange("(kc p) n -> p kc n", p=128)
        for i in range(4):
            kb.dma("pool", wres[:, :, i * 512:(i + 1) * 512], wv[:, :, i * 512:(i + 1) * 512], writes=[key], sem="ldwr")

    def gated_proj(wsrc, inT, inkey, gateT, gkey, second):
        with ExitStack() as pes:
            wres = kb.sb(pes, "wres", [128, KD, D], BF16)
            ins = [kb.sb(pes, "gin%d" % i, [128, KD, 512], BF16) for i in range(2)]
            gts = [kb.sb(pes, "ggt%d" % i, [128, 512], F32) for i in range(2)]
            pts_ = [kb.sb(pes, "gpt%d" % i, [128, 512], F32) for i in range(2)]
            so = [kb.sb(pes, "gso%d" % i, [128, 512], F32) for i in range(2)]
            sob = [kb.sb(pes, "gsob%d" % i, [128, 512], BF16) for i in range(2)]
            load_w2048(wres, wsrc, "wres")
            iv = inT.rearrange("(k p) t -> p k t", p=128)
            n = 0
            for tl in range(T // 512):
                ts_ = slice(tl * 512, (tl + 1) * 512)
                kb.dma("sp", ins[tl % 2][:], iv[:, :, ts_], reads=[inkey], writes=["gin%d" % (tl % 2)], sem="ldgi%d" % (tl % 2))
                for cb in range(16):
                    rs = slice(cb * 128, (cb + 1) * 128)
                    j = n % 2
                    kb.dma("sp", gts[j][:], gateT[rs, ts_], reads=[gkey], writes=["ggt%d" % j], sem="ldgg%d" % j)
                    if second:
                        kb.dma("sp", pts_[j][:], PART_T[rs, ts_], reads=["D:PART_T"], writes=["gpt%d" % j], sem="ldgp%d" % j)
                    bank = n % 4
                    kb.mm(PS[bank][:, :], [(wres[:, k, rs], ins[tl % 2][:, k, :]) for k in range(KD)], reads=["wres", "gin%d" % (tl % 2)], writes=[PK[bank]])
                    if not second:
                        kb.dve(lambda v: v.tensor_tensor(out=so[j][:], in0=PS[bank][:, :], in1=gts[j][:], op=ALU.mult), reads=[PK[bank], "ggt%d" % j], writes=["gso%d" % j])
                        kb.dma("sp", PART_T[rs, ts_], so[j][:], reads=["gso%d" % j], writes=["D:PART_T"], sem="stgo%d" % j)
                    else:
                        kb.dve(lambda v: v.tensor_tensor(out=so[j][:], in0=PS[bank][:, :], in1=gts[j][:], op=ALU.mult), reads=[PK[bank], "ggt%d" % j], writes=["gso%d" % j])
                        kb.dve(lambda g_: g_.tensor_tensor(out=sob[j][:], in0=so[j][:], in1=pts_[j][:], op=ALU.add), reads=["gso%d" % j, "gpt%d" % j], writes=["gsob%d" % j])
                        kb.dma("sp", MG_T[rs, ts_], sob[j][:], reads=["gsob%d" % j], writes=["D:MG_T"], sem="stgo%d" % j)
                    n += 1
            kb.barrier()

    if not SKIP:
        gated_proj(w_ssd_br, YN_T, "D:YN_T", GS_T, "D:gs", False)
    if stop_here("gp1"):
        return nc
    if not SKIP:
        gated_proj(w_cmlp_br, CM_T, "D:CM_T", GC_T, "D:gc", True)
    if stop_here("gp"):
        return nc

    kb.dve(lambda v: v.memset(gates[:, :, NE:NE + 1], 1.0), writes=["gates1"])
    with ExitStack() as pes:
        wres = kb.sb(pes, "wres_o", [128, KD, D], BF16)
        g1r = kb.sb(pes, "g1r", [128, D], F32)
        l1g = kb.sb(pes, "l1g", [128, D], F32)
        l1b = kb.sb(pes, "l1b", [128, D], F32)
        mgs = [kb.sb(pes, "mg%d" % i, [128, KD, 512], BF16) for i in range(1)]
        hls = [kb.sb(pes, "hl3%d" % i, [128, D], F32) for i in range(2)]
        rss = [kb.sb(pes, "res%d" % i, [128, D], F32) for i in range(2)]
        a2f = kb.sb(pes, "a2f", [128, KD, 128], F32)
        a2s = [kb.sb(pes, "a2s%d" % i, [128, KD, 512], BF16) for i in range(1)]
        wr = kb.sb(pes, "wr", [128, KD, NE], F32)
        bst = kb.sb(pes, "bst3", [128, 4, 6], F32)
        mv = kb.sb(pes, "mv3", [128, 4], F32)
        rt = kb.sb(pes, "rt", [128, 10, 64], F32)
        load_w2048(wres, w_o, "wres_o")
        row_load(g1r[:], ADAFLAT[2 * D:3 * D], "g1r")
        kb._deps("sp", ["D:ADAROW"], ())
        row_load(l1g[:], ln1_g, "l1g")
        row_load(l1b[:], ln1_b, "l1b")
        kb.dma("sp", wr[:], w_router.rearrange("(k p) e -> p k e", p=128), writes=["wr"], sem="k_wr")
        mv_ = MG_T.rearrange("(k p) t -> p k t", p=128)
        SCR, BIA, M8, GSC, T8, GM, M1, MSK, SEL, WW = range(10)
        for tl in range(dbg.get("p3c_tiles", T // 512)):
            mg = mgs[0]; mk = "mg0"
            kb.dma("sp", mg[:], mv_[:, :, tl * 512:(tl + 1) * 512], reads=["D:MG_T"], writes=[mk], sem="ldmg0")
            a2 = a2s[0]; a2k = "a2s0"
            for tc in range(4):
                c = tl * 4 + tc
                hl = hls[c % 2]; hk = "hl3%d" % (c % 2)
                rs_ = rss[c % 2]; rk = "res%d" % (c % 2)
                kb.dma("sp", hl[:], XN[c * 128:(c + 1) * 128, :], reads=["D:XN"], writes=[hk], sem="ldh%d" % (c % 2))
                CUT = dbg.get("p3c_cut", 99)
                if CUT <= 1:
                    continue
                for db in range(4):
                    bank = db
                    ds_ = slice(db * 512, (db + 1) * 512)
                    kb.mm(PS[bank][:, :], [(mg[:, k, tc * 128:(tc + 1) * 128], wres[:, k, ds_]) for k in range(KD)], reads=[mk, "wres_o"], writes=[PK[bank]])
                    kb.dve(lambda v: v.tensor_tensor(out=rs_[:, ds_], in0=PS[bank][:, :], in1=g1r[:, ds_], op=ALU.mult), reads=[PK[bank], "g1r"], writes=[rk + "_%d" % db, rk])
                rks = [rk + "_%d" % db for db in range(4)]
                if CUT <= 2:
                    continue
                kb.dve(lambda v: v.scalar_tensor_tensor(out=rs_[:], in0=hl[:], scalar=ALPHA, in1=rs_[:], op0=ALU.mult, op1=ALU.add), reads=rks + [hk], writes=[rk])
                for q in range(4):
                    kb.dve(lambda v: v.bn_stats(out=bst[:, q, :], in_=rs_[:, q * 512:(q + 1) * 512]), reads=[rk], writes=["bst3"])
                kb.dve(lambda v: v.bn_aggr(out=mv[:, 0:2], in_=bst[:].rearrange("p a b -> p (a b)")), reads=["bst3"], writes=["mv3"])
                kb.act(lambda a: a.activation(out=mv[:, 2:3], in_=mv[:, 1:2], func=AF.Sqrt, bias=EPS), reads=["mv3"], writes=["mv32"])
                kb.dve(lambda v: v.reciprocal(out=mv[:, 2:3], in_=mv[:, 2:3]), reads=["mv32"], writes=["mv32"])
                kb.dve(lambda v: v.scalar_tensor_tensor(out=mv[:, 3:4], in0=mv[:, 0:1], scalar=-1.0, in1=mv[:, 2:3], op0=ALU.mult, op1=ALU.mult),
                       reads=["mv3", "mv32"], writes=["mv33"])
                kb.act(lambda a: a.activation(out=rs_[:], in_=rs_[:], func=AF.Identity, scale=mv[:, 2:3], bias=mv[:, 3:4]), reads=[rk, "mv32", "mv33"], writes=[rk])
                kb.dve(lambda v: v.tensor_tensor(out=rs_[:], in0=rs_[:], in1=l1g[:], op=ALU.mult), reads=[rk, "l1g"], writes=[rk])
                kb.dve(lambda g_: g_.tensor_tensor(out=rs_[:], in0=rs_[:], in1=l1b[:], op=ALU.add), reads=[rk, "l1b"], writes=[rk] + rks)
                if CUT <= 3:
                    continue
                kb.dma("sp", H1[c * 128:(c + 1) * 128, :], rs_[:], reads=[rk], writes=["D:H1"], sem="sth%d" % (c % 2))
                if CUT <= 4:
                    continue
                for q in range(4):
                    bank = 4 + q
                    for j in range(4):
                        dk = q * 4 + j
                        kb.tr(PS[bank][:, j * 128:(j + 1) * 128], rs_[:, dk * 128:(dk + 1) * 128], ident, reads=[rk, "cm"], writes=[PK[bank]])
                    for j in range(4):
                        dk = q * 4 + j
                        kb.dve(lambda v: v.tensor_scalar(out=a2f[:, dk, :], in0=PS[bank][:, j * 128:(j + 1) * 128], scalar1=cols[:, C_A2, dk:dk + 1],
                                                         scalar2=cols[:, C_B2, dk:dk + 1], op0=ALU.mult, op1=ALU.add),
                               reads=[PK[bank], "colsA2", "colsB2"], writes=["a2f%d" % q])
                    kb.act(lambda a: a.copy(out=a2[:, q * 4:q * 4 + 4, tc * 128:(tc + 1) * 128], in_=a2f[:, q * 4:q * 4 + 4, :]),
                           reads=["a2f%d" % q], writes=[a2k])
                if not dbg.get("norouter"):
                    kb.mm(PS[0][:, 0:NE], [(a2f[:, dk, :], wr[:, dk, :]) for dk in range(KD)], reads=["a2f%d" % q for q in range(4)] + ["wr"], writes=[PK[0]])
                    kb.act(lambda a: a.activation(out=rt[:, SCR, :], in_=PS[0][:, 0:NE], func=AF.Sigmoid), reads=[PK[0]], writes=["rt"])
                    R = lambda fn, **kw: kb.dve(fn, reads=["rt", "rows64"], writes=["rt"])
                    R(lambda v: v.tensor_tensor(out=rt[:, BIA, :], in0=rt[:, SCR, :], in1=rows64[:, 2, :], op=ALU.add))
                    for g in range(8):
                        R(lambda v: v.max(out=rt[:, M8, g * 8:(g + 1) * 8], in_=rt[:, BIA, g * 8:(g + 1) * 8]))
                    m8v = rt[:, M8, :].rearrange("p (g e) -> p g e", e=8)
                    R(lambda v: v.tensor_tensor(out=rt[:, GSC, 0:8], in0=m8v[:, :, 0], in1=m8v[:, :, 1], op=ALU.add))
                    R(lambda v: v.max(out=rt[:, T8, 0:8], in_=rt[:, GSC, 0:8]))
                    R(lambda v: v.tensor_scalar(out=rt[:, GM, 0:8], in0=rt[:, GSC, 0:8], scalar1=rt[:, T8, 3:4], scalar2=None, op0=ALU.is_ge))
                    R(lambda v: v.tensor_scalar(out=rt[:, M1, 0:8], in0=rt[:, GM, 0:8], scalar1=4.0, scalar2=-4.0, op0=ALU.mult, op1=ALU.add))
                    R(lambda v: v.tensor_tensor(out=rt[:, MSK, :].rearrange("p (g e) -> p g e", e=8), in0=rt[:, BIA, :].rearrange("p (g e) -> p g e", e=8),
                                                in1=rt[:, GM, 0:8].unsqueeze(2).to_broadcast([128, 8, 8]), op=ALU.mult))
                    R(lambda v: v.tensor_tensor(out=rt[:, MSK, :].rearrange("p (g e) -> p g e", e=8), in0=rt[:, MSK, :].rearrange("p (g e) -> p g e", e=8),
                                                in1=rt[:, M1, 0:8].unsqueeze(2).to_broadcast([128, 8, 8]), op=ALU.add))
                    R(lambda v: v.max(out=rt[:, T8, 8:16], in_=rt[:, MSK, :]))
                    R(lambda v: v.tensor_scalar(out=rt[:, SEL, :], in0=rt[:, MSK, :], scalar1=rt[:, T8, 15:16], scalar2=None, op0=ALU.is_ge))
                    R(lambda v: v.tensor_tensor(out=rt[:, WW, :], in0=rt[:, SCR, :], in1=rt[:, SEL, :], op=ALU.mult))
                    R(lambda v: v.tensor_reduce(out=rt[:, T8, 16:17], in_=rt[:, WW, :], axis=mybir.AxisListType.X, op=ALU.add))
                    R(lambda v: v.reciprocal(out=rt[:, T8, 16:17], in_=rt[:, T8, 16:17]))
                    kb.dve(lambda v: v.tensor_scalar(out=gates[:, c, 0:NE], in0=rt[:, WW, :], scalar1=rt[:, T8, 16:17], scalar2=2.5, op0=ALU.mult, op1=ALU.mult),
                           reads=["rt"], writes=["gates"])
            kb.dma("sp", A2_T.rearrange("(k p) t -> p k t", p=128)[:, :, tl * 512:(tl + 1) * 512], a2[:], reads=[a2k], writes=["D:A2_T"], sem="sta2%d" % (tl % 2))
        kb.barrier()

    if stop == "p3c":
        if DBG_A is not None:
            kb.dma("sp", DBG_A[:, 0:NCH * (NE + 1)], gates[:].rearrange("p a b -> p (a b)"), reads=["gates", "gates1"], writes=["D:DBG_A"], sem="st0")
        kb.barrier()
        return nc
    with ExitStack() as pes:
        acc = kb.sb(pes, "acc", [128, 8, D], F32)
        a2t = [kb.sb(pes, "a2t%d" % i, [128, KD, 512], BF16) for i in range(2)]
        wg = [kb.sb(pes, "wg%d" % i, [128, KD, 256], BF16) for i in range(2)]
        wu = [kb.sb(pes, "wu%d" % i, [128, KD, 256], BF16) for i in range(2)]
        wd = [kb.sb(pes, "wd%d" % i, [128, 2, D], BF16) for i in range(2)]
        sgs = [kb.sb(pes, "sg%d" % i, [128, 512], F32) for i in range(2)]
        hT = [kb.sb(pes, "hT%d" % i, [128, 2, 512], BF16) for i in range(2)]
        a2v = A2_T.rearrange("(k p) t -> p k t", p=128)
        NHE = 2 * (NE + 1)

        def wsrc(he):
            e, hf = he // 2, he % 2
            cs = slice(hf * 256, (hf + 1) * 256)
            if e < NE:
                return (w_e_gate[e].rearrange("(k p) n -> p k n", p=128)[:, :, cs], w_e_up[e].rearrange("(k p) n -> p k n", p=128)[:, :, cs],
                        w_e_down[e].rearrange("(j p) n -> p j n", p=128)[:, hf * 2:hf * 2 + 2, :])
            return (w_sh_gate.rearrange("(k p) n -> p k n", p=128)[:, :, cs], w_sh_up.rearrange("(k p) n -> p k n", p=128)[:, :, cs],
                    w_sh_down.rearrange("(j p) n -> p j n", p=128)[:, hf * 2:hf * 2 + 2, :])

        def load_he(n, he):
            sg_, su_, sd_ = wsrc(he)
            j = n % 2
            kb.dma("pool", wg[j][:], sg_, writes=["wg%d" % j], sem="ldwg%d" % j)
            kb.dma("pool", wu[j][:], su_, writes=["wu%d" % j], sem="ldwu%d" % j)
            kb.dma("pool", wd[j][:], sd_, writes=["wd%d" % j], sem="ldwd%d" % j)

        pbc = [0]

        def gateup_groups(st, he, n, tt):
            j = n % 2
            h = hT[tt]; hk = "hT%d" % tt
            items = []
            for jb in range(2):
                def f(jb=jb):
                    bg, bu = pbc[0] % 4, (pbc[0] + 1) % 4
                    pbc[0] += 2
                    kb.mm(PS[bg][:, :], [(wg[j][:, k, jb * 128:(jb + 1) * 128], a2t[tt][:, k, :]) for k in range(KD)], reads=["wg%d" % j, "a2t%d" % tt], writes=[PK[bg]])
                    kb.mm(PS[bu][:, :], [(wu[j][:, k, jb * 128:(jb + 1) * 128], a2t[tt][:, k, :]) for k in range(KD)], reads=["wu%d" % j, "a2t%d" % tt], writes=[PK[bu]])
                    sg_ = sgs[jb]; sgk = "sg%d" % jb
                    kb.act(lambda a: a.activation(out=sg_[:], in_=PS[bg][:, :], func=AF.Silu), reads=[PK[bg]], writes=[sgk])
                    kb.dve(lambda v: v.tensor_tensor(out=h[:, jb, :], in0=sg_[:], in1=PS[bu][:, :], op=ALU.mult), reads=[sgk, PK[bu]], writes=[hk + "_%d" % jb])
                items.append(f)
            return items

        def down_groups(st, he, n, tt):
            j = n % 2
            e = he // 2
            h = hT[tt]; hk = "hT%d" % tt
            items = []
            for tc in range(4):
                for db in range(4):
                    def f(tc=tc, db=db):
                        ch = tt * 4 + tc
                        c = st * 8 + ch
                        bo = 4 + (db % 4)
                        ds_ = slice(db * 512, (db + 1) * 512)
                        kb.mm(PS[bo][:, :], [(h[:, jb, tc * 128:(tc + 1) * 128], wd[j][:, jb, ds_]) for jb in range(2)],
                              reads=[hk + "_0", hk + "_1", "wd%d" % j], writes=[PK[bo]])
                        ak = "acc%d_%d" % (ch, db)
                        if he == 0:
                            kb.dve(lambda v: v.tensor_scalar(out=acc[:, ch, ds_], in0=PS[bo][:, :], scalar1=gates[:, c, e:e + 1], scalar2=None, op0=ALU.mult),
                                   reads=[PK[bo], "gates", "gates1"], writes=[ak])
                        else:
                            kb.dve(lambda v: v.scalar_tensor_tensor(out=acc[:, ch, ds_], in0=PS[bo][:, :], scalar=gates[:, c, e:e + 1], in1=acc[:, ch, ds_],
                                                                    op0=ALU.mult, op1=ALU.add),
                                   reads=[PK[bo], "gates", "gates1", ak], writes=[ak])
                    items.append(f)
            return items

        n = 0
        for st in range(4):
            for tt in range(2):
                kb.dma("sp", a2t[tt][:], a2v[:, :, st * 1024 + tt * 512:st * 1024 + (tt + 1) * 512], reads=["D:A2_T"], writes=["a2t%d" % tt], sem="lda2%d" % tt)
            units = [(he, tt) for he in range(NHE) for tt in range(2)]
            load_he(n, 0)
            for it in gateup_groups(st, 0, n, 0):
                it()
            for ui, (he, tt) in enumerate(units):
                if tt == 0 and he + 1 < NHE:
                    load_he(n + 1, he + 1)
                dn = down_groups(st, he, n, tt)
                if ui + 1 < len(units):
                    he2, tt2 = units[ui + 1]
                    gu = gateup_groups(st, he2, n + (1 if he2 != he else 0), tt2)
                else:
                    gu = []
                for di, d_ in enumerate(dn):
                    d_()
                    if di == 3 and len(gu) > 0:
                        gu[0]()
                    if di == 11 and len(gu) > 1:
                        gu[1]()
                if tt == 1:
                    n += 1
            for ch in range(8):
                c = st * 8 + ch
                kb.dma("sp", FOUT[c * 128:(c + 1) * 128, :], acc[:, ch, :], reads=["acc%d_%d" % (ch, db) for db in range(4)], writes=["D:FOUT"], sem="stfo%d" % ch)
        kb.barrier()

    if stop_here("moe"):
        return nc
    with ExitStack() as pes:
        g2r = kb.sb(pes, "g2r", [128, D], F32)
        l2g = kb.sb(pes, "l2g", [128, D], F32)
        l2b = kb.sb(pes, "l2b", [128, D], F32)
        fs = [kb.sb(pes, "ff%d" % i, [128, D], F32) for i in range(2)]
        hs = [kb.sb(pes, "fh%d" % i, [128, D], F32) for i in range(2)]
        bst = kb.sb(pes, "bstf", [128, 4, 6], F32)
        mv = kb.sb(pes, "mvf", [128, 4], F32)
        row_load(g2r[:], ADAFLAT[5 * D:6 * D], "g2r")
        row_load(l2g[:], ln2_g, "l2g")
        row_load(l2b[:], ln2_b, "l2b")
        for c in range(NCH):
            f = fs[c % 2]; fk = "ff%d" % (c % 2)
            h = hs[c % 2]; hk = "fh%d" % (c % 2)
            kb.dma("sp", f[:], FOUT[c * 128:(c + 1) * 128, :], reads=["D:FOUT"], writes=[fk], sem="ldf%d" % (c % 2))
            kb.dma("sp", h[:], H1[c * 128:(c + 1) * 128, :], reads=["D:H1"], writes=[hk], sem="ldfh%d" % (c % 2))
            kb.dve(lambda g_: g_.tensor_tensor(out=f[:], in0=f[:], in1=g2r[:], op=ALU.mult), reads=[fk, "g2r"], writes=[fk])
            kb.dve(lambda v: v.scalar_tensor_tensor(out=f[:], in0=h[:], scalar=ALPHA, in1=f[:], op0=ALU.mult, op1=ALU.add), reads=[fk, hk], writes=[fk])
            for q in range(4):
                kb.dve(lambda v: v.bn_stats(out=bst[:, q, :], in_=f[:, q * 512:(q + 1) * 512]), reads=[fk], writes=["bstf"])
            kb.dve(lambda v: v.bn_aggr(out=mv[:, 0:2], in_=bst[:].rearrange("p a b -> p (a b)")), reads=["bstf"], writes=["mvf"])
            kb.act(lambda a: a.activation(out=mv[:, 2:3], in_=mv[:, 1:2], func=AF.Sqrt, bias=EPS), reads=["mvf"], writes=["mvf2"])
            kb.dve(lambda v: v.reciprocal(out=mv[:, 2:3], in_=mv[:, 2:3]), reads=["mvf2"], writes=["mvf2"])
            kb.dve(lambda v: v.scalar_tensor_tensor(out=mv[:, 3:4], in0=mv[:, 0:1], scalar=-1.0, in1=mv[:, 2:3], op0=ALU.mult, op1=ALU.mult),
                   reads=["mvf", "mvf2"], writes=["mvf3"])
            kb.act(lambda a: a.activation(out=f[:], in_=f[:], func=AF.Identity, scale=mv[:, 2:3], bias=mv[:, 3:4]), reads=[fk, "mvf2", "mvf3"], writes=[fk])
            kb.dve(lambda v: v.tensor_tensor(out=f[:], in0=f[:], in1=l2g[:], op=ALU.mult), reads=[fk, "l2g"], writes=[fk])
            kb.dve(lambda g_: g_.tensor_tensor(out=f[:], in0=f[:], in1=l2b[:], op=ALU.add), reads=[fk, "l2b"], writes=[fk])
            kb.dma("sp", y[c * 128:(c + 1) * 128, :], f[:], reads=[fk], writes=["D:y"], sem="sty%d" % (c % 2))
    kb.finish(["D:y"])
    kb.barrier()
    return nc


def _prep_inputs(inputs):
    f = lambda a: np.ascontiguousarray(np.asarray(a, dtype=np.float32))
    sq = lambda a: f(a)[0]
    shared = {
        "c_ctx": f(inputs["c_ctx"]), "ln_in_g": f(inputs["ln_in_g"]), "ln_in_b": f(inputs["ln_in_b"]),
        "w_ada": sq(inputs["w_ada"]), "b_ada": sq(inputs["b_ada"]), "w_in": sq(inputs["w_in"]),
        "conv_w": sq(inputs["conv_w"]), "conv_b": sq(inputs["conv_b"]),
        "dt_bias": sq(inputs["dt_bias"]).reshape(64), "a_log": sq(inputs["a_log"]).reshape(64),
        "d_skip": sq(inputs["d_skip"]), "ssd_norm_w": sq(inputs["ssd_norm_w"]), "w_ssd_br": sq(inputs["w_ssd_br"]),
        "cmlp_ln_g": sq(inputs["cmlp_ln_g"]), "cmlp_ln_b": sq(inputs["cmlp_ln_b"]), "cmlp_ws": sq(inputs["cmlp_ws"]),
        "cmlp_bs": sq(inputs["cmlp_bs"]), "w_cmlp_br": sq(inputs["w_cmlp_br"]), "w_o": sq(inputs["w_o"]),
        "ln1_g": sq(inputs["ln1_g"]), "ln1_b": sq(inputs["ln1_b"]), "w_router": sq(inputs["w_router"]),
        "router_bias": sq(inputs["router_bias"]), "w_e_gate": sq(inputs["w_e_gate"]), "w_e_up": sq(inputs["w_e_up"]),
        "w_e_down": sq(inputs["w_e_down"]), "w_sh_gate": sq(inputs["w_sh_gate"]), "w_sh_up": sq(inputs["w_sh_up"]),
        "w_sh_down": sq(inputs["w_sh_down"]), "ln2_g": sq(inputs["ln2_g"]), "ln2_b": sq(inputs["ln2_b"]),
    }
    i = np.arange(128)
    lp, l = i[:, None], i[None, :]
    masks = np.stack([(lp == l), (lp <= l), (lp > l), (lp >= l), (lp < l), np.ones((128, 128), bool)], axis=1)
    shared["cmask"] = np.ascontiguousarray(masks.astype(np.float32).reshape(128, 6 * 128))
    xs, cs, cx = f(inputs["x"]), f(inputs["c"]), f(inputs["ctx"])
    in_maps = []
    for b in range(8):
        m = dict(shared)
        m["x"] = xs[b]
        m["c"] = cs[b]
        m["ctx"] = cx[b]
        in_maps.append(m)
    return in_maps


def kernel(**inputs):
    nc = build_program(DEBUG)
    in_maps = _prep_inputs(inputs)
    res = run_bass_kernel_spmd(nc, in_maps, core_ids=list(range(8)))
    return np.stack([r["y"] for r in res.results], axis=0)
```

```python
import math
from contextlib import ExitStack

import numpy as np
import concourse.bass as bass
import concourse.mybir as mybir
from concourse.bass_utils import run_bass_kernel_spmd

F32 = mybir.dt.float32
BF16 = mybir.dt.bfloat16
I32 = mybir.dt.int32
AF = mybir.ActivationFunctionType
ALU = mybir.AluOpType

T = 4096
CTX = 256
D = 2048
KD = 16
NCH = T // 128
IN_DIM = 13376
XBC = 3072
NE = 64
ALPHA = 2.0 ** 0.25
EPS = 1e-5

DEBUG = None


class KB:
    def __init__(self):
        self.nc = bass.Bass("TRN2", target_bir_lowering=False)
        self.es = ExitStack()
        nc = self.nc
        self.eng = {"pe": nc.tensor, "dve": nc.vector, "act": nc.scalar, "pool": nc.gpsimd, "sp": nc.sync}
        self.sems = {}
        self.cnt = {}
        for e in self.eng:
            self.sems["s_" + e] = self.es.enter_context(nc.semaphore("s_" + e))
            self.cnt["s_" + e] = 0
        self.waited = {e: {} for e in self.eng}
        self.lastw = {}
        self.readers = {}
        self.same_sync = {"pe": False, "dve": True, "act": True, "pool": True, "sp": False}
        self.nps = 0

    def sb(self, es, name, shape, dt):
        self.nps += 1
        return es.enter_context(self.nc.sbuf_tensor("%s_%d" % (name, self.nps), shape, dt))

    def sem(self, name):
        if name not in self.sems:
            self.sems[name] = self.es.enter_context(self.nc.semaphore(name))
            self.cnt[name] = 0
        return self.sems[name]

    def _deps(self, e, reads, writes):
        req = {}

        def add(d):
            for sk, v in d.items():
                if req.get(sk, 0) < v:
                    req[sk] = v

        for k in reads:
            add(self.lastw.get(k, {}))
        for k in writes:
            add(self.lastw.get(k, {}))
            add(self.readers.get(k, {}))
        w = self.waited[e]
        for sk, v in req.items():
            if sk == "s_" + e and not self.same_sync[e]:
                continue
            if w.get(sk, 0) >= v:
                continue
            self.eng[e].wait_ge(self.sems[sk], v)
            w[sk] = v

    def _record(self, sk, v, reads, writes):
        for k in reads:
            d = self.readers.setdefault(k, {})
            if d.get(sk, 0) < v:
                d[sk] = v
        for k in writes:
            if k.startswith("D:"):
                d = self.lastw.setdefault(k, {})
                if d.get(sk, 0) < v:
                    d[sk] = v
            else:
                self.lastw[k] = {sk: v}
                self.readers[k] = {}

    def op(self, e, fn, reads=(), writes=()):
        self._deps(e, reads, writes)
        ins = fn(self.eng[e])
        sk = "s_" + e
        self.cnt[sk] += 1
        ins.then_inc(self.sems[sk], 1)
        self._record(sk, self.cnt[sk], reads, writes)
        return ins

    def dve(self, fn, reads=(), writes=()):
        return self.op("dve", fn, reads, writes)

    def act(self, fn, reads=(), writes=()):
        return self.op("act", fn, reads, writes)

    def pool(self, fn, reads=(), writes=()):
        return self.op("pool", fn, reads, writes)

    def mm(self, out, pairs, reads=(), writes=(), first_start=True):
        self._deps("pe", reads, writes)
        n = len(pairs)
        ins = None
        for i, (l, r) in enumerate(pairs):
            ins = self.nc.tensor.matmul(out, lhsT=l, rhs=r, start=(i == 0 and first_start), stop=(i == n - 1))
        self.cnt["s_pe"] += 1
        ins.then_inc(self.sems["s_pe"], 1)
        self._record("s_pe", self.cnt["s_pe"], reads, writes)

    def tr(self, out, in_, ident, reads=(), writes=()):
        self._deps("pe", reads, writes)
        ins = self.nc.tensor.transpose(out=out, in_=in_, identity=ident)
        self.cnt["s_pe"] += 1
        ins.then_inc(self.sems["s_pe"], 1)
        self._record("s_pe", self.cnt["s_pe"], reads, writes)

    def dma(self, q, out, in_, reads=(), writes=(), sem="dm", **kw):
        self._deps(q, reads, writes)
        s = self.sem(sem)
        ins = self.eng[q].dma_start(out=out, in_=in_, **kw)
        self.cnt[sem] += 16
        ins.then_inc(s, 16)
        self._record(sem, self.cnt[sem], reads, writes)

    def barrier(self):
        for e in self.eng:
            w = self.waited[e]
            for sk, v in self.cnt.items():
                if v == 0 or sk == "s_" + e or w.get(sk, 0) >= v:
                    continue
                self.eng[e].wait_ge(self.sems[sk], v)
                w[sk] = v

    def finish(self, keys):
        self._deps("sp", keys, ())
        self._deps("act", keys, ())


def build_program(dbg=None):
    kb = KB()
    nc = kb.nc
    dbg = dbg or {}
    dump = set(dbg.get("dump", []))
    stop = dbg.get("stop")

    def din(name, shape):
        return nc.dram_tensor(name, list(shape), F32, kind="ExternalInput").ap()

    def dscr(name, shape, dt=F32):
        kind = "ExternalOutput" if name in dump else "Internal"
        return nc.dram_tensor(name, list(shape), dt, kind=kind).ap()

    x = din("x", [T, D])
    ctx = din("ctx", [CTX, D])
    cvec = din("c", [D])
    c_ctx = din("c_ctx", [D])
    ln_in_g = din("ln_in_g", [D])
    ln_in_b = din("ln_in_b", [D])
    w_ada = din("w_ada", [D, 6 * D])
    b_ada = din("b_ada", [6 * D])
    w_in = din("w_in", [D, IN_DIM])
    conv_w = din("conv_w", [5, XBC])
    conv_b = din("conv_b", [XBC])
    dt_bias = din("dt_bias", [64])
    a_log = din("a_log", [64])
    d_skip = din("d_skip", [32])
    ssd_norm_w = din("ssd_norm_w", [D])
    w_ssd_br = din("w_ssd_br", [D, D])
    cmlp_ln_g = din("cmlp_ln_g", [D])
    cmlp_ln_b = din("cmlp_ln_b", [D])
    cmlp_ws = din("cmlp_ws", [8, 128, 128])
    cmlp_bs = din("cmlp_bs", [8, 128])
    w_cmlp_br = din("w_cmlp_br", [D, D])
    w_o = din("w_o", [D, D])
    ln1_g = din("ln1_g", [D])
    ln1_b = din("ln1_b", [D])
    w_router = din("w_router", [D, NE])
    router_bias = din("router_bias", [NE])
    w_e_gate = din("w_e_gate", [NE, D, 512])
    w_e_up = din("w_e_up", [NE, D, 512])
    w_e_down = din("w_e_down", [NE, 512, D])
    w_sh_gate = din("w_sh_gate", [D, 512])
    w_sh_up = din("w_sh_up", [D, 512])
    w_sh_down = din("w_sh_down", [512, D])
    ln2_g = din("ln2_g", [D])
    ln2_b = din("ln2_b", [D])
    cmask = din("cmask", [128, 6 * 128])
    y = nc.dram_tensor("y", [T, D], F32, kind="ExternalOutput").ap()

    RPOS = dscr("RPOS", [64, 1024])
    ADAROW = dscr("ADAROW", [96, 128])
    XN = dscr("XN", [T, D])
    XBC_T = dscr("XBC_T", [XBC, T + 4], BF16)
    XBC_C = dscr("XBC_C", [XBC, CTX + 4], BF16)
    ZS = dscr("ZS", [T, D])
    U_T = dscr("U_T", [D, T])
    VG = dscr("VG", [T, D])
    GS_T = dscr("GS_T", [D, T])
    GC_T = dscr("GC_T", [D, T])
    SBW = dscr("SBW", [NCH, 128, D], BF16)
    YN_T = dscr("YN_T", [D, T], BF16)
    CM_T = dscr("CM_T", [D, T], BF16)
    PART_T = dscr("PART_T", [D, T])
    MG_T = dscr("MG_T", [D, T], BF16)
    H1 = dscr("H1", [T, D])
    A2_T = dscr("A2_T", [D, T], BF16)
    FOUT = dscr("FOUT", [T, D])
    DBG_A = dscr("DBG_A", [128, 4096]) if "DBG_A" in dump else None

    es = kb.es

    def stop_here(name):
        if stop != name:
            return False
        kb.barrier()
        return True

    PS = [es.enter_context(nc.psum_tensor("ps%d" % i, [128, 512], F32)) for i in range(8)]
    PK = ["ps%d" % i for i in range(8)]

    cm = kb.sb(es, "cm", [128, 6, 128], F32)
    IDN, MU, MSL, ML, MSU, ONES = 0, 1, 2, 3, 4, 5
    dt_all = kb.sb(es, "dt_all", [128, NCH, 64], F32)
    dt_ctx = kb.sb(es, "dt_ctx", [128, 2, 64], F32)
    gates = kb.sb(es, "gates", [128, NCH, NE + 1], F32)
    cols = kb.sb(es, "cols", [128, 40, 16], F32)
    adac = kb.sb(es, "adac", [128, 96, 2], F32)
    convw = kb.sb(es, "convw", [128, 5, 24], F32)
    convb = kb.sb(es, "convb", [128, 24], F32)
    rows64 = kb.sb(es, "rows64", [128, 4, 64], F32)
    stats = kb.sb(es, "stats", [128, 64], F32)
    tmpr = kb.sb(es, "tmpr", [128, 128], F32)
    C_LNG, C_LNB, C_A1L, C_B1L, C_A1C, C_B1C, C_A2, C_B2, C_CMG, C_T0, C_T1 = range(11)

    kb.dma("sp", cm[:].rearrange("p a b -> p (a b)"), cmask[:, :], writes=["cm"], sem="k_cm")
    ident = cm[:, IDN, :]

    def col_load(dst, src1d, n, key):
        kb.dma("sp", tmpr[0:n, :], src1d.rearrange("(j p) -> j p", p=128), writes=["tmpr"], sem="k_tmpr")
        kb.tr(PS[0][:, 0:n], tmpr[0:n, :], cm[0:n, IDN, 0:n], reads=["tmpr", "cm"], writes=[PK[0]])
        kb.dve(lambda v: v.tensor_copy(out=dst, in_=PS[0][:, 0:n]), reads=[PK[0]], writes=[key])

    def row_load(dst, src1d, key, q="sp", sem=None):
        kb.dma(q, dst, src1d.partition_broadcast(128), writes=[key], sem="k_" + key)

    col_load(cols[:, C_LNG, :], ln_in_g, 16, "cols")
    col_load(cols[:, C_LNB, :], ln_in_b, 16, "cols")
    for k in range(5):
        col_load(convw[:, k, :], conv_w[k, :], 24, "convw")
    col_load(convb[:, :], conv_b, 24, "convb")
    row_load(rows64[:, 0, :], a_log, "rows64")
    row_load(rows64[:, 1, :], dt_bias, "rows64")
    row_load(rows64[:, 2, :], router_bias, "rows64")
    row_load(rows64[:, 3, 0:32], d_skip, "rows64")
    kb.act(lambda a: a.activation(out=rows64[:, 0, :], in_=rows64[:, 0, :], func=AF.Exp), reads=["rows64"], writes=["rows64"])
    kb.dve(lambda v: v.tensor_scalar(out=rows64[:, 0, :], in0=rows64[:, 0, :], scalar1=-1.0, scalar2=None, op0=ALU.mult),
           reads=["rows64"], writes=["rows64"])

    with ExitStack() as pes:
        ccol = kb.sb(pes, "ccol", [128, 16, 2], F32)
        craw = kb.sb(pes, "craw", [128, 32], F32)
        bcol = kb.sb(pes, "bcol", [128, 96], F32)
        wts = [kb.sb(pes, "wada%d" % i, [128, 16, 512], F32) for i in range(2)]
        col_load(craw[:, 0:16], cvec, 16, "craw")
        col_load(craw[:, 16:32], c_ctx, 16, "craw")
        col_load(bcol[:, :], b_ada, 96, "bcol")
        kb.act(lambda a: a.activation(out=ccol[:, :, 0], in_=craw[:, 0:16], func=AF.Silu), reads=["craw"], writes=["ccol"])
        kb.act(lambda a: a.activation(out=ccol[:, :, 1], in_=craw[:, 16:32], func=AF.Silu), reads=["craw"], writes=["ccol"])
        wav = w_ada.rearrange("(kc p) n -> p kc n", p=128)
        NT_A = 24

        def load_wada(ct):
            kb.dma("sp", wts[ct % 2][:], wav[:, :, ct * 512:(ct + 1) * 512], writes=["wada%d" % (ct % 2)], sem="wada%d" % (ct % 2))

        load_wada(0)
        for ct in range(NT_A):
            if ct + 1 < NT_A:
                load_wada(ct + 1)
            wt = wts[ct % 2]
            bank = 1 + (ct % 2)
            for cb in range(4):
                kb.mm(PS[bank][:, cb * 2:cb * 2 + 2],
                      [(wt[:, kc, cb * 128:(cb + 1) * 128], ccol[:, kc, :]) for kc in range(16)],
                      reads=["wada%d" % (ct % 2), "ccol"], writes=[PK[bank]])
            j0 = ct * 4
            kb.dve(lambda v: v.tensor_tensor(out=adac[:, j0:j0 + 4, :],
                                             in0=PS[bank][:, 0:8].rearrange("p (a b) -> p a b", b=2),
                                             in1=bcol[:, j0:j0 + 4].unsqueeze(2).to_broadcast([128, 4, 2]), op=ALU.add),
                   reads=[PK[bank], "bcol"], writes=["adac"])
        for w, (ca, cbb) in enumerate(((C_A1L, C_B1L), (C_A1C, C_B1C))):
            kb.dve(lambda v: v.tensor_scalar(out=cols[:, C_T0, :], in0=adac[:, 16:32, w], scalar1=1.0, scalar2=None, op0=ALU.add),
                   reads=["adac"], writes=["colsT"])
            kb.dve(lambda v: v.tensor_tensor(out=cols[:, ca, :], in0=cols[:, C_LNG, :], in1=cols[:, C_T0, :], op=ALU.mult),
                   reads=["colsT", "cols"], writes=["colsA%d" % w])
            kb.dve(lambda v: v.tensor_tensor(out=cols[:, C_T1, :], in0=cols[:, C_LNB, :], in1=cols[:, C_T0, :], op=ALU.mult),
                   reads=["colsT", "cols"], writes=["colsT1"])
            kb.dve(lambda v: v.tensor_tensor(out=cols[:, cbb, :], in0=cols[:, C_T1, :], in1=adac[:, 0:16, w], op=ALU.add),
                   reads=["colsT1", "adac"], writes=["colsB%d" % w])
        kb.dve(lambda v: v.tensor_scalar(out=cols[:, C_A2, :], in0=adac[:, 64:80, 0], scalar1=1.0, scalar2=None, op0=ALU.add),
               reads=["adac"], writes=["colsA2"])
        kb.dve(lambda v: v.tensor_copy(out=cols[:, C_B2, :], in_=adac[:, 48:64, 0]), reads=["adac"], writes=["colsB2"])
        kb.dve(lambda v: v.tensor_copy(out=bcol[:, :], in_=adac[:, :, 0]), reads=["adac"], writes=["bcol"])
        kb.tr(PS[0][0:96, 0:128], bcol[:, 0:96], ident, reads=["bcol", "cm"], writes=[PK[0]])
        kb.dve(lambda v: v.tensor_copy(out=tmpr[0:96, :], in_=PS[0][0:96, 0:128]), reads=[PK[0]], writes=["tmpr"])
        kb.dma("sp", ADAROW[:, :], tmpr[0:96, :], reads=["tmpr"], writes=["D:ADAROW"], sem="st0")
        kb.barrier()
    ADAFLAT = ADAROW.rearrange("a b -> (a b)")

    PC = kb.sb(es, "PC", [128, 1024], F32)
    with ExitStack() as pes:
        ji = kb.sb(pes, "ji", [128, 512], I32)
        om = kb.sb(pes, "om", [128, 512], F32)
        pi_ = kb.sb(pes, "pi_", [128, 1], I32)
        pf = kb.sb(pes, "pf", [128, 2], F32)
        kf = kb.sb(pes, "kf", [128, 512], F32)
        ki = kb.sb(pes, "ki", [128, 512], I32)
        kb.pool(lambda g: g.iota(out=ji[:], pattern=[[1, 512]], base=0, channel_multiplier=0), writes=["ji"])
        kb.pool(lambda g: g.iota(out=pi_[:], pattern=[[1, 1]], base=0, channel_multiplier=1), writes=["pi_"])
        kb.dve(lambda v: v.tensor_copy(out=om[:], in_=ji[:]), reads=["ji"], writes=["om"])
        kb.dve(lambda v: v.tensor_copy(out=pf[:, 0:1], in_=pi_[:]), reads=["pi_"], writes=["pf"])
        kb.dve(lambda v: v.tensor_scalar(out=pf[:, 1:2], in0=pf[:, 0:1], scalar1=63.5, scalar2=-64.0, op0=ALU.is_gt, op1=ALU.mult),
               reads=["pf"], writes=["pf1"])
        kb.dve(lambda v: v.tensor_tensor(out=pf[:, 0:1], in0=pf[:, 0:1], in1=pf[:, 1:2], op=ALU.add), reads=["pf", "pf1"], writes=["pf"])
        kb.act(lambda a: a.activation(out=om[:], in_=om[:], func=AF.Exp, scale=-math.log(10000.0) / 512.0), reads=["om"], writes=["om"])
        kb.dve(lambda v: v.tensor_scalar(out=om[:], in0=om[:], scalar1=pf[:, 0:1], scalar2=None, op0=ALU.mult), reads=["om", "pf"], writes=["om"])
        for half, shift in ((0, 0.0), (1, math.pi / 2)):
            dst = PC[:, half * 512:(half + 1) * 512]
            key = "PC%d" % half
            kb.dve(lambda v: v.tensor_scalar(out=dst, in0=om[:], scalar1=shift, scalar2=None, op0=ALU.add), reads=["om"], writes=[key])
            kb.dve(lambda v: v.tensor_scalar(out=kf[:], in0=dst, scalar1=1.0 / (2 * math.pi), scalar2=None, op0=ALU.mult), reads=[key], writes=["kf"])
            kb.dve(lambda v: v.tensor_copy(out=ki[:], in_=kf[:]), reads=["kf"], writes=["ki"])
            kb.dve(lambda v: v.tensor_copy(out=kf[:], in_=ki[:]), reads=["ki"], writes=["kf"])
            kb.dve(lambda v: v.scalar_tensor_tensor(out=dst, in0=kf[:], scalar=-2 * math.pi, in1=dst, op0=ALU.mult, op1=ALU.add),
                   reads=["kf", key], writes=[key])
            kb.dve(lambda v: v.tensor_scalar(out=kf[:], in0=dst, scalar1=math.pi, scalar2=-2 * math.pi, op0=ALU.is_gt, op1=ALU.mult), reads=[key], writes=["kf"])
            kb.dve(lambda v: v.tensor_tensor(out=dst, in0=dst, in1=kf[:], op=ALU.add), reads=["kf", key], writes=[key])
            kb.dve(lambda v: v.tensor_scalar(out=kf[:], in0=dst, scalar1=-math.pi, scalar2=2 * math.pi, op0=ALU.is_lt, op1=ALU.mult), reads=[key], writes=["kf"])
            kb.dve(lambda v: v.tensor_tensor(out=dst, in0=dst, in1=kf[:], op=ALU.add), reads=["kf", key], writes=[key])
            kb.act(lambda a: a.activation(out=dst, in_=dst, func=AF.Sin), reads=[key], writes=[key])
        kb.dma("sp", RPOS[:, :], PC[0:64, :], reads=["PC0", "PC1"], writes=["D:RPOS"], sem="st0")
        kb.barrier()

    wiv = w_in.rearrange("(kc p) n -> p kc n", p=128)

    def softplus_dt(ps_ap, pskey, dst, dkey):
        kb.dve(lambda v: v.tensor_tensor(out=stats[:, 0:64], in0=ps_ap, in1=rows64[:, 1, :], op=ALU.add), reads=["rows64", pskey], writes=["sp_x"])
        kb.dve(lambda v: v.scalar_tensor_tensor(out=dst, in0=stats[:, 0:64], scalar=-1.0, in1=stats[:, 0:64], op0=ALU.mult, op1=ALU.max), reads=["sp_x"], writes=[dkey])
        kb.act(lambda a: a.activation(out=dst, in_=dst, func=AF.Exp, scale=-1.0), reads=[dkey], writes=[dkey])
        kb.act(lambda a: a.activation(out=dst, in_=dst, func=AF.Ln, bias=1.0), reads=[dkey], writes=[dkey])
        kb.dve(lambda v: v.scalar_tensor_tensor(out=dst, in0=stats[:, 0:64], scalar=0.0, in1=dst, op0=ALU.max, op1=ALU.add),
               reads=["sp_x", dkey], writes=[dkey])

    def proj_phase(src, ntok, a_col, b_col, with_pos, xbc_dst, dt_dst, full, tag):
        NT = min(1024, ntok)
        nsup = ntok // NT
        TW = min(512, NT)
        with ExitStack() as pes:
            aT = kb.sb(pes, "aT" + tag, [128, KD, NT], BF16)
            xts = [kb.sb(pes, "xt%d%s" % (i, tag), [128, D], F32) for i in range(2)]
            xns = [kb.sb(pes, "xn%d%s" % (i, tag), [128, D], F32) for i in range(2)]
            pts = [kb.sb(pes, "pt%d%s" % (i, tag), [128, 1024], F32) for i in range(2)] if with_pos else None
            bst = kb.sb(pes, "bst" + tag, [128, 4, 6], F32)
            if full:
                lngr = kb.sb(pes, "lngr", [128, D], F32)
                lnbr = kb.sb(pes, "lnbr", [128, D], F32)
                hls = [kb.sb(pes, "hl%d" % i, [128, D], F32) for i in range(2)]
                row_load(lngr[:], ln_in_g, "lngr")
                row_load(lnbr[:], ln_in_b, "lnbr")
            mv = kb.sb(pes, "mv" + tag, [128, 4], F32)
            wbs = [kb.sb(pes, "wb%d%s" % (i, tag), [128, KD, 512], BF16) for i in range(2)]
            NSTG = 4
            stg = [kb.sb(pes, "stg%d%s" % (i, tag), [128, 512], F32) for i in range(NSTG)]
            stgb = [kb.sb(pes, "stgb%d%s" % (i, tag), [128, 512], BF16) for i in range(2)]
            segs = [("xbc", 0, XBC, "F"), ("dt", XBC, 64, "T")]
            if full:
                segs += [("z", 3136, D, "T"), ("u", 5184, D, "F"), ("v", 7232, D, "T"), ("gs", 9280, D, "F"), ("gc", 11328, D, "F")]
            wtiles = []
            for (nm, c0, ncol, lay) in segs:
                o = 0
                while o < ncol:
                    w = min(512, ncol - o)
                    wtiles.append((nm, c0 + o, o, w, lay))
                    o += w
            sti = [0]
            for s in range(nsup):
                for ci in range(NT // 128):
                    c = s * (NT // 128) + ci
                    xt = xts[c % 2]; xk = "xt%d%s" % (c % 2, tag)
                    xn = xns[c % 2]; nk = "xn%d%s" % (c % 2, tag)
                    kb.dma("sp", xt[:], src[c * 128:(c + 1) * 128, :], writes=[xk], sem="ldx%d" % (c % 2))
                    if with_pos:
                        pt = pts[c % 2]; pk = "pt%d%s" % (c % 2, tag)
                        kb.dma("sp", pt[0:64, :], RPOS[2 * c, :].partition_broadcast(64), reads=["D:RPOS"], writes=[pk], sem="ldp%d" % (c % 2))
                        kb.dma("sp", pt[64:128, :], RPOS[2 * c + 1, :].partition_broadcast(64), reads=["D:RPOS"], writes=[pk], sem="ldp%d" % (c % 2))
                        kb.dve(lambda v: v.tensor_tensor(out=xt[:, 0:1024], in0=xt[:, 0:1024], in1=pt[:], op=ALU.add), reads=[xk, pk], writes=[xk])
                        kb.dve(lambda g: g.tensor_tensor(out=xt[:, 1024:2048], in0=xt[:, 1024:2048], in1=PC[:], op=ALU.add),
                                reads=[xk, "PC0", "PC1"], writes=[xk + "h"])
                    for q in range(4):
                        kb.dve(lambda v: v.bn_stats(out=bst[:, q, :], in_=xt[:, q * 512:(q + 1) * 512]), reads=[xk, xk + "h"], writes=["bst" + tag])
                    kb.dve(lambda v: v.bn_aggr(out=mv[:, 0:2], in_=bst[:].rearrange("p a b -> p (a b)")), reads=["bst" + tag], writes=["mv" + tag])
                    kb.act(lambda a: a.activation(out=mv[:, 2:3], in_=mv[:, 1:2], func=AF.Sqrt, bias=EPS), reads=["mv" + tag], writes=["mv2" + tag])
                    kb.dve(lambda v: v.reciprocal(out=mv[:, 2:3], in_=mv[:, 2:3]), reads=["mv2" + tag], writes=["mv2" + tag])
                    kb.dve(lambda v: v.scalar_tensor_tensor(out=mv[:, 3:4], in0=mv[:, 0:1], scalar=-1.0, in1=mv[:, 2:3], op0=ALU.mult, op1=ALU.mult),
                           reads=["mv" + tag, "mv2" + tag], writes=["mv3" + tag])
                    kb.act(lambda a: a.activation(out=xn[:], in_=xt[:], func=AF.Identity, scale=mv[:, 2:3], bias=mv[:, 3:4]),
                           reads=[xk, xk + "h", "mv2" + tag, "mv3" + tag], writes=[nk])
                    if full:
                        hl = hls[c % 2]; hk = "hl%d" % (c % 2)
                        kb.dve(lambda v: v.tensor_tensor(out=hl[:], in0=xn[:], in1=lngr[:], op=ALU.mult), reads=[nk, "lngr"], writes=[hk])
                        kb.dve(lambda g: g.tensor_tensor(out=hl[:], in0=hl[:], in1=lnbr[:], op=ALU.add), reads=[hk, "lnbr"], writes=[hk])
                        kb.dma("sp", XN[c * 128:(c + 1) * 128, :], hl[:], reads=[hk], writes=["D:XN"], sem="stn%d" % (c % 2))
                    for qd in range(4):
                        bank = qd % 4
                        for j in range(4):
                            dk = qd * 4 + j
                            kb.tr(PS[bank][:, j * 128:(j + 1) * 128], xn[:, dk * 128:(dk + 1) * 128], ident, reads=[nk, "cm"], writes=[PK[bank]])
                        for j in range(4):
                            dk = qd * 4 + j
                            kb.act(lambda a: a.activation(out=aT[:, dk, ci * 128:(ci + 1) * 128], in_=PS[bank][:, j * 128:(j + 1) * 128],
                                                          func=AF.Identity, scale=a_col[:, dk:dk + 1], bias=b_col[:, dk:dk + 1]),
                                   reads=[PK[bank], "colsA0", "colsA1", "colsB0", "colsB1"], writes=["aT%s_%d" % (tag, ci // 4)])
                aTkeys = ["aT%s_%d" % (tag, i) for i in range(max(1, NT // 512))]

                def load_w(i):
                    nm, cabs, o, w, lay = wtiles[i]
                    kb.dma("pool", wbs[i % 2][:, :, 0:w], wiv[:, :, cabs:cabs + w], writes=["wb%d%s" % (i % 2, tag)], sem="ldw%d" % (i % 2))

                load_w(0)
                for i, (nm, cabs, o, w, lay) in enumerate(wtiles):
                    if i + 1 < len(wtiles):
                        load_w(i + 1)
                    wb = wbs[i % 2]; wk = "wb%d%s" % (i % 2, tag)
                    if lay == "F":
                        for cb in range(w // 128):
                            for tt in range(NT // TW):
                                bank = 4 + (sti[0] % 4)
                                kb.mm(PS[bank][:, 0:TW], [(wb[:, k, cb * 128:(cb + 1) * 128], aT[:, k, tt * TW:(tt + 1) * TW]) for k in range(KD)],
                                      reads=[wk] + aTkeys, writes=[PK[bank]])
                                row0 = o + cb * 128
                                tok0 = s * NT + tt * TW
                                if nm == "xbc":
                                    sg = stgb[sti[0] % 2]; sk = "stgb%d%s" % (sti[0] % 2, tag)
                                    kb.dve(lambda v: v.tensor_copy(out=sg[:, 0:TW], in_=PS[bank][:, 0:TW]), reads=[PK[bank]], writes=[sk])
                                    kb.dma("sp", xbc_dst[row0:row0 + 128, 2 + tok0:2 + tok0 + TW], sg[:, 0:TW], reads=[sk], writes=["D:XBC" + tag],
                                           sem="stb%d" % (sti[0] % 2))
                                else:
                                    sg = stg[sti[0] % NSTG]; sk = "stg%d%s" % (sti[0] % NSTG, tag)
                                    fn = AF.Gelu_apprx_tanh if nm == "u" else AF.Sigmoid
                                    dst = {"u": U_T, "gs": GS_T, "gc": GC_T}[nm]
                                    kb.act(lambda a: a.activation(out=sg[:, 0:TW], in_=PS[bank][:, 0:TW], func=fn), reads=[PK[bank]], writes=[sk])
                                    kb.dma("sp", dst[row0:row0 + 128, tok0:tok0 + TW], sg[:, 0:TW], reads=[sk], writes=["D:" + nm], sem="stf%d" % (sti[0] % NSTG))
                                sti[0] += 1
                    else:
                        for tc in range(NT // 128):
                            bank = 4 + (sti[0] % 4)
                            kb.mm(PS[bank][:, 0:w], [(aT[:, k, tc * 128:(tc + 1) * 128], wb[:, k, 0:w]) for k in range(KD)],
                                  reads=[wk] + aTkeys, writes=[PK[bank]])
                            c = s * (NT // 128) + tc
                            if nm == "dt":
                                softplus_dt(PS[bank][:, 0:64], PK[bank], dt_dst[:, c, :], "dt" + tag)
                            else:
                                sg = stg[sti[0] % NSTG]; sk = "stg%d%s" % (sti[0] % NSTG, tag)
                                fn = AF.Silu if nm == "z" else AF.Gelu_apprx_tanh
                                dst = ZS if nm == "z" else VG
                                kb.act(lambda a: a.activation(out=sg[:, 0:w], in_=PS[bank][:, 0:w], func=fn), reads=[PK[bank]], writes=[sk])
                                kb.dma("sp", dst[c * 128:(c + 1) * 128, o:o + w], sg[:, 0:w], reads=[sk], writes=["D:" + nm], sem="stf%d" % (sti[0] % NSTG))
                            sti[0] += 1
            kb.barrier()

    zt = kb.sb(es, "zt", [128, 2], BF16)
    kb.dve(lambda v: v.memset(zt[:], 0.0), writes=["zt"])
    for dst_, n_, tg in ((XBC_T, T, "L"), (XBC_C, CTX, "C")):
        for b in range(24):
            kb.dma("sp", dst_[b * 128:(b + 1) * 128, 0:2], zt[:], reads=["zt"], writes=["D:XBC" + tg], sem="st0")
            kb.dma("sp", dst_[b * 128:(b + 1) * 128, n_ + 2:n_ + 4], zt[:], reads=["zt"], writes=["D:XBC" + tg], sem="st0")

    SKIP = dbg.get("skip", False)
    if not SKIP:
        proj_phase(ctx, CTX, cols[:, C_A1C, :], cols[:, C_B1C, :], False, XBC_C, dt_ctx, False, "C")
        proj_phase(x, T, cols[:, C_A1L, :], cols[:, C_B1L, :], True, XBC_T, dt_all, True, "L")

    if stop == "proj":
        if DBG_A is not None:
            kb.dma("sp", DBG_A[:, 0:2048], dt_all[:].rearrange("p a b -> p (a b)"), reads=["dtL"], writes=["D:DBG_A"], sem="st0")
            kb.dma("sp", DBG_A[:, 2048:2048 + 192], adac[:].rearrange("p a b -> p (a b)"), reads=["adac"], writes=["D:DBG_A"], sem="st0")
            kb.dma("sp", DBG_A[:, 2304:2304 + 128], dt_ctx[:].rearrange("p a b -> p (a b)"), reads=["dtC"], writes=["D:DBG_A"], sem="st0")
            kb.dma("sp", DBG_A[:, 2560:2560 + 1024], PC[:], reads=["PC0", "PC1"], writes=["D:DBG_A"], sem="st0")
        kb.finish([k for k in kb.lastw if k.startswith("D:")])
        return nc

    ssd_es = ExitStack()
    dg = kb.sb(ssd_es, "dg", [128, 120, 128], BF16)
    S_init = kb.sb(ssd_es, "S_init", [128, 2, D], F32)
    for k in range(5):
        for b in range(24):
            kb.dve(lambda v: v.tensor_scalar(out=dg[:, k * 24 + b, :], in0=ident, scalar1=convw[:, k, b:b + 1], scalar2=None, op0=ALU.mult),
                   reads=["cm", "convw"], writes=["dg"])
    wrk = [0]

    def wbank():
        wrk[0] += 1
        return wrk[0] % 2

    def ssd_phase(XSRC, xkey, dtsrc, nch, mode, S_in, S_out_slot):
        with ExitStack() as pes:
            xins = [kb.sb(pes, "xin%d" % i, [128, 24, 132], BF16) for i in range(2)]
            xcT = kb.sb(pes, "xcT", [128, 16, 128], F32)
            BTf = kb.sb(pes, "BTf", [128, 4, 128], F32)
            BT = kb.sb(pes, "BT", [128, 4, 128], BF16)
            CT = kb.sb(pes, "CT", [128, 4, 128], BF16)
            Btok = kb.sb(pes, "Btok", [128, 4, 128], BF16)
            avec = kb.sb(pes, "avec", [128, 64], F32)
            Edec = kb.sb(pes, "Edec", [128, 5, 64], F32)
            w1 = kb.sb(pes, "w1", [128, 64], F32)
            xst = kb.sb(pes, "xst", [128, D], BF16)
            Sf = kb.sb(pes, "Sf", [128, D], F32)
            Sfb = kb.sb(pes, "Sfb", [128, D], BF16)
            xv = XSRC.rearrange("(b p) t -> p b t", p=128)
            if mode == "M":
                Sbb = [kb.sb(pes, "Sbb%d" % i, [128, D], BF16) for i in range(2)]
                zss = [kb.sb(pes, "zs%d" % i, [128, D], F32) for i in range(1)]
                xdt = [kb.sb(pes, "xdt%d" % i, [128, D], BF16) for i in range(2)]
                scm = [kb.sb(pes, "scm%d" % i, [128, 128], BF16) for i in range(2)]
                rhsD = [kb.sb(pes, "rhsD%d" % i, [128, 4, 128], F32) for i in range(2)]
                Ex = [kb.sb(pes, "Ex%d" % i, [128, 512], BF16) for i in range(2)]
                MT = [kb.sb(pes, "MT%d" % i, [128, 4, 128], BF16) for i in range(2)]
                t1 = kb.sb(pes, "t1", [128, 512], F32)
                t2 = kb.sb(pes, "t2", [128, 512], F32)
                ysb = kb.sb(pes, "ysb", [128, D], F32)
                xsk = kb.sb(pes, "xsk", [128, D], F32)
                nwr = kb.sb(pes, "nwr", [128, D], F32)
                ynT = [kb.sb(pes, "ynT%d" % i, [128, KD, 512], BF16) for i in range(1)]
                ss = kb.sb(pes, "ss", [128, 2], F32)
                row_load(nwr[:], ssd_norm_w, "nwr")
            if S_in is None:
                kb.dve(lambda v: v.memset(Sf[:], 0.0), writes=["Sf%d" % g for g in range(4)])
            else:
                kb.dve(lambda v: v.tensor_copy(out=Sf[:], in_=S_in), reads=["S_init"], writes=["Sf%d" % g for g in range(4)])
            kb.act(lambda a: a.copy(out=Sfb[:], in_=Sf[:]), reads=["Sf%d" % g for g in range(4)], writes=["Sfb%d" % g for g in range(4)])
            order = list(range(nch)) if mode != "B" else list(range(nch - 1, -1, -1))

            def load_x(i):
                c = order[i]
                kb.dma("sp", xins[i % 2][:], xv[:, :, c * 128:c * 128 + 132], reads=[xkey], writes=["xin%d" % (i % 2)], sem="ldxi%d" % (i % 2))

            load_x(0)
            for i, c in enumerate(order):
                if i + 1 < nch:
                    load_x(i + 1)
                xin = xins[i % 2]; xik = "xin%d" % (i % 2)
                if mode == "M":
                    kb.dma("sp", Sbb[i % 2][:], SBW[c, :, :], reads=["D:SBW"], writes=["Sbb%d" % (i % 2)], sem="ldsb%d" % (i % 2))
                    kb.dma("sp", zss[0][:], ZS[c * 128:(c + 1) * 128, :], reads=["D:z"], writes=["zs0"], sem="ldz0")
                for q in range(6):
                    bank = wbank()
                    for j in range(4):
                        b = q * 4 + j
                        kb.mm(PS[bank][:, j * 128:(j + 1) * 128], [(dg[:, k * 24 + b, :], xin[:, b, k:k + 128]) for k in range(5)],
                              reads=[xik, "dg"], writes=[PK[bank]])
                    for j in range(4):
                        b = q * 4 + j
                        if b < 16:
                            dst, key = xcT[:, b, :], "xcT"
                        elif b < 20:
                            dst, key = BTf[:, b - 16, :], "BTf"
                        else:
                            dst, key = CT[:, b - 20, :], "CT"
                        kb.act(lambda a: a.activation(out=dst, in_=PS[bank][:, j * 128:(j + 1) * 128], func=AF.Silu, bias=convb[:, b:b + 1]),
                               reads=[PK[bank], "convb"], writes=[key + str(q)])
                xcTk = ["xcT%d" % q for q in range(4)]
                kb.dve(lambda g_: g_.tensor_copy(out=BT[:], in_=BTf[:]), reads=["BTf4"], writes=["BT"])
                dtc = dtsrc[:, c, :]
                kb.dve(lambda v: v.tensor_tensor(out=avec[:], in0=dtc, in1=rows64[:, 0, :], op=ALU.mult), reads=["dtL", "dtC", "rows64"], writes=["avec"])
                bank = wbank()
                for j, mi in enumerate((MU, MSL, ML, MSU, ONES)):
                    kb.mm(PS[bank][:, j * 64:(j + 1) * 64], [(cm[:, mi, :], avec[:])], reads=["avec", "cm"], writes=[PK[bank]])
                kb.act(lambda a: a.activation(out=Edec[:].rearrange("p a b -> p (a b)"), in_=PS[bank][:, 0:320], func=AF.Exp), reads=[PK[bank]], writes=["Edec"])
                d_state = 1 if mode == "B" else 0
                ei = 3 if d_state == 1 else 1
                kb.dve(lambda v: v.tensor_tensor(out=w1[:, 0:32], in0=dtc[:, d_state * 32:(d_state + 1) * 32], in1=Edec[:, ei, d_state * 32:(d_state + 1) * 32], op=ALU.mult),
                       reads=["dtL", "dtC", "Edec"], writes=["w1"])
                for q in range(4):
                    bank = wbank()
                    for j in range(4):
                        kb.tr(PS[bank][:, j * 128:(j + 1) * 128], xcT[:, q * 4 + j, :], ident, reads=["xcT%d" % q, "cm"], writes=[PK[bank]])
                    qs = slice(q * 512, (q + 1) * 512)
                    pv = PS[bank][:, :].rearrange("p (h q) -> p h q", q=64)

                    def prod(dst, sc, rkeys, wkey):
                        kb.dve(lambda v: v.tensor_tensor(out=dst[:, qs].rearrange("p (h q) -> p h q", q=64), in0=pv,
                                                         in1=sc.unsqueeze(2).to_broadcast([128, 8, 64]), op=ALU.mult),
                               reads=[PK[bank]] + rkeys, writes=[wkey])
                    prod(xst, w1[:, q * 8:(q + 1) * 8], ["w1"], "xst%d" % q)
                    if mode == "M":
                        for d in range(2):
                            prod(xdt[d], dtc[:, d * 32 + q * 8:d * 32 + (q + 1) * 8], ["dtL"], "xdt%d_%d" % (d, q))
                        prod(xsk, rows64[:, 3, q * 8:(q + 1) * 8], ["rows64"], "xsk%d" % q)
                bank = wbank()
                for g in range(4):
                    kb.tr(PS[bank][:, g * 128:(g + 1) * 128], BTf[:, g, :], ident, reads=["BTf4", "cm"], writes=[PK[bank]])
                kb.act(lambda a: a.copy(out=Btok[:].rearrange("p a b -> p (a b)"), in_=PS[bank][:, :]), reads=[PK[bank]], writes=["Btok"])
                if mode == "B" and nch == NCH:
                    kb.dma("sp", SBW[c, :, :], Sfb[:], reads=["Sfb%d" % g for g in range(4)], writes=["D:SBW"], sem="stsb")
                if mode == "M":
                    Sb = Sbb[i % 2]; sbk = "Sbb%d" % (i % 2)
                    for g in range(4):
                        gs = slice(g * 512, (g + 1) * 512)
                        kb.mm(PS[2][:, 0:128], [(BT[:, g, :], CT[:, g, :])], reads=["BT", "CT5"], writes=[PK[2]])
                        kb.dve(lambda v: v.tensor_tensor(out=scm[0][:], in0=PS[2][:, 0:128], in1=cm[:, MU, :], op=ALU.mult), reads=[PK[2], "cm"], writes=["scm0"])
                        kb.dve(lambda v: v.tensor_tensor(out=scm[1][:], in0=PS[2][:, 0:128], in1=cm[:, ML, :], op=ALU.mult), reads=[PK[2], "cm"], writes=["scm1"])
                        kb.mm(PS[3][:, :], [(CT[:, g, :], Sfb[:, gs])], reads=["CT5", "Sfb%d" % g], writes=[PK[3]])
                        kb.mm(PS[4][:, :], [(CT[:, g, :], Sb[:, gs])], reads=["CT5", sbk], writes=[PK[4]])
                        for hq in range(2):
                            h0 = g * 8 + hq * 4
                            for d in range(2):
                                mrhs = MU if d == 0 else ML
                                mlhs = MSL if d == 0 else MSU
                                for i4 in range(4):
                                    kb.act(lambda a: a.activation(out=rhsD[d][:, i4, :], in_=cm[:, mrhs, :], func=AF.Identity,
                                                                  scale=avec[:, d * 32 + h0 + i4:d * 32 + h0 + i4 + 1]),
                                           reads=["avec", "cm"], writes=["rhsD%d_%d" % (d, i4)])
                                bd = 6 + d
                                kb.mm(PS[bd][:, :], [(cm[:, mlhs, :], rhsD[d][:].rearrange("p a b -> p (a b)"))], reads=["rhsD%d_%d" % (d, i4) for i4 in range(4)] + ["cm"], writes=[PK[bd]])
                                kb.act(lambda a: a.activation(out=Ex[d][:], in_=PS[bd][:, :], func=AF.Exp), reads=[PK[bd]], writes=["Ex%d" % d])
                                kb.dve(lambda v: v.tensor_tensor(out=MT[d][:], in0=Ex[d][:].rearrange("p (a b) -> p a b", b=128),
                                                                 in1=scm[d][:].unsqueeze(1).to_broadcast([128, 4, 128]), op=ALU.mult),
                                       reads=["Ex%d" % d, "scm%d" % d], writes=["MT%d" % d])
                            for ii in range(4):
                                h = h0 + ii
                                col = (hq * 4 + ii) * 64
                                kb.mm(PS[5][:, col:col + 64],
                                      [(MT[0][:, ii, :], xdt[0][:, h * 64:(h + 1) * 64]), (MT[1][:, ii, :], xdt[1][:, h * 64:(h + 1) * 64])],
                                      reads=["MT0", "MT1", "xdt0_%d" % g, "xdt1_%d" % g], writes=[PK[5]])
                        kb.dve(lambda v: v.tensor_tensor(out=t1[:].rearrange("p (h q) -> p h q", q=64), in0=PS[3][:, :].rearrange("p (h q) -> p h q", q=64),
                                                         in1=Edec[:, 0, g * 8:g * 8 + 8].unsqueeze(2).to_broadcast([128, 8, 64]), op=ALU.mult),
                               reads=[PK[3], "Edec"], writes=["t1"])
                        kb.dve(lambda v: v.tensor_tensor(out=t2[:].rearrange("p (h q) -> p h q", q=64), in0=PS[4][:, :].rearrange("p (h q) -> p h q", q=64),
                                                         in1=Edec[:, 2, 32 + g * 8:32 + g * 8 + 8].unsqueeze(2).to_broadcast([128, 8, 64]), op=ALU.mult),
                               reads=[PK[4], "Edec"], writes=["t2"])
                        kb.dve(lambda g_: g_.tensor_tensor(out=t1[:], in0=t1[:], in1=t2[:], op=ALU.add), reads=["t1", "t2"], writes=["t1"])
                        kb.dve(lambda v: v.tensor_tensor(out=ysb[:, gs], in0=PS[5][:, :], in1=t1[:], op=ALU.add), reads=[PK[5], "t1"], writes=["ysb%d" % g])
                for g in range(4):
                    gs = slice(g * 512, (g + 1) * 512)
                    bank = wbank()
                    kb.mm(PS[bank][:, :], [(Btok[:, g, :], xst[:, gs])], reads=["Btok", "xst%d" % g], writes=[PK[bank]])
                    kb.dve(lambda v: v.tensor_tensor(out=Sf[:, gs].rearrange("p (h q) -> p h q", q=64), in0=Sf[:, gs].rearrange("p (h q) -> p h q", q=64),
                                                     in1=Edec[:, 4, d_state * 32 + g * 8:d_state * 32 + g * 8 + 8].unsqueeze(2).to_broadcast([128, 8, 64]), op=ALU.mult),
                           reads=["Edec", "Sf%d" % g], writes=["Sf%d" % g])
                    kb.dve(lambda v: v.tensor_tensor(out=Sf[:, gs], in0=Sf[:, gs], in1=PS[bank][:, :], op=ALU.add), reads=[PK[bank], "Sf%d" % g], writes=["Sf%d" % g])
                    kb.act(lambda a: a.copy(out=Sfb[:, gs], in_=Sf[:, gs]), reads=["Sf%d" % g], writes=["Sfb%d" % g])
                if mode == "M":
                    ysk = ["ysb%d" % g for g in range(4)]
                    kb.dve(lambda v: v.tensor_tensor(out=ysb[:], in0=ysb[:], in1=xsk[:], op=ALU.add), reads=ysk + ["xsk%d" % q for q in range(4)], writes=["ysbA"])
                    kb.dve(lambda v: v.tensor_tensor(out=ysb[:], in0=ysb[:], in1=zss[0][:], op=ALU.mult), reads=["ysbA", "zs0"], writes=["ysbA"])
                    kb.act(lambda a: a.activation(out=xsk[:], in_=ysb[:], func=AF.Square, accum_out=ss[:, 0:1]), reads=["ysbA"] + ["xsk%d" % q for q in range(4)], writes=["ss"] + ["xsk%d" % q for q in range(4)])
                    kb.act(lambda a: a.activation(out=ss[:, 1:2], in_=ss[:, 0:1], func=AF.Sqrt, scale=1.0 / D, bias=EPS), reads=["ss"], writes=["ss1"])
                    kb.dve(lambda v: v.reciprocal(out=ss[:, 1:2], in_=ss[:, 1:2]), reads=["ss1"], writes=["ss1"])
                    kb.dve(lambda v: v.scalar_tensor_tensor(out=ysb[:], in0=ysb[:], scalar=ss[:, 1:2], in1=nwr[:], op0=ALU.mult, op1=ALU.mult),
                           reads=["ysbA", "ss1", "nwr"], writes=["ysbA"] + ysk)
                    yt = ynT[0]; ytk = "ynT0"
                    for q in range(4):
                        bank = wbank()
                        for j in range(4):
                            kb.tr(PS[bank][:, j * 128:(j + 1) * 128], ysb[:, (q * 4 + j) * 128:(q * 4 + j + 1) * 128], ident, reads=["ysbA", "cm"], writes=[PK[bank]])
                        o_ap = yt[:, q * 4:q * 4 + 4, (c % 4) * 128:(c % 4 + 1) * 128]
                        i_ap = PS[bank][:, :].rearrange("p (a b) -> p a b", b=128)
                        if q % 2 == 0:
                            kb.act(lambda a: a.copy(out=o_ap, in_=i_ap), reads=[PK[bank]], writes=[ytk])
                        else:
                            kb.dve(lambda v: v.tensor_copy(out=o_ap, in_=i_ap), reads=[PK[bank]], writes=[ytk])
                    if c % 4 == 3:
                        tl = c // 4
                        kb.dma("sp", YN_T.rearrange("(k p) t -> p k t", p=128)[:, :, tl * 512:(tl + 1) * 512], yt[:], reads=[ytk], writes=["D:YN_T"], sem="styn%d" % (tl % 2))
            if S_out_slot is not None and mode != "M":
                kb.dve(lambda v: v.tensor_copy(out=S_out_slot, in_=Sf[:]), reads=["Sf%d" % g for g in range(4)], writes=["S_init"])
            kb.barrier()

    if not SKIP:
        ssd_phase(XBC_C, "D:XBCC", dt_ctx, 2, "F", None, S_init[:, 0, :])
        ssd_phase(XBC_C, "D:XBCC", dt_ctx, 2, "B", None, S_init[:, 1, :])
    if stop == "ctxssd":
        if DBG_A is not None:
            kb.dma("sp", DBG_A[:, 0:4096], S_init[:].rearrange("p a b -> p (a b)"), reads=["S_init"], writes=["D:DBG_A"], sem="st0")
        kb.barrier()
        return nc
    if not SKIP:
        ssd_phase(XBC_T, "D:XBCL", dt_all, NCH, "B", S_init[:, 1, :], None)
    if stop_here("ssdB"):
        return nc
    if not SKIP:
        ssd_phase(XBC_T, "D:XBCL", dt_all, NCH, "M", S_init[:, 0, :], None)
    ssd_es.close()
    if stop_here("ssdM"):
        return nc

    with ExitStack() as pes:
      if not SKIP:
            wsr = kb.sb(pes, "wsr", [128, 8, 128], F32)
            wsTf = kb.sb(pes, "wsTf", [128, 8, 128], F32)
            wsTb = kb.sb(pes, "wsTb", [128, 8, 128], BF16)
            Bbc = kb.sb(pes, "Bbc", [128, D], F32)
            bsr = kb.sb(pes, "bsr", [1, 8, 128], F32)
            Rc = kb.sb(pes, "Rc", [128, 16, 128], F32)
            vgs = [kb.sb(pes, "vg%d" % i, [128, D], F32) for i in range(2)]
            vh = [kb.sb(pes, "vh%d" % i, [128, D], BF16) for i in range(4)]
            uT = [kb.sb(pes, "uT%d" % i, [128, KD, 512], F32) for i in range(1)]
            cmT = [kb.sb(pes, "cmT%d" % i, [128, KD, 512], BF16) for i in range(1)]
            tmpc = [kb.sb(pes, "tmpc%d" % i, [128, 512], F32) for i in range(2)]
            bst = kb.sb(pes, "bstc", [128, 4, 6], F32)
            mv = kb.sb(pes, "mvc", [128, 4], F32)
            col_load(cols[:, C_CMG, :], cmlp_ln_g, 16, "colsCMG")
            kb.dma("sp", wsr[:], cmlp_ws.rearrange("g t s -> t g s"), writes=["wsr"], sem="k_wsr")
            row_load(Bbc[:], cmlp_ln_b, "Bbc")
            kb.dma("sp", bsr[0:1, :, :], cmlp_bs.rearrange("g t -> (g t)").rearrange("(o g t) -> o g t", o=1, g=8), writes=["bsr"], sem="k_bsr")
            for g in range(8):
                kb.tr(PS[0][:, g % 4 * 128:(g % 4 + 1) * 128], wsr[:, g, :], ident, reads=["wsr", "cm"], writes=[PK[0]])
                kb.dve(lambda v: v.tensor_copy(out=wsTf[:, g, :], in_=PS[0][:, g % 4 * 128:(g % 4 + 1) * 128]), reads=[PK[0]], writes=["wsTf"])
            kb.dve(lambda v: v.tensor_copy(out=wsTb[:], in_=wsTf[:]), reads=["wsTf"], writes=["wsTb"])
            for blk in range(16):
                g = blk // 2
                kb.mm(PS[1][:, 0:128], [(Bbc[:, blk * 128:(blk + 1) * 128], wsTf[:, g, :]), (cm[0:1, ONES, :], bsr[0:1, g, :])],
                      reads=["Bbc", "wsTf", "bsr", "cm"], writes=[PK[1]])
                kb.dve(lambda v: v.tensor_copy(out=Rc[:, blk, :], in_=PS[1][:, 0:128]), reads=[PK[1]], writes=["Rc"])
            utv = U_T.rearrange("(k p) t -> p k t", p=128)
            for tl in range(T // 512):
                kb.dma("sp", uT[0][:], utv[:, :, tl * 512:(tl + 1) * 512], reads=["D:u"], writes=["uT0"], sem="ldu0")
                for ci in range(4):
                    c = tl * 4 + ci
                    vg = vgs[c % 2]; vk = "vg%d" % (c % 2)
                    kb.dma("sp", vg[:], VG[c * 128:(c + 1) * 128, :], reads=["D:v"], writes=[vk], sem="ldv%d" % (c % 2))
                    for q in range(4):
                        kb.dve(lambda v: v.bn_stats(out=bst[:, q, :], in_=vg[:, q * 512:(q + 1) * 512]), reads=[vk], writes=["bstc"])
                    kb.dve(lambda v: v.bn_aggr(out=mv[:, 0:2], in_=bst[:].rearrange("p a b -> p (a b)")), reads=["bstc"], writes=["mvc"])
                    kb.act(lambda a: a.activation(out=mv[:, 2:3], in_=mv[:, 1:2], func=AF.Sqrt, bias=EPS), reads=["mvc"], writes=["mvc2"])
                    kb.dve(lambda v: v.reciprocal(out=mv[:, 2:3], in_=mv[:, 2:3]), reads=["mvc2"], writes=["mvc2"])
                    kb.dve(lambda v: v.scalar_tensor_tensor(out=mv[:, 3:4], in0=mv[:, 0:1], scalar=-1.0, in1=mv[:, 2:3], op0=ALU.mult, op1=ALU.mult),
                           reads=["mvc", "mvc2"], writes=["mvc3"])
                    kb.act(lambda a: a.activation(out=vh[ci][:], in_=vg[:], func=AF.Identity, scale=mv[:, 2:3], bias=mv[:, 3:4]),
                           reads=[vk, "mvc2", "mvc3"], writes=["vh%d" % ci])
                ct = cmT[0]; ck = "cmT0"
                for blk in range(16):
                    g = blk // 2
                    bank = 2 + blk % 4
                    for ci in range(4):
                        kb.mm(PS[bank][:, ci * 128:(ci + 1) * 128], [(vh[ci][:, blk * 128:(blk + 1) * 128], wsTb[:, g, :])],
                              reads=["vh%d" % ci, "wsTb"], writes=[PK[bank]])
                    tp = tmpc[blk % 2]; tk = "tmpc%d" % (blk % 2)
                    kb.dve(lambda v: v.scalar_tensor_tensor(out=tp[:].rearrange("p (a b) -> p a b", b=128), in0=PS[bank][:, :].rearrange("p (a b) -> p a b", b=128),
                                                            scalar=cols[:, C_CMG, blk:blk + 1], in1=Rc[:, blk, :].unsqueeze(1).to_broadcast([128, 4, 128]),
                                                            op0=ALU.mult, op1=ALU.add),
                           reads=[PK[bank], "colsCMG", "Rc"], writes=[tk])
                    kb.dve(lambda g_: g_.tensor_tensor(out=ct[:, blk, :], in0=tp[:], in1=uT[0][:, blk, :], op=ALU.mult),
                            reads=[tk, "uT0"], writes=[ck])
                kb.dma("sp", CM_T.rearrange("(k p) t -> p k t", p=128)[:, :, tl * 512:(tl + 1) * 512], ct[:], reads=[ck], writes=["D:CM_T"], sem="stcm%d" % (tl % 2))
            if stop == "cmlp" and DBG_A is not None:
                kb.dma("sp", DBG_A[:, 0:2048], Rc[:].rearrange("p a b -> p (a b)"), reads=["Rc"], writes=["D:DBG_A"], sem="st0")
                kb.dma("sp", DBG_A[:, 2048:3072], wsTf[:].rearrange("p a b -> p (a b)"), reads=["wsTf"], writes=["D:DBG_A"], sem="st0")
                kb.dma("sp", DBG_A[:, 3072:3088], cols[:, C_CMG, :], reads=["colsCMG"], writes=["D:DBG_A"], sem="st0")
            kb.barrier()

    if stop_here("cmlp"):
        return nc
    def load_w2048(wres, wsrc, key):
        wv = wsrc.rearrange("(kc p) n -> p kc n", p=128)
        for i in range(4):
            kb.dma("pool", wres[:, :, i * 512:(i + 1) * 512], wv[:, :, i * 512:(i + 1) * 512], writes=[key], sem="ldwr")

    def gated_proj(wsrc, inT, inkey, gateT, gkey, second):
        with ExitStack() as pes:
            wres = kb.sb(pes, "wres", [128, KD, D], BF16)
            ins = [kb.sb(pes, "gin%d" % i, [128, KD, 512], BF16) for i in range(2)]
            gts = [kb.sb(pes, "ggt%d" % i, [128, 512], F32) for i in range(2)]
            pts_ = [kb.sb(pes, "gpt%d" % i, [128, 512], F32) for i in range(2)]
            so = [kb.sb(pes, "gso%d" % i, [128, 512], F32) for i in range(2)]
            sob = [kb.sb(pes, "gsob%d" % i, [128, 512], BF16) for i in range(2)]
            load_w2048(wres, wsrc, "wres")
            iv = inT.rearrange("(k p) t -> p k t", p=128)
            n = 0
            for tl in range(T // 512):
                ts_ = slice(tl * 512, (tl + 1) * 512)
                kb.dma("sp", ins[tl % 2][:], iv[:, :, ts_], reads=[inkey], writes=["gin%d" % (tl % 2)], sem="ldgi%d" % (tl % 2))
                for cb in range(16):
                    rs = slice(cb * 128, (cb + 1) * 128)
                    j = n % 2
                    kb.dma("sp", gts[j][:], gateT[rs, ts_], reads=[gkey], writes=["ggt%d" % j], sem="ldgg%d" % j)
                    if second:
                        kb.dma("sp", pts_[j][:], PART_T[rs, ts_], reads=["D:PART_T"], writes=["gpt%d" % j], sem="ldgp%d" % j)
                    bank = n % 4
                    kb.mm(PS[bank][:, :], [(wres[:, k, rs], ins[tl % 2][:, k, :]) for k in range(KD)], reads=["wres", "gin%d" % (tl % 2)], writes=[PK[bank]])
                    if not second:
                        kb.dve(lambda v: v.tensor_tensor(out=so[j][:], in0=PS[bank][:, :], in1=gts[j][:], op=ALU.mult), reads=[PK[bank], "ggt%d" % j], writes=["gso%d" % j])
                        kb.dma("sp", PART_T[rs, ts_], so[j][:], reads=["gso%d" % j], writes=["D:PART_T"], sem="stgo%d" % j)
                    else:
                        kb.dve(lambda v: v.tensor_tensor(out=so[j][:], in0=PS[bank][:, :], in1=gts[j][:], op=ALU.mult), reads=[PK[bank], "ggt%d" % j], writes=["gso%d" % j])
                        kb.dve(lambda g_: g_.tensor_tensor(out=sob[j][:], in0=so[j][:], in1=pts_[j][:], op=ALU.add), reads=["gso%d" % j, "gpt%d" % j], writes=["gsob%d" % j])
                        kb.dma("sp", MG_T[rs, ts_], sob[j][:], reads=["gsob%d" % j], writes=["D:MG_T"], sem="stgo%d" % j)
                    n += 1
            kb.barrier()

    if not SKIP:
        gated_proj(w_ssd_br, YN_T, "D:YN_T", GS_T, "D:gs", False)
    if stop_here("gp1"):
        return nc
    if not SKIP:
        gated_proj(w_cmlp_br, CM_T, "D:CM_T", GC_T, "D:gc", True)
    if stop_here("gp"):
        return nc

    kb.dve(lambda v: v.memset(gates[:, :, NE:NE + 1], 1.0), writes=["gates1"])
    with ExitStack() as pes:
        wres = kb.sb(pes, "wres_o", [128, KD, D], BF16)
        g1r = kb.sb(pes, "g1r", [128, D], F32)
        l1g = kb.sb(pes, "l1g", [128, D], F32)
        l1b = kb.sb(pes, "l1b", [128, D], F32)
        mgs = [kb.sb(pes, "mg%d" % i, [128, KD, 512], BF16) for i in range(1)]
        hls = [kb.sb(pes, "hl3%d" % i, [128, D], F32) for i in range(2)]
        rss = [kb.sb(pes, "res%d" % i, [128, D], F32) for i in range(2)]
        a2f = kb.sb(pes, "a2f", [128, KD, 128], F32)
        a2s = [kb.sb(pes, "a2s%d" % i, [128, KD, 512], BF16) for i in range(1)]
        wr = kb.sb(pes, "wr", [128, KD, NE], F32)
        bst = kb.sb(pes, "bst3", [128, 4, 6], F32)
        mv = kb.sb(pes, "mv3", [128, 4], F32)
        rt = kb.sb(pes, "rt", [128, 10, 64], F32)
        load_w2048(wres, w_o, "wres_o")
        row_load(g1r[:], ADAFLAT[2 * D:3 * D], "g1r")
        kb._deps("sp", ["D:ADAROW"], ())
        row_load(l1g[:], ln1_g, "l1g")
        row_load(l1b[:], ln1_b, "l1b")
        kb.dma("sp", wr[:], w_router.rearrange("(k p) e -> p k e", p=128), writes=["wr"], sem="k_wr")
        mv_ = MG_T.rearrange("(k p) t -> p k t", p=128)
        SCR, BIA, M8, GSC, T8, GM, M1, MSK, SEL, WW = range(10)
        NTL3 = dbg.get("p3c_tiles", T // 512)
        a2 = a2s[0]; a2k = "a2s0"
        mg = mgs[0]; mk = "mg0"

        def names(c):
            return hls[c % 2], "hl3%d" % (c % 2), rss[c % 2], "res%d" % (c % 2)

        def part_a(c):
            tl, tc = c // 4, c % 4
            hl, hk, rs_, rk = names(c)
            if tc == 0:
                kb.dma("sp", mg[:], mv_[:, :, tl * 512:(tl + 1) * 512], reads=["D:MG_T"], writes=[mk], sem="ldmg0")
            kb.dma("sp", hl[:], XN[c * 128:(c + 1) * 128, :], reads=["D:XN"], writes=[hk], sem="ldh%d" % (c % 2))
            for db in range(4):
                bank = db
                ds_ = slice(db * 512, (db + 1) * 512)
                kb.mm(PS[bank][:, :], [(mg[:, k, tc * 128:(tc + 1) * 128], wres[:, k, ds_]) for k in range(KD)], reads=[mk, "wres_o"], writes=[PK[bank]])
                kb.dve(lambda v: v.tensor_tensor(out=rs_[:, ds_], in0=PS[bank][:, :], in1=g1r[:, ds_], op=ALU.mult), reads=[PK[bank], "g1r"], writes=[rk + "_%d" % db, rk])

        def part_b(c):
            tl, tc = c // 4, c % 4
            hl, hk, rs_, rk = names(c)
            rks = [rk + "_%d" % db for db in range(4)]
            kb.dve(lambda v: v.scalar_tensor_tensor(out=rs_[:], in0=hl[:], scalar=ALPHA, in1=rs_[:], op0=ALU.mult, op1=ALU.add), reads=rks + [hk], writes=[rk])
            for q in range(4):
                kb.dve(lambda v: v.bn_stats(out=bst[:, q, :], in_=rs_[:, q * 512:(q + 1) * 512]), reads=[rk], writes=["bst3"])
            kb.dve(lambda v: v.bn_aggr(out=mv[:, 0:2], in_=bst[:].rearrange("p a b -> p (a b)")), reads=["bst3"], writes=["mv3"])
            kb.act(lambda a: a.activation(out=mv[:, 2:3], in_=mv[:, 1:2], func=AF.Sqrt, bias=EPS), reads=["mv3"], writes=["mv32"])
            kb.dve(lambda v: v.reciprocal(out=mv[:, 2:3], in_=mv[:, 2:3]), reads=["mv32"], writes=["mv32"])
            kb.dve(lambda v: v.scalar_tensor_tensor(out=mv[:, 3:4], in0=mv[:, 0:1], scalar=-1.0, in1=mv[:, 2:3], op0=ALU.mult, op1=ALU.mult),
                   reads=["mv3", "mv32"], writes=["mv33"])
            kb.act(lambda a: a.activation(out=rs_[:], in_=rs_[:], func=AF.Identity, scale=mv[:, 2:3], bias=mv[:, 3:4]), reads=[rk, "mv32", "mv33"], writes=[rk])
            kb.dve(lambda v: v.tensor_tensor(out=rs_[:], in0=rs_[:], in1=l1g[:], op=ALU.mult), reads=[rk, "l1g"], writes=[rk])
            kb.dve(lambda g_: g_.tensor_tensor(out=rs_[:], in0=rs_[:], in1=l1b[:], op=ALU.add), reads=[rk, "l1b"], writes=[rk] + rks)
            kb.dma("sp", H1[c * 128:(c + 1) * 128, :], rs_[:], reads=[rk], writes=["D:H1"], sem="sth%d" % (c % 2))
            for q in range(4):
                bank = 4 + q
                for j in range(4):
                    dk = q * 4 + j
                    kb.tr(PS[bank][:, j * 128:(j + 1) * 128], rs_[:, dk * 128:(dk + 1) * 128], ident, reads=[rk, "cm"], writes=[PK[bank]])
                for j in range(4):
                    dk = q * 4 + j
                    kb.dve(lambda v: v.tensor_scalar(out=a2f[:, dk, :], in0=PS[bank][:, j * 128:(j + 1) * 128], scalar1=cols[:, C_A2, dk:dk + 1],
                                                     scalar2=cols[:, C_B2, dk:dk + 1], op0=ALU.mult, op1=ALU.add),
                           reads=[PK[bank], "colsA2", "colsB2"], writes=["a2f%d" % q])
                kb.act(lambda a: a.copy(out=a2[:, q * 4:q * 4 + 4, tc * 128:(tc + 1) * 128], in_=a2f[:, q * 4:q * 4 + 4, :]),
                       reads=["a2f%d" % q], writes=[a2k])
            if not dbg.get("norouter"):
                kb.mm(PS[7][:, 0:NE], [(a2f[:, dk, :], wr[:, dk, :]) for dk in range(KD)], reads=["a2f%d" % q for q in range(4)] + ["wr"], writes=[PK[7]])
                kb.act(lambda a: a.activation(out=rt[:, SCR, :], in_=PS[7][:, 0:NE], func=AF.Sigmoid), reads=[PK[7]], writes=["rt"])
                R = lambda fn, **kw: kb.dve(fn, reads=["rt", "rows64"], writes=["rt"])
                R(lambda v: v.tensor_tensor(out=rt[:, BIA, :], in0=rt[:, SCR, :], in1=rows64[:, 2, :], op=ALU.add))
                for g in range(8):
                    R(lambda v: v.max(out=rt[:, M8, g * 8:(g + 1) * 8], in_=rt[:, BIA, g * 8:(g + 1) * 8]))
                m8v = rt[:, M8, :].rearrange("p (g e) -> p g e", e=8)
                R(lambda v: v.tensor_tensor(out=rt[:, GSC, 0:8], in0=m8v[:, :, 0], in1=m8v[:, :, 1], op=ALU.add))
                R(lambda v: v.max(out=rt[:, T8, 0:8], in_=rt[:, GSC, 0:8]))
                R(lambda v: v.tensor_scalar(out=rt[:, GM, 0:8], in0=rt[:, GSC, 0:8], scalar1=rt[:, T8, 3:4], scalar2=None, op0=ALU.is_ge))
                R(lambda v: v.tensor_scalar(out=rt[:, M1, 0:8], in0=rt[:, GM, 0:8], scalar1=4.0, scalar2=-4.0, op0=ALU.mult, op1=ALU.add))
                R(lambda v: v.tensor_tensor(out=rt[:, MSK, :].rearrange("p (g e) -> p g e", e=8), in0=rt[:, BIA, :].rearrange("p (g e) -> p g e", e=8),
                                            in1=rt[:, GM, 0:8].unsqueeze(2).to_broadcast([128, 8, 8]), op=ALU.mult))
                R(lambda v: v.tensor_tensor(out=rt[:, MSK, :].rearrange("p (g e) -> p g e", e=8), in0=rt[:, MSK, :].rearrange("p (g e) -> p g e", e=8),
                                            in1=rt[:, M1, 0:8].unsqueeze(2).to_broadcast([128, 8, 8]), op=ALU.add))
                R(lambda v: v.max(out=rt[:, T8, 8:16], in_=rt[:, MSK, :]))
                R(lambda v: v.tensor_scalar(out=rt[:, SEL, :], in0=rt[:, MSK, :], scalar1=rt[:, T8, 15:16], scalar2=None, op0=ALU.is_ge))
                R(lambda v: v.tensor_tensor(out=rt[:, WW, :], in0=rt[:, SCR, :], in1=rt[:, SEL, :], op=ALU.mult))
                R(lambda v: v.tensor_reduce(out=rt[:, T8, 16:17], in_=rt[:, WW, :], axis=mybir.AxisListType.X, op=ALU.add))
                R(lambda v: v.reciprocal(out=rt[:, T8, 16:17], in_=rt[:, T8, 16:17]))
                kb.dve(lambda v: v.tensor_scalar(out=gates[:, c, 0:NE], in0=rt[:, WW, :], scalar1=rt[:, T8, 16:17], scalar2=2.5, op0=ALU.mult, op1=ALU.mult),
                       reads=["rt"], writes=["gates"])
            if tc == 3:
                kb.dma("sp", A2_T.rearrange("(k p) t -> p k t", p=128)[:, :, tl * 512:(tl + 1) * 512], a2[:], reads=[a2k], writes=["D:A2_T"], sem="sta2%d" % (tl % 2))

        NC3 = NTL3 * 4
        if NC3 > 0:
            part_a(0)
        for c in range(NC3):
            if c + 1 < NC3:
                part_a(c + 1)
            part_b(c)
        kb.barrier()

    if stop == "p3c":
        if DBG_A is not None:
            kb.dma("sp", DBG_A[:, 0:NCH * (NE + 1)], gates[:].rearrange("p a b -> p (a b)"), reads=["gates", "gates1"], writes=["D:DBG_A"], sem="st0")
        kb.barrier()
        return nc
    with ExitStack() as pes:
        acc = kb.sb(pes, "acc", [128, 8, D], F32)
        a2t = [kb.sb(pes, "a2t%d" % i, [128, KD, 512], BF16) for i in range(2)]
        wg = [kb.sb(pes, "wg%d" % i, [128, KD, 256], BF16) for i in range(2)]
        wu = [kb.sb(pes, "wu%d" % i, [128, KD, 256], BF16) for i in range(2)]
        wd = [kb.sb(pes, "wd%d" % i, [128, 2, D], BF16) for i in range(2)]
        sgs = [kb.sb(pes, "sg%d" % i, [128, 512], F32) for i in range(2)]
        hT = [kb.sb(pes, "hT%d" % i, [128, 2, 512], BF16) for i in range(2)]
        a2v = A2_T.rearrange("(k p) t -> p k t", p=128)
        NHE = 2 * (NE + 1)

        def wsrc(he):
            e, hf = he // 2, he % 2
            cs = slice(hf * 256, (hf + 1) * 256)
            if e < NE:
                return (w_e_gate[e].rearrange("(k p) n -> p k n", p=128)[:, :, cs], w_e_up[e].rearrange("(k p) n -> p k n", p=128)[:, :, cs],
                        w_e_down[e].rearrange("(j p) n -> p j n", p=128)[:, hf * 2:hf * 2 + 2, :])
            return (w_sh_gate.rearrange("(k p) n -> p k n", p=128)[:, :, cs], w_sh_up.rearrange("(k p) n -> p k n", p=128)[:, :, cs],
                    w_sh_down.rearrange("(j p) n -> p j n", p=128)[:, hf * 2:hf * 2 + 2, :])

        def load_he(n, he):
            sg_, su_, sd_ = wsrc(he)
            j = n % 2
            kb.dma("pool", wg[j][:], sg_, writes=["wg%d" % j], sem="ldwg%d" % j)
            kb.dma("pool", wu[j][:], su_, writes=["wu%d" % j], sem="ldwu%d" % j)
            kb.dma("pool", wd[j][:], sd_, writes=["wd%d" % j], sem="ldwd%d" % j)

        pbc = [0]

        def gateup_groups(st, he, n, tt):
            j = n % 2
            h = hT[tt]; hk = "hT%d" % tt
            items = []
            for jb in range(2):
                def f(jb=jb):
                    bg, bu = pbc[0] % 4, (pbc[0] + 1) % 4
                    pbc[0] += 2
                    kb.mm(PS[bg][:, :], [(wg[j][:, k, jb * 128:(jb + 1) * 128], a2t[tt][:, k, :]) for k in range(KD)], reads=["wg%d" % j, "a2t%d" % tt], writes=[PK[bg]])
                    kb.mm(PS[bu][:, :], [(wu[j][:, k, jb * 128:(jb + 1) * 128], a2t[tt][:, k, :]) for k in range(KD)], reads=["wu%d" % j, "a2t%d" % tt], writes=[PK[bu]])
                    sg_ = sgs[jb]; sgk = "sg%d" % jb
                    kb.act(lambda a: a.activation(out=sg_[:], in_=PS[bg][:, :], func=AF.Silu), reads=[PK[bg]], writes=[sgk])
                    kb.dve(lambda v: v.tensor_tensor(out=h[:, jb, :], in0=sg_[:], in1=PS[bu][:, :], op=ALU.mult), reads=[sgk, PK[bu]], writes=[hk + "_%d" % jb])
                items.append(f)
            return items

        def down_groups(st, he, n, tt):
            j = n % 2
            e = he // 2
            h = hT[tt]; hk = "hT%d" % tt
            items = []
            for tc in range(4):
                for db in range(4):
                    def f(tc=tc, db=db):
                        ch = tt * 4 + tc
                        c = st * 8 + ch
                        bo = 4 + (db % 4)
                        ds_ = slice(db * 512, (db + 1) * 512)
                        kb.mm(PS[bo][:, :], [(h[:, jb, tc * 128:(tc + 1) * 128], wd[j][:, jb, ds_]) for jb in range(2)],
                              reads=[hk + "_0", hk + "_1", "wd%d" % j], writes=[PK[bo]])
                        ak = "acc%d_%d" % (ch, db)
                        if he == 0:
                            kb.dve(lambda v: v.tensor_scalar(out=acc[:, ch, ds_], in0=PS[bo][:, :], scalar1=gates[:, c, e:e + 1], scalar2=None, op0=ALU.mult),
                                   reads=[PK[bo], "gates", "gates1"], writes=[ak])
                        else:
                            kb.dve(lambda v: v.scalar_tensor_tensor(out=acc[:, ch, ds_], in0=PS[bo][:, :], scalar=gates[:, c, e:e + 1], in1=acc[:, ch, ds_],
                                                                    op0=ALU.mult, op1=ALU.add),
                                   reads=[PK[bo], "gates", "gates1", ak], writes=[ak])
                    items.append(f)
            return items

        n = 0
        for st in range(4):
            for tt in range(2):
                kb.dma("sp", a2t[tt][:], a2v[:, :, st * 1024 + tt * 512:st * 1024 + (tt + 1) * 512], reads=["D:A2_T"], writes=["a2t%d" % tt], sem="lda2%d" % tt)
            units = [(he, tt) for he in range(NHE) for tt in range(2)]
            load_he(n, 0)
            for it in gateup_groups(st, 0, n, 0):
                it()
            for ui, (he, tt) in enumerate(units):
                if tt == 0 and he + 1 < NHE:
                    load_he(n + 1, he + 1)
                dn = down_groups(st, he, n, tt)
                if ui + 1 < len(units):
                    he2, tt2 = units[ui + 1]
                    gu = gateup_groups(st, he2, n + (1 if he2 != he else 0), tt2)
                else:
                    gu = []
                for di, d_ in enumerate(dn):
                    d_()
                    if di == 3 and len(gu) > 0:
                        gu[0]()
                    if di == 11 and len(gu) > 1:
                        gu[1]()
                if tt == 1:
                    n += 1
            for ch in range(8):
                c = st * 8 + ch
                kb.dma("sp", FOUT[c * 128:(c + 1) * 128, :], acc[:, ch, :], reads=["acc%d_%d" % (ch, db) for db in range(4)], writes=["D:FOUT"], sem="stfo%d" % ch)
        kb.barrier()

    if stop_here("moe"):
        return nc
    with ExitStack() as pes:
        g2r = kb.sb(pes, "g2r", [128, D], F32)
        l2g = kb.sb(pes, "l2g", [128, D], F32)
        l2b = kb.sb(pes, "l2b", [128, D], F32)
        fs = [kb.sb(pes, "ff%d" % i, [128, D], F32) for i in range(2)]
        hs = [kb.sb(pes, "fh%d" % i, [128, D], F32) for i in range(2)]
        bst = kb.sb(pes, "bstf", [128, 4, 6], F32)
        mv = kb.sb(pes, "mvf", [128, 4], F32)
        row_load(g2r[:], ADAFLAT[5 * D:6 * D], "g2r")
        row_load(l2g[:], ln2_g, "l2g")
        row_load(l2b[:], ln2_b, "l2b")
        for c in range(NCH):
            f = fs[c % 2]; fk = "ff%d" % (c % 2)
            h = hs[c % 2]; hk = "fh%d" % (c % 2)
            kb.dma("sp", f[:], FOUT[c * 128:(c + 1) * 128, :], reads=["D:FOUT"], writes=[fk], sem="ldf%d" % (c % 2))
            kb.dma("sp", h[:], H1[c * 128:(c + 1) * 128, :], reads=["D:H1"], writes=[hk], sem="ldfh%d" % (c % 2))
            kb.dve(lambda g_: g_.tensor_tensor(out=f[:], in0=f[:], in1=g2r[:], op=ALU.mult), reads=[fk, "g2r"], writes=[fk])
            kb.dve(lambda v: v.scalar_tensor_tensor(out=f[:], in0=h[:], scalar=ALPHA, in1=f[:], op0=ALU.mult, op1=ALU.add), reads=[fk, hk], writes=[fk])
            for q in range(4):
                kb.dve(lambda v: v.bn_stats(out=bst[:, q, :], in_=f[:, q * 512:(q + 1) * 512]), reads=[fk], writes=["bstf"])
            kb.dve(lambda v: v.bn_aggr(out=mv[:, 0:2], in_=bst[:].rearrange("p a b -> p (a b)")), reads=["bstf"], writes=["mvf"])
            kb.act(lambda a: a.activation(out=mv[:, 2:3], in_=mv[:, 1:2], func=AF.Sqrt, bias=EPS), reads=["mvf"], writes=["mvf2"])
            kb.dve(lambda v: v.reciprocal(out=mv[:, 2:3], in_=mv[:, 2:3]), reads=["mvf2"], writes=["mvf2"])
            kb.dve(lambda v: v.scalar_tensor_tensor(out=mv[:, 3:4], in0=mv[:, 0:1], scalar=-1.0, in1=mv[:, 2:3], op0=ALU.mult, op1=ALU.mult),
                   reads=["mvf", "mvf2"], writes=["mvf3"])
            kb.act(lambda a: a.activation(out=f[:], in_=f[:], func=AF.Identity, scale=mv[:, 2:3], bias=mv[:, 3:4]), reads=[fk, "mvf2", "mvf3"], writes=[fk])
            kb.dve(lambda v: v.tensor_tensor(out=f[:], in0=f[:], in1=l2g[:], op=ALU.mult), reads=[fk, "l2g"], writes=[fk])
            kb.dve(lambda g_: g_.tensor_tensor(out=f[:], in0=f[:], in1=l2b[:], op=ALU.add), reads=[fk, "l2b"], writes=[fk])
            kb.dma("sp", y[c * 128:(c + 1) * 128, :], f[:], reads=[fk], writes=["D:y"], sem="sty%d" % (c % 2))
    kb.finish(["D:y"])
    kb.barrier()
    return nc


def _prep_inputs(inputs):
    f = lambda a: np.ascontiguousarray(np.asarray(a, dtype=np.float32))
    sq = lambda a: f(a)[0]
    shared = {
        "c_ctx": f(inputs["c_ctx"]), "ln_in_g": f(inputs["ln_in_g"]), "ln_in_b": f(inputs["ln_in_b"]),
        "w_ada": sq(inputs["w_ada"]), "b_ada": sq(inputs["b_ada"]), "w_in": sq(inputs["w_in"]),
        "conv_w": sq(inputs["conv_w"]), "conv_b": sq(inputs["conv_b"]),
        "dt_bias": sq(inputs["dt_bias"]).reshape(64), "a_log": sq(inputs["a_log"]).reshape(64),
        "d_skip": sq(inputs["d_skip"]), "ssd_norm_w": sq(inputs["ssd_norm_w"]), "w_ssd_br": sq(inputs["w_ssd_br"]),
        "cmlp_ln_g": sq(inputs["cmlp_ln_g"]), "cmlp_ln_b": sq(inputs["cmlp_ln_b"]), "cmlp_ws": sq(inputs["cmlp_ws"]),
        "cmlp_bs": sq(inputs["cmlp_bs"]), "w_cmlp_br": sq(inputs["w_cmlp_br"]), "w_o": sq(inputs["w_o"]),
        "ln1_g": sq(inputs["ln1_g"]), "ln1_b": sq(inputs["ln1_b"]), "w_router": sq(inputs["w_router"]),
        "router_bias": sq(inputs["router_bias"]), "w_e_gate": sq(inputs["w_e_gate"]), "w_e_up": sq(inputs["w_e_up"]),
        "w_e_down": sq(inputs["w_e_down"]), "w_sh_gate": sq(inputs["w_sh_gate"]), "w_sh_up": sq(inputs["w_sh_up"]),
        "w_sh_down": sq(inputs["w_sh_down"]), "ln2_g": sq(inputs["ln2_g"]), "ln2_b": sq(inputs["ln2_b"]),
    }
    i = np.arange(128)
    lp, l = i[:, None], i[None, :]
    masks = np.stack([(lp == l), (lp <= l), (lp > l), (lp >= l), (lp < l), np.ones((128, 128), bool)], axis=1)
    shared["cmask"] = np.ascontiguousarray(masks.astype(np.float32).reshape(128, 6 * 128))
    xs, cs, cx = f(inputs["x"]), f(inputs["c"]), f(inputs["ctx"])
    in_maps = []
    for b in range(8):
        m = dict(shared)
        m["x"] = xs[b]
        m["c"] = cs[b]
        m["ctx"] = cx[b]
        in_maps.append(m)
    return in_maps


def kernel(**inputs):
    nc = build_program(DEBUG)
    in_maps = _prep_inputs(inputs)
    res = run_bass_kernel_spmd(nc, in_maps, core_ids=list(range(8)))
    return np.stack([r["y"] for r in res.results], axis=0)
```

```python
import math
from contextlib import ExitStack

import numpy as np
import concourse.bass as bass
import concourse.mybir as mybir
from concourse.bass_utils import run_bass_kernel_spmd

F32 = mybir.dt.float32
BF16 = mybir.dt.bfloat16
I32 = mybir.dt.int32
AF = mybir.ActivationFunctionType
ALU = mybir.AluOpType

T = 4096
CTX = 256
D = 2048
KD = 16
NCH = T // 128
IN_DIM = 13376
XBC = 3072
NE = 64
ALPHA = 2.0 ** 0.25
EPS = 1e-5

DEBUG = None


class KB:
    def __init__(self):
        self.nc = bass.Bass("TRN2", target_bir_lowering=False)
        self.es = ExitStack()
        nc = self.nc
        self.eng = {"pe": nc.tensor, "dve": nc.vector, "act": nc.scalar, "pool": nc.gpsimd, "sp": nc.sync}
        self.sems = {}
        self.cnt = {}
        for e in self.eng:
            self.sems["s_" + e] = self.es.enter_context(nc.semaphore("s_" + e))
            self.cnt["s_" + e] = 0
        self.waited = {e: {} for e in self.eng}
        self.lastw = {}
        self.readers = {}
        self.same_sync = {"pe": False, "dve": True, "act": True, "pool": True, "sp": False}
        self.nps = 0

    def sb(self, es, name, shape, dt):
        self.nps += 1
        return es.enter_context(self.nc.sbuf_tensor("%s_%d" % (name, self.nps), shape, dt))

    def sem(self, name):
        if name not in self.sems:
            self.sems[name] = self.es.enter_context(self.nc.semaphore(name))
            self.cnt[name] = 0
        return self.sems[name]

    def _deps(self, e, reads, writes):
        req = {}

        def add(d):
            for sk, v in d.items():
                if req.get(sk, 0) < v:
                    req[sk] = v

        for k in reads:
            add(self.lastw.get(k, {}))
        for k in writes:
            add(self.lastw.get(k, {}))
            add(self.readers.get(k, {}))
        w = self.waited[e]
        for sk, v in req.items():
            if sk == "s_" + e and not self.same_sync[e]:
                continue
            if w.get(sk, 0) >= v:
                continue
            self.eng[e].wait_ge(self.sems[sk], v)
            w[sk] = v

    def _record(self, sk, v, reads, writes):
        for k in reads:
            d = self.readers.setdefault(k, {})
            if d.get(sk, 0) < v:
                d[sk] = v
        for k in writes:
            if k.startswith("D:"):
                d = self.lastw.setdefault(k, {})
                if d.get(sk, 0) < v:
                    d[sk] = v
            else:
                self.lastw[k] = {sk: v}
                self.readers[k] = {}

    def op(self, e, fn, reads=(), writes=()):
        self._deps(e, reads, writes)
        ins = fn(self.eng[e])
        sk = "s_" + e
        self.cnt[sk] += 1
        ins.then_inc(self.sems[sk], 1)
        self._record(sk, self.cnt[sk], reads, writes)
        return ins

    def dve(self, fn, reads=(), writes=()):
        return self.op("dve", fn, reads, writes)

    def act(self, fn, reads=(), writes=()):
        return self.op("act", fn, reads, writes)

    def pool(self, fn, reads=(), writes=()):
        return self.op("pool", fn, reads, writes)

    def mm(self, out, pairs, reads=(), writes=(), first_start=True):
        self._deps("pe", reads, writes)
        n = len(pairs)
        ins = None
        for i, (l, r) in enumerate(pairs):
            ins = self.nc.tensor.matmul(out, lhsT=l, rhs=r, start=(i == 0 and first_start), stop=(i == n - 1))
        self.cnt["s_pe"] += 1
        ins.then_inc(self.sems["s_pe"], 1)
        self._record("s_pe", self.cnt["s_pe"], reads, writes)

    def tr(self, out, in_, ident, reads=(), writes=()):
        self._deps("pe", reads, writes)
        ins = self.nc.tensor.transpose(out=out, in_=in_, identity=ident)
        self.cnt["s_pe"] += 1
        ins.then_inc(self.sems["s_pe"], 1)
        self._record("s_pe", self.cnt["s_pe"], reads, writes)

    def dma(self, q, out, in_, reads=(), writes=(), sem="dm", **kw):
        self._deps(q, reads, writes)
        s = self.sem(sem)
        ins = self.eng[q].dma_start(out=out, in_=in_, **kw)
        self.cnt[sem] += 16
        ins.then_inc(s, 16)
        self._record(sem, self.cnt[sem], reads, writes)

    def barrier(self):
        for e in self.eng:
            w = self.waited[e]
            for sk, v in self.cnt.items():
                if v == 0 or sk == "s_" + e or w.get(sk, 0) >= v:
                    continue
                self.eng[e].wait_ge(self.sems[sk], v)
                w[sk] = v

    def finish(self, keys):
        self._deps("sp", keys, ())
        self._deps("act", keys, ())


def build_program(dbg=None):
    kb = KB()
    nc = kb.nc
    dbg = dbg or {}
    dump = set(dbg.get("dump", []))
    stop = dbg.get("stop")

    def din(name, shape):
        return nc.dram_tensor(name, list(shape), F32, kind="ExternalInput").ap()

    def dscr(name, shape, dt=F32):
        kind = "ExternalOutput" if name in dump else "Internal"
        return nc.dram_tensor(name, list(shape), dt, kind=kind).ap()

    x = din("x", [T, D])
    ctx = din("ctx", [CTX, D])
    cvec = din("c", [D])
    c_ctx = din("c_ctx", [D])
    ln_in_g = din("ln_in_g", [D])
    ln_in_b = din("ln_in_b", [D])
    w_ada = din("w_ada", [D, 6 * D])
    b_ada = din("b_ada", [6 * D])
    w_in = din("w_in", [D, IN_DIM])
    conv_w = din("conv_w", [5, XBC])
    conv_b = din("conv_b", [XBC])
    dt_bias = din("dt_bias", [64])
    a_log = din("a_log", [64])
    d_skip = din("d_skip", [32])
    ssd_norm_w = din("ssd_norm_w", [D])
    w_ssd_br = din("w_ssd_br", [D, D])
    cmlp_ln_g = din("cmlp_ln_g", [D])
    cmlp_ln_b = din("cmlp_ln_b", [D])
    cmlp_ws = din("cmlp_ws", [8, 128, 128])
    cmlp_bs = din("cmlp_bs", [8, 128])
    w_cmlp_br = din("w_cmlp_br", [D, D])
    w_o = din("w_o", [D, D])
    ln1_g = din("ln1_g", [D])
    ln1_b = din("ln1_b", [D])
    w_router = din("w_router", [D, NE])
    router_bias = din("router_bias", [NE])
    w_e_gate = din("w_e_gate", [NE, D, 512])
    w_e_up = din("w_e_up", [NE, D, 512])
    w_e_down = din("w_e_down", [NE, 512, D])
    w_sh_gate = din("w_sh_gate", [D, 512])
    w_sh_up = din("w_sh_up", [D, 512])
    w_sh_down = din("w_sh_down", [512, D])
    ln2_g = din("ln2_g", [D])
    ln2_b = din("ln2_b", [D])
    cmask = din("cmask", [128, 6 * 128])
    y = nc.dram_tensor("y", [T, D], F32, kind="ExternalOutput").ap()

    RPOS = dscr("RPOS", [64, 1024])
    ADAROW = dscr("ADAROW", [96, 128])
    XN = dscr("XN", [T, D])
    XBC_T = dscr("XBC_T", [XBC, T + 4], BF16)
    XBC_C = dscr("XBC_C", [XBC, CTX + 4], BF16)
    ZS = dscr("ZS", [T, D])
    U_T = dscr("U_T", [D, T])
    VG = dscr("VG", [T, D])
    GS_T = dscr("GS_T", [D, T])
    GC_T = dscr("GC_T", [D, T])
    SBW = dscr("SBW", [NCH, 128, D], BF16)
    YN_T = dscr("YN_T", [D, T], BF16)
    CM_T = dscr("CM_T", [D, T], BF16)
    PART_T = dscr("PART_T", [D, T])
    MG_T = dscr("MG_T", [D, T], BF16)
    H1 = dscr("H1", [T, D])
    A2_T = dscr("A2_T", [D, T], BF16)
    FOUT = dscr("FOUT", [T, D])
    DBG_A = dscr("DBG_A", [128, 4096]) if "DBG_A" in dump else None

    es = kb.es

    def stop_here(name):
        if stop != name:
            return False
        kb.barrier()
        return True

    PS = [es.enter_context(nc.psum_tensor("ps%d" % i, [128, 512], F32)) for i in range(8)]
    PK = ["ps%d" % i for i in range(8)]

    cm = kb.sb(es, "cm", [128, 6, 128], F32)
    IDN, MU, MSL, ML, MSU, ONES = 0, 1, 2, 3, 4, 5
    dt_all = kb.sb(es, "dt_all", [128, NCH, 64], F32)
    dt_ctx = kb.sb(es, "dt_ctx", [128, 2, 64], F32)
    gates = kb.sb(es, "gates", [128, NCH, NE + 1], F32)
    cols = kb.sb(es, "cols", [128, 40, 16], F32)
    adac = kb.sb(es, "adac", [128, 96, 2], F32)
    convw = kb.sb(es, "convw", [128, 5, 24], F32)
    convb = kb.sb(es, "convb", [128, 24], F32)
    rows64 = kb.sb(es, "rows64", [128, 4, 64], F32)
    stats = kb.sb(es, "stats", [128, 64], F32)
    tmpr = kb.sb(es, "tmpr", [128, 128], F32)
    C_LNG, C_LNB, C_A1L, C_B1L, C_A1C, C_B1C, C_A2, C_B2, C_CMG, C_T0, C_T1 = range(11)

    kb.dma("sp", cm[:].rearrange("p a b -> p (a b)"), cmask[:, :], writes=["cm"], sem="k_cm")
    ident = cm[:, IDN, :]

    def col_load(dst, src1d, n, key):
        kb.dma("sp", tmpr[0:n, :], src1d.rearrange("(j p) -> j p", p=128), writes=["tmpr"], sem="k_tmpr")
        kb.tr(PS[0][:, 0:n], tmpr[0:n, :], cm[0:n, IDN, 0:n], reads=["tmpr", "cm"], writes=[PK[0]])
        kb.dve(lambda v: v.tensor_copy(out=dst, in_=PS[0][:, 0:n]), reads=[PK[0]], writes=[key])

    def row_load(dst, src1d, key, q="sp", sem=None):
        kb.dma(q, dst, src1d.partition_broadcast(128), writes=[key], sem="k_" + key)

    col_load(cols[:, C_LNG, :], ln_in_g, 16, "cols")
    col_load(cols[:, C_LNB, :], ln_in_b, 16, "cols")
    for k in range(5):
        col_load(convw[:, k, :], conv_w[k, :], 24, "convw")
    col_load(convb[:, :], conv_b, 24, "convb")
    row_load(rows64[:, 0, :], a_log, "rows64")
    row_load(rows64[:, 1, :], dt_bias, "rows64")
    row_load(rows64[:, 2, :], router_bias, "rows64")
    row_load(rows64[:, 3, 0:32], d_skip, "rows64")
    kb.act(lambda a: a.activation(out=rows64[:, 0, :], in_=rows64[:, 0, :], func=AF.Exp), reads=["rows64"], writes=["rows64"])
    kb.dve(lambda v: v.tensor_scalar(out=rows64[:, 0, :], in0=rows64[:, 0, :], scalar1=-1.0, scalar2=None, op0=ALU.mult),
           reads=["rows64"], writes=["rows64"])

    with ExitStack() as pes:
        ccol = kb.sb(pes, "ccol", [128, 16, 2], F32)
        craw = kb.sb(pes, "craw", [128, 32], F32)
        bcol = kb.sb(pes, "bcol", [128, 96], F32)
        wts = [kb.sb(pes, "wada%d" % i, [128, 16, 512], F32) for i in range(2)]
        col_load(craw[:, 0:16], cvec, 16, "craw")
        col_load(craw[:, 16:32], c_ctx, 16, "craw")
        col_load(bcol[:, :], b_ada, 96, "bcol")
        kb.act(lambda a: a.activation(out=ccol[:, :, 0], in_=craw[:, 0:16], func=AF.Silu), reads=["craw"], writes=["ccol"])
        kb.act(lambda a: a.activation(out=ccol[:, :, 1], in_=craw[:, 16:32], func=AF.Silu), reads=["craw"], writes=["ccol"])
        wav = w_ada.rearrange("(kc p) n -> p kc n", p=128)
        NT_A = 24

        def load_wada(ct):
            kb.dma("sp", wts[ct % 2][:], wav[:, :, ct * 512:(ct + 1) * 512], writes=["wada%d" % (ct % 2)], sem="wada%d" % (ct % 2))

        load_wada(0)
        for ct in range(NT_A):
            if ct + 1 < NT_A:
                load_wada(ct + 1)
            wt = wts[ct % 2]
            bank = 1 + (ct % 2)
            for cb in range(4):
                kb.mm(PS[bank][:, cb * 2:cb * 2 + 2],
                      [(wt[:, kc, cb * 128:(cb + 1) * 128], ccol[:, kc, :]) for kc in range(16)],
                      reads=["wada%d" % (ct % 2), "ccol"], writes=[PK[bank]])
            j0 = ct * 4
            kb.dve(lambda v: v.tensor_tensor(out=adac[:, j0:j0 + 4, :],
                                             in0=PS[bank][:, 0:8].rearrange("p (a b) -> p a b", b=2),
                                             in1=bcol[:, j0:j0 + 4].unsqueeze(2).to_broadcast([128, 4, 2]), op=ALU.add),
                   reads=[PK[bank], "bcol"], writes=["adac"])
        for w, (ca, cbb) in enumerate(((C_A1L, C_B1L), (C_A1C, C_B1C))):
            kb.dve(lambda v: v.tensor_scalar(out=cols[:, C_T0, :], in0=adac[:, 16:32, w], scalar1=1.0, scalar2=None, op0=ALU.add),
                   reads=["adac"], writes=["colsT"])
            kb.dve(lambda v: v.tensor_tensor(out=cols[:, ca, :], in0=cols[:, C_LNG, :], in1=cols[:, C_T0, :], op=ALU.mult),
                   reads=["colsT", "cols"], writes=["colsA%d" % w])
            kb.dve(lambda v: v.tensor_tensor(out=cols[:, C_T1, :], in0=cols[:, C_LNB, :], in1=cols[:, C_T0, :], op=ALU.mult),
                   reads=["colsT", "cols"], writes=["colsT1"])
            kb.dve(lambda v: v.tensor_tensor(out=cols[:, cbb, :], in0=cols[:, C_T1, :], in1=adac[:, 0:16, w], op=ALU.add),
                   reads=["colsT1", "adac"], writes=["colsB%d" % w])
        kb.dve(lambda v: v.tensor_scalar(out=cols[:, C_A2, :], in0=adac[:, 64:80, 0], scalar1=1.0, scalar2=None, op0=ALU.add),
               reads=["adac"], writes=["colsA2"])
        kb.dve(lambda v: v.tensor_copy(out=cols[:, C_B2, :], in_=adac[:, 48:64, 0]), reads=["adac"], writes=["colsB2"])
        kb.dve(lambda v: v.tensor_copy(out=bcol[:, :], in_=adac[:, :, 0]), reads=["adac"], writes=["bcol"])
        kb.tr(PS[0][0:96, 0:128], bcol[:, 0:96], ident, reads=["bcol", "cm"], writes=[PK[0]])
        kb.dve(lambda v: v.tensor_copy(out=tmpr[0:96, :], in_=PS[0][0:96, 0:128]), reads=[PK[0]], writes=["tmpr"])
        kb.dma("sp", ADAROW[:, :], tmpr[0:96, :], reads=["tmpr"], writes=["D:ADAROW"], sem="st0")
        kb.barrier()
    ADAFLAT = ADAROW.rearrange("a b -> (a b)")

    PC = kb.sb(es, "PC", [128, 1024], F32)
    with ExitStack() as pes:
        ji = kb.sb(pes, "ji", [128, 512], I32)
        om = kb.sb(pes, "om", [128, 512], F32)
        pi_ = kb.sb(pes, "pi_", [128, 1], I32)
        pf = kb.sb(pes, "pf", [128, 2], F32)
        kf = kb.sb(pes, "kf", [128, 512], F32)
        ki = kb.sb(pes, "ki", [128, 512], I32)
        kb.pool(lambda g: g.iota(out=ji[:], pattern=[[1, 512]], base=0, channel_multiplier=0), writes=["ji"])
        kb.pool(lambda g: g.iota(out=pi_[:], pattern=[[1, 1]], base=0, channel_multiplier=1), writes=["pi_"])
        kb.dve(lambda v: v.tensor_copy(out=om[:], in_=ji[:]), reads=["ji"], writes=["om"])
        kb.dve(lambda v: v.tensor_copy(out=pf[:, 0:1], in_=pi_[:]), reads=["pi_"], writes=["pf"])
        kb.dve(lambda v: v.tensor_scalar(out=pf[:, 1:2], in0=pf[:, 0:1], scalar1=63.5, scalar2=-64.0, op0=ALU.is_gt, op1=ALU.mult),
               reads=["pf"], writes=["pf1"])
        kb.dve(lambda v: v.tensor_tensor(out=pf[:, 0:1], in0=pf[:, 0:1], in1=pf[:, 1:2], op=ALU.add), reads=["pf", "pf1"], writes=["pf"])
        kb.act(lambda a: a.activation(out=om[:], in_=om[:], func=AF.Exp, scale=-math.log(10000.0) / 512.0), reads=["om"], writes=["om"])
        kb.dve(lambda v: v.tensor_scalar(out=om[:], in0=om[:], scalar1=pf[:, 0:1], scalar2=None, op0=ALU.mult), reads=["om", "pf"], writes=["om"])
        for half, shift in ((0, 0.0), (1, math.pi / 2)):
            dst = PC[:, half * 512:(half + 1) * 512]
            key = "PC%d" % half
            kb.dve(lambda v: v.tensor_scalar(out=dst, in0=om[:], scalar1=shift, scalar2=None, op0=ALU.add), reads=["om"], writes=[key])
            kb.dve(lambda v: v.tensor_scalar(out=kf[:], in0=dst, scalar1=1.0 / (2 * math.pi), scalar2=None, op0=ALU.mult), reads=[key], writes=["kf"])
            kb.dve(lambda v: v.tensor_copy(out=ki[:], in_=kf[:]), reads=["kf"], writes=["ki"])
            kb.dve(lambda v: v.tensor_copy(out=kf[:], in_=ki[:]), reads=["ki"], writes=["kf"])
            kb.dve(lambda v: v.scalar_tensor_tensor(out=dst, in0=kf[:], scalar=-2 * math.pi, in1=dst, op0=ALU.mult, op1=ALU.add),
                   reads=["kf", key], writes=[key])
            kb.dve(lambda v: v.tensor_scalar(out=kf[:], in0=dst, scalar1=math.pi, scalar2=-2 * math.pi, op0=ALU.is_gt, op1=ALU.mult), reads=[key], writes=["kf"])
            kb.dve(lambda v: v.tensor_tensor(out=dst, in0=dst, in1=kf[:], op=ALU.add), reads=["kf", key], writes=[key])
            kb.dve(lambda v: v.tensor_scalar(out=kf[:], in0=dst, scalar1=-math.pi, scalar2=2 * math.pi, op0=ALU.is_lt, op1=ALU.mult), reads=[key], writes=["kf"])
            kb.dve(lambda v: v.tensor_tensor(out=dst, in0=dst, in1=kf[:], op=ALU.add), reads=["kf", key], writes=[key])
            kb.act(lambda a: a.activation(out=dst, in_=dst, func=AF.Sin), reads=[key], writes=[key])
        kb.dma("sp", RPOS[:, :], PC[0:64, :], reads=["PC0", "PC1"], writes=["D:RPOS"], sem="st0")
        kb.barrier()

    wiv = w_in.rearrange("(kc p) n -> p kc n", p=128)

    def softplus_dt(ps_ap, pskey, dst, dkey):
        kb.dve(lambda v: v.tensor_tensor(out=stats[:, 0:64], in0=ps_ap, in1=rows64[:, 1, :], op=ALU.add), reads=["rows64", pskey], writes=["sp_x"])
        kb.dve(lambda v: v.scalar_tensor_tensor(out=dst, in0=stats[:, 0:64], scalar=-1.0, in1=stats[:, 0:64], op0=ALU.mult, op1=ALU.max), reads=["sp_x"], writes=[dkey])
        kb.act(lambda a: a.activation(out=dst, in_=dst, func=AF.Exp, scale=-1.0), reads=[dkey], writes=[dkey])
        kb.act(lambda a: a.activation(out=dst, in_=dst, func=AF.Ln, bias=1.0), reads=[dkey], writes=[dkey])
        kb.dve(lambda v: v.scalar_tensor_tensor(out=dst, in0=stats[:, 0:64], scalar=0.0, in1=dst, op0=ALU.max, op1=ALU.add),
               reads=["sp_x", dkey], writes=[dkey])

    def proj_phase(src, ntok, a_col, b_col, with_pos, xbc_dst, dt_dst, full, tag):
        NT = min(1024, ntok)
        nsup = ntok // NT
        TW = min(512, NT)
        with ExitStack() as pes:
            aT = kb.sb(pes, "aT" + tag, [128, KD, NT], BF16)
            xts = [kb.sb(pes, "xt%d%s" % (i, tag), [128, D], F32) for i in range(2)]
            xns = [kb.sb(pes, "xn%d%s" % (i, tag), [128, D], F32) for i in range(2)]
            pts = [kb.sb(pes, "pt%d%s" % (i, tag), [128, 1024], F32) for i in range(2)] if with_pos else None
            bst = kb.sb(pes, "bst" + tag, [128, 4, 6], F32)
            if full:
                lngr = kb.sb(pes, "lngr", [128, D], F32)
                lnbr = kb.sb(pes, "lnbr", [128, D], F32)
                hls = [kb.sb(pes, "hl%d" % i, [128, D], F32) for i in range(2)]
                row_load(lngr[:], ln_in_g, "lngr")
                row_load(lnbr[:], ln_in_b, "lnbr")
            mv = kb.sb(pes, "mv" + tag, [128, 4], F32)
            wbs = [kb.sb(pes, "wb%d%s" % (i, tag), [128, KD, 512], BF16) for i in range(2)]
            NSTG = 4
            stg = [kb.sb(pes, "stg%d%s" % (i, tag), [128, 512], F32) for i in range(NSTG)]
            stgb = [kb.sb(pes, "stgb%d%s" % (i, tag), [128, 512], BF16) for i in range(2)]
            segs = [("xbc", 0, XBC, "F"), ("dt", XBC, 64, "T")]
            if full:
                segs += [("z", 3136, D, "T"), ("u", 5184, D, "F"), ("v", 7232, D, "T"), ("gs", 9280, D, "F"), ("gc", 11328, D, "F")]
            wtiles = []
            for (nm, c0, ncol, lay) in segs:
                o = 0
                while o < ncol:
                    w = min(512, ncol - o)
                    wtiles.append((nm, c0 + o, o, w, lay))
                    o += w
            sti = [0]
            for s in range(nsup):
                for ci in range(NT // 128):
                    c = s * (NT // 128) + ci
                    xt = xts[c % 2]; xk = "xt%d%s" % (c % 2, tag)
                    xn = xns[c % 2]; nk = "xn%d%s" % (c % 2, tag)
                    kb.dma("sp", xt[:], src[c * 128:(c + 1) * 128, :], writes=[xk], sem="ldx%d" % (c % 2))
                    if with_pos:
                        pt = pts[c % 2]; pk = "pt%d%s" % (c % 2, tag)
                        kb.dma("sp", pt[0:64, :], RPOS[2 * c, :].partition_broadcast(64), reads=["D:RPOS"], writes=[pk], sem="ldp%d" % (c % 2))
                        kb.dma("sp", pt[64:128, :], RPOS[2 * c + 1, :].partition_broadcast(64), reads=["D:RPOS"], writes=[pk], sem="ldp%d" % (c % 2))
                        kb.dve(lambda v: v.tensor_tensor(out=xt[:, 0:1024], in0=xt[:, 0:1024], in1=pt[:], op=ALU.add), reads=[xk, pk], writes=[xk])
                        kb.dve(lambda g: g.tensor_tensor(out=xt[:, 1024:2048], in0=xt[:, 1024:2048], in1=PC[:], op=ALU.add),
                                reads=[xk, "PC0", "PC1"], writes=[xk + "h"])
                    for q in range(4):
                        kb.dve(lambda v: v.bn_stats(out=bst[:, q, :], in_=xt[:, q * 512:(q + 1) * 512]), reads=[xk, xk + "h"], writes=["bst" + tag])
                    kb.dve(lambda v: v.bn_aggr(out=mv[:, 0:2], in_=bst[:].rearrange("p a b -> p (a b)")), reads=["bst" + tag], writes=["mv" + tag])
                    kb.act(lambda a: a.activation(out=mv[:, 2:3], in_=mv[:, 1:2], func=AF.Sqrt, bias=EPS), reads=["mv" + tag], writes=["mv2" + tag])
                    kb.dve(lambda v: v.reciprocal(out=mv[:, 2:3], in_=mv[:, 2:3]), reads=["mv2" + tag], writes=["mv2" + tag])
                    kb.dve(lambda v: v.scalar_tensor_tensor(out=mv[:, 3:4], in0=mv[:, 0:1], scalar=-1.0, in1=mv[:, 2:3], op0=ALU.mult, op1=ALU.mult),
                           reads=["mv" + tag, "mv2" + tag], writes=["mv3" + tag])
                    kb.act(lambda a: a.activation(out=xn[:], in_=xt[:], func=AF.Identity, scale=mv[:, 2:3], bias=mv[:, 3:4]),
                           reads=[xk, xk + "h", "mv2" + tag, "mv3" + tag], writes=[nk])
                    if full:
                        hl = hls[c % 2]; hk = "hl%d" % (c % 2)
                        kb.dve(lambda v: v.tensor_tensor(out=hl[:], in0=xn[:], in1=lngr[:], op=ALU.mult), reads=[nk, "lngr"], writes=[hk])
                        kb.dve(lambda g: g.tensor_tensor(out=hl[:], in0=hl[:], in1=lnbr[:], op=ALU.add), reads=[hk, "lnbr"], writes=[hk])
                        kb.dma("sp", XN[c * 128:(c + 1) * 128, :], hl[:], reads=[hk], writes=["D:XN"], sem="stn%d" % (c % 2))
                    for qd in range(4):
                        bank = qd % 4
                        for j in range(4):
                            dk = qd * 4 + j
                            kb.tr(PS[bank][:, j * 128:(j + 1) * 128], xn[:, dk * 128:(dk + 1) * 128], ident, reads=[nk, "cm"], writes=[PK[bank]])
                        for j in range(4):
                            dk = qd * 4 + j
                            kb.act(lambda a: a.activation(out=aT[:, dk, ci * 128:(ci + 1) * 128], in_=PS[bank][:, j * 128:(j + 1) * 128],
                                                          func=AF.Identity, scale=a_col[:, dk:dk + 1], bias=b_col[:, dk:dk + 1]),
                                   reads=[PK[bank], "colsA0", "colsA1", "colsB0", "colsB1"], writes=["aT%s_%d" % (tag, ci // 4)])
                aTkeys = ["aT%s_%d" % (tag, i) for i in range(max(1, NT // 512))]

                def load_w(i):
                    nm, cabs, o, w, lay = wtiles[i]
                    kb.dma("pool", wbs[i % 2][:, :, 0:w], wiv[:, :, cabs:cabs + w], writes=["wb%d%s" % (i % 2, tag)], sem="ldw%d" % (i % 2))

                load_w(0)
                for i, (nm, cabs, o, w, lay) in enumerate(wtiles):
                    if i + 1 < len(wtiles):
                        load_w(i + 1)
                    wb = wbs[i % 2]; wk = "wb%d%s" % (i % 2, tag)
                    if lay == "F":
                        for cb in range(w // 128):
                            for tt in range(NT // TW):
                                bank = 4 + (sti[0] % 4)
                                kb.mm(PS[bank][:, 0:TW], [(wb[:, k, cb * 128:(cb + 1) * 128], aT[:, k, tt * TW:(tt + 1) * TW]) for k in range(KD)],
                                      reads=[wk] + aTkeys, writes=[PK[bank]])
                                row0 = o + cb * 128
                                tok0 = s * NT + tt * TW
                                if nm == "xbc":
                                    sg = stgb[sti[0] % 2]; sk = "stgb%d%s" % (sti[0] % 2, tag)
                                    kb.dve(lambda v: v.tensor_copy(out=sg[:, 0:TW], in_=PS[bank][:, 0:TW]), reads=[PK[bank]], writes=[sk])
                                    kb.dma("sp", xbc_dst[row0:row0 + 128, 2 + tok0:2 + tok0 + TW], sg[:, 0:TW], reads=[sk], writes=["D:XBC" + tag],
                                           sem="stb%d" % (sti[0] % 2))
                                else:
                                    sg = stg[sti[0] % NSTG]; sk = "stg%d%s" % (sti[0] % NSTG, tag)
                                    fn = AF.Gelu_apprx_tanh if nm == "u" else AF.Sigmoid
                                    dst = {"u": U_T, "gs": GS_T, "gc": GC_T}[nm]
                                    kb.act(lambda a: a.activation(out=sg[:, 0:TW], in_=PS[bank][:, 0:TW], func=fn), reads=[PK[bank]], writes=[sk])
                                    kb.dma("sp", dst[row0:row0 + 128, tok0:tok0 + TW], sg[:, 0:TW], reads=[sk], writes=["D:" + nm], sem="stf%d" % (sti[0] % NSTG))
                                sti[0] += 1
                    else:
                        for tc in range(NT // 128):
                            bank = 4 + (sti[0] % 4)
                            kb.mm(PS[bank][:, 0:w], [(aT[:, k, tc * 128:(tc + 1) * 128], wb[:, k, 0:w]) for k in range(KD)],
                                  reads=[wk] + aTkeys, writes=[PK[bank]])
                            c = s * (NT // 128) + tc
                            if nm == "dt":
                                softplus_dt(PS[bank][:, 0:64], PK[bank], dt_dst[:, c, :], "dt" + tag)
                            else:
                                sg = stg[sti[0] % NSTG]; sk = "stg%d%s" % (sti[0] % NSTG, tag)
                                fn = AF.Silu if nm == "z" else AF.Gelu_apprx_tanh
                                dst = ZS if nm == "z" else VG
                                kb.act(lambda a: a.activation(out=sg[:, 0:w], in_=PS[bank][:, 0:w], func=fn), reads=[PK[bank]], writes=[sk])
                                kb.dma("sp", dst[c * 128:(c + 1) * 128, o:o + w], sg[:, 0:w], reads=[sk], writes=["D:" + nm], sem="stf%d" % (sti[0] % NSTG))
                            sti[0] += 1
            kb.barrier()

    zt = kb.sb(es, "zt", [128, 2], BF16)
    kb.dve(lambda v: v.memset(zt[:], 0.0), writes=["zt"])
    for dst_, n_, tg in ((XBC_T, T, "L"), (XBC_C, CTX, "C")):
        for b in range(24):
            kb.dma("sp", dst_[b * 128:(b + 1) * 128, 0:2], zt[:], reads=["zt"], writes=["D:XBC" + tg], sem="st0")
            kb.dma("sp", dst_[b * 128:(b + 1) * 128, n_ + 2:n_ + 4], zt[:], reads=["zt"], writes=["D:XBC" + tg], sem="st0")

    SKIP = dbg.get("skip", False)
    if not SKIP:
        proj_phase(ctx, CTX, cols[:, C_A1C, :], cols[:, C_B1C, :], False, XBC_C, dt_ctx, False, "C")
        proj_phase(x, T, cols[:, C_A1L, :], cols[:, C_B1L, :], True, XBC_T, dt_all, True, "L")

    if stop == "proj":
        if DBG_A is not None:
            kb.dma("sp", DBG_A[:, 0:2048], dt_all[:].rearrange("p a b -> p (a b)"), reads=["dtL"], writes=["D:DBG_A"], sem="st0")
            kb.dma("sp", DBG_A[:, 2048:2048 + 192], adac[:].rearrange("p a b -> p (a b)"), reads=["adac"], writes=["D:DBG_A"], sem="st0")
            kb.dma("sp", DBG_A[:, 2304:2304 + 128], dt_ctx[:].rearrange("p a b -> p (a b)"), reads=["dtC"], writes=["D:DBG_A"], sem="st0")
            kb.dma("sp", DBG_A[:, 2560:2560 + 1024], PC[:], reads=["PC0", "PC1"], writes=["D:DBG_A"], sem="st0")
        kb.finish([k for k in kb.lastw if k.startswith("D:")])
        return nc

    ssd_es = ExitStack()
    dg = kb.sb(ssd_es, "dg", [128, 120, 128], BF16)
    S_init = kb.sb(ssd_es, "S_init", [128, 2, D], F32)
    for k in range(5):
        for b in range(24):
            kb.dve(lambda v: v.tensor_scalar(out=dg[:, k * 24 + b, :], in0=ident, scalar1=convw[:, k, b:b + 1], scalar2=None, op0=ALU.mult),
                   reads=["cm", "convw"], writes=["dg"])
    wrk = [0]

    def wbank():
        wrk[0] += 1
        return wrk[0] % 2

    def ssd_phase(XSRC, xkey, dtsrc, nch, mode, S_in, S_out_slot):
        with ExitStack() as pes:
            xins = [kb.sb(pes, "xin%d" % i, [128, 24, 132], BF16) for i in range(2)]
            xcT = kb.sb(pes, "xcT", [128, 16, 128], F32)
            BTf = kb.sb(pes, "BTf", [128, 4, 128], F32)
            BT = kb.sb(pes, "BT", [128, 4, 128], BF16)
            CT = kb.sb(pes, "CT", [128, 4, 128], BF16)
            Btok = kb.sb(pes, "Btok", [128, 4, 128], BF16)
            avec = kb.sb(pes, "avec", [128, 64], F32)
            Edec = kb.sb(pes, "Edec", [128, 5, 64], F32)
            w1 = kb.sb(pes, "w1", [128, 64], F32)
            xst = kb.sb(pes, "xst", [128, D], BF16)
            Sf = kb.sb(pes, "Sf", [128, D], F32)
            Sfb = kb.sb(pes, "Sfb", [128, D], BF16)
            xv = XSRC.rearrange("(b p) t -> p b t", p=128)
            if mode == "M":
                Sbb = [kb.sb(pes, "Sbb%d" % i, [128, D], BF16) for i in range(2)]
                zss = [kb.sb(pes, "zs%d" % i, [128, D], F32) for i in range(1)]
                xdt = [kb.sb(pes, "xdt%d" % i, [128, D], BF16) for i in range(2)]
                scm = [kb.sb(pes, "scm%d" % i, [128, 128], BF16) for i in range(2)]
                rhsD = [kb.sb(pes, "rhsD%d" % i, [128, 4, 128], F32) for i in range(2)]
                Ex = [kb.sb(pes, "Ex%d" % i, [128, 512], BF16) for i in range(2)]
                MT = [kb.sb(pes, "MT%d" % i, [128, 4, 128], BF16) for i in range(2)]
                t1 = kb.sb(pes, "t1", [128, 512], F32)
                t2 = kb.sb(pes, "t2", [128, 512], F32)
                ysb = kb.sb(pes, "ysb", [128, D], F32)
                xsk = kb.sb(pes, "xsk", [128, D], F32)
                nwr = kb.sb(pes, "nwr", [128, D], F32)
                ynT = [kb.sb(pes, "ynT%d" % i, [128, KD, 512], BF16) for i in range(1)]
                ss = kb.sb(pes, "ss", [128, 2], F32)
                row_load(nwr[:], ssd_norm_w, "nwr")
            if S_in is None:
                kb.dve(lambda v: v.memset(Sf[:], 0.0), writes=["Sf%d" % g for g in range(4)])
            else:
                kb.dve(lambda v: v.tensor_copy(out=Sf[:], in_=S_in), reads=["S_init"], writes=["Sf%d" % g for g in range(4)])
            kb.act(lambda a: a.copy(out=Sfb[:], in_=Sf[:]), reads=["Sf%d" % g for g in range(4)], writes=["Sfb%d" % g for g in range(4)])
            order = list(range(nch)) if mode != "B" else list(range(nch - 1, -1, -1))

            def load_x(i):
                c = order[i]
                kb.dma("sp", xins[i % 2][:], xv[:, :, c * 128:c * 128 + 132], reads=[xkey], writes=["xin%d" % (i % 2)], sem="ldxi%d" % (i % 2))

            load_x(0)
            for i, c in enumerate(order):
                if i + 1 < nch:
                    load_x(i + 1)
                xin = xins[i % 2]; xik = "xin%d" % (i % 2)
                if mode == "M":
                    kb.dma("sp", Sbb[i % 2][:], SBW[c, :, :], reads=["D:SBW"], writes=["Sbb%d" % (i % 2)], sem="ldsb%d" % (i % 2))
                    kb.dma("sp", zss[0][:], ZS[c * 128:(c + 1) * 128, :], reads=["D:z"], writes=["zs0"], sem="ldz0")
                for q in range(6):
                    bank = wbank()
                    for j in range(4):
                        b = q * 4 + j
                        kb.mm(PS[bank][:, j * 128:(j + 1) * 128], [(dg[:, k * 24 + b, :], xin[:, b, k:k + 128]) for k in range(5)],
                              reads=[xik, "dg"], writes=[PK[bank]])
                    for j in range(4):
                        b = q * 4 + j
                        if b < 16:
                            dst, key = xcT[:, b, :], "xcT"
                        elif b < 20:
                            dst, key = BTf[:, b - 16, :], "BTf"
                        else:
                            dst, key = CT[:, b - 20, :], "CT"
                        kb.act(lambda a: a.activation(out=dst, in_=PS[bank][:, j * 128:(j + 1) * 128], func=AF.Silu, bias=convb[:, b:b + 1]),
                               reads=[PK[bank], "convb"], writes=[key + str(q)])
                xcTk = ["xcT%d" % q for q in range(4)]
                kb.act(lambda a: a.copy(out=BT[:], in_=BTf[:]), reads=["BTf4"], writes=["BT"])
                dtc = dtsrc[:, c, :]
                kb.dve(lambda v: v.tensor_tensor(out=avec[:], in0=dtc, in1=rows64[:, 0, :], op=ALU.mult), reads=["dtL", "dtC", "rows64"], writes=["avec"])
                bank = wbank()
                for j, mi in enumerate((MU, MSL, ML, MSU, ONES)):
                    kb.mm(PS[bank][:, j * 64:(j + 1) * 64], [(cm[:, mi, :], avec[:])], reads=["avec", "cm"], writes=[PK[bank]])
                kb.act(lambda a: a.activation(out=Edec[:].rearrange("p a b -> p (a b)"), in_=PS[bank][:, 0:320], func=AF.Exp), reads=[PK[bank]], writes=["Edec"])
                d_state = 1 if mode == "B" else 0
                ei = 3 if d_state == 1 else 1
                kb.dve(lambda v: v.tensor_tensor(out=w1[:, 0:32], in0=dtc[:, d_state * 32:(d_state + 1) * 32], in1=Edec[:, ei, d_state * 32:(d_state + 1) * 32], op=ALU.mult),
                       reads=["dtL", "dtC", "Edec"], writes=["w1"])
                for q in range(4):
                    bank = wbank()
                    for j in range(4):
                        kb.tr(PS[bank][:, j * 128:(j + 1) * 128], xcT[:, q * 4 + j, :], ident, reads=["xcT%d" % q, "cm"], writes=[PK[bank]])
                    qs = slice(q * 512, (q + 1) * 512)
                    pv = PS[bank][:, :].rearrange("p (h q) -> p h q", q=64)

                    def prod(dst, sc, rkeys, wkey):
                        kb.dve(lambda v: v.tensor_tensor(out=dst[:, qs].rearrange("p (h q) -> p h q", q=64), in0=pv,
                                                         in1=sc.unsqueeze(2).to_broadcast([128, 8, 64]), op=ALU.mult),
                               reads=[PK[bank]] + rkeys, writes=[wkey])
                    prod(xst, w1[:, q * 8:(q + 1) * 8], ["w1"], "xst%d" % q)
                    if mode == "M":
                        for d in range(2):
                            prod(xdt[d], dtc[:, d * 32 + q * 8:d * 32 + (q + 1) * 8], ["dtL"], "xdt%d_%d" % (d, q))
                        prod(xsk, rows64[:, 3, q * 8:(q + 1) * 8], ["rows64"], "xsk%d" % q)
                bank = wbank()
                for g in range(4):
                    kb.tr(PS[bank][:, g * 128:(g + 1) * 128], BTf[:, g, :], ident, reads=["BTf4", "cm"], writes=[PK[bank]])
                kb.act(lambda a: a.copy(out=Btok[:].rearrange("p a b -> p (a b)"), in_=PS[bank][:, :]), reads=[PK[bank]], writes=["Btok"])
                if mode == "B" and nch == NCH:
                    kb.dma("sp", SBW[c, :, :], Sfb[:], reads=["Sfb%d" % g for g in range(4)], writes=["D:SBW"], sem="stsb")
                if mode == "M":
                    Sb = Sbb[i % 2]; sbk = "Sbb%d" % (i % 2)
                    for g in range(4):
                        gs = slice(g * 512, (g + 1) * 512)
                        kb.mm(PS[2][:, 0:128], [(BT[:, g, :], CT[:, g, :])], reads=["BT", "CT5"], writes=[PK[2]])
                        kb.dve(lambda v: v.tensor_tensor(out=scm[0][:], in0=PS[2][:, 0:128], in1=cm[:, MU, :], op=ALU.mult), reads=[PK[2], "cm"], writes=["scm0"])
                        kb.dve(lambda v: v.tensor_tensor(out=scm[1][:], in0=PS[2][:, 0:128], in1=cm[:, ML, :], op=ALU.mult), reads=[PK[2], "cm"], writes=["scm1"])
                        kb.mm(PS[3][:, :], [(CT[:, g, :], Sfb[:, gs])], reads=["CT5", "Sfb%d" % g], writes=[PK[3]])
                        kb.mm(PS[4][:, :], [(CT[:, g, :], Sb[:, gs])], reads=["CT5", sbk], writes=[PK[4]])
                        for hq in range(2):
                            h0 = g * 8 + hq * 4
                            for d in range(2):
                                mrhs = MU if d == 0 else ML
                                mlhs = MSL if d == 0 else MSU
                                for i4 in range(4):
                                    kb.act(lambda a: a.activation(out=rhsD[d][:, i4, :], in_=cm[:, mrhs, :], func=AF.Identity,
                                                                  scale=avec[:, d * 32 + h0 + i4:d * 32 + h0 + i4 + 1]),
                                           reads=["avec", "cm"], writes=["rhsD%d_%d" % (d, i4)])
                                bd = 6 + d
                                kb.mm(PS[bd][:, :], [(cm[:, mlhs, :], rhsD[d][:].rearrange("p a b -> p (a b)"))], reads=["rhsD%d_%d" % (d, i4) for i4 in range(4)] + ["cm"], writes=[PK[bd]])
                                kb.act(lambda a: a.activation(out=Ex[d][:], in_=PS[bd][:, :], func=AF.Exp), reads=[PK[bd]], writes=["Ex%d" % d])
                                kb.dve(lambda v: v.tensor_tensor(out=MT[d][:], in0=Ex[d][:].rearrange("p (a b) -> p a b", b=128),
                                                                 in1=scm[d][:].unsqueeze(1).to_broadcast([128, 4, 128]), op=ALU.mult),
                                       reads=["Ex%d" % d, "scm%d" % d], writes=["MT%d" % d])
                            for ii in range(4):
                                h = h0 + ii
                                col = (hq * 4 + ii) * 64
                                kb.mm(PS[5][:, col:col + 64],
                                      [(MT[0][:, ii, :], xdt[0][:, h * 64:(h + 1) * 64]), (MT[1][:, ii, :], xdt[1][:, h * 64:(h + 1) * 64])],
                                      reads=["MT0", "MT1", "xdt0_%d" % g, "xdt1_%d" % g], writes=[PK[5]])
                        kb.dve(lambda v: v.tensor_tensor(out=t1[:].rearrange("p (h q) -> p h q", q=64), in0=PS[3][:, :].rearrange("p (h q) -> p h q", q=64),
                                                         in1=Edec[:, 0, g * 8:g * 8 + 8].unsqueeze(2).to_broadcast([128, 8, 64]), op=ALU.mult),
                               reads=[PK[3], "Edec"], writes=["t1"])
                        kb.dve(lambda v: v.tensor_tensor(out=t2[:].rearrange("p (h q) -> p h q", q=64), in0=PS[4][:, :].rearrange("p (h q) -> p h q", q=64),
                                                         in1=Edec[:, 2, 32 + g * 8:32 + g * 8 + 8].unsqueeze(2).to_broadcast([128, 8, 64]), op=ALU.mult),
                               reads=[PK[4], "Edec"], writes=["t2"])
                        kb.dve(lambda g_: g_.tensor_tensor(out=t1[:], in0=t1[:], in1=t2[:], op=ALU.add), reads=["t1", "t2"], writes=["t1"])
                        kb.dve(lambda v: v.tensor_tensor(out=ysb[:, gs], in0=PS[5][:, :], in1=t1[:], op=ALU.add), reads=[PK[5], "t1"], writes=["ysb%d" % g])
                for g in range(4):
                    gs = slice(g * 512, (g + 1) * 512)
                    bank = wbank()
                    kb.mm(PS[bank][:, :], [(Btok[:, g, :], xst[:, gs])], reads=["Btok", "xst%d" % g], writes=[PK[bank]])
                    kb.dve(lambda v: v.tensor_tensor(out=Sf[:, gs].rearrange("p (h q) -> p h q", q=64), in0=Sf[:, gs].rearrange("p (h q) -> p h q", q=64),
                                                     in1=Edec[:, 4, d_state * 32 + g * 8:d_state * 32 + g * 8 + 8].unsqueeze(2).to_broadcast([128, 8, 64]), op=ALU.mult),
                           reads=["Edec", "Sf%d" % g], writes=["Sf%d" % g])
                    kb.dve(lambda v: v.tensor_tensor(out=Sf[:, gs], in0=Sf[:, gs], in1=PS[bank][:, :], op=ALU.add), reads=[PK[bank], "Sf%d" % g], writes=["Sf%d" % g])
                    kb.act(lambda a: a.copy(out=Sfb[:, gs], in_=Sf[:, gs]), reads=["Sf%d" % g], writes=["Sfb%d" % g])
                if mode == "M":
                    ysk = ["ysb%d" % g for g in range(4)]
                    kb.dve(lambda v: v.tensor_tensor(out=ysb[:], in0=ysb[:], in1=xsk[:], op=ALU.add), reads=ysk + ["xsk%d" % q for q in range(4)], writes=["ysbA"])
                    kb.dve(lambda v: v.tensor_tensor(out=ysb[:], in0=ysb[:], in1=zss[0][:], op=ALU.mult), reads=["ysbA", "zs0"], writes=["ysbA"])
                    kb.act(lambda a: a.activation(out=xsk[:], in_=ysb[:], func=AF.Square, accum_out=ss[:, 0:1]), reads=["ysbA"] + ["xsk%d" % q for q in range(4)], writes=["ss"] + ["xsk%d" % q for q in range(4)])
                    kb.act(lambda a: a.activation(out=ss[:, 1:2], in_=ss[:, 0:1], func=AF.Sqrt, scale=1.0 / D, bias=EPS), reads=["ss"], writes=["ss1"])
                    kb.dve(lambda v: v.reciprocal(out=ss[:, 1:2], in_=ss[:, 1:2]), reads=["ss1"], writes=["ss1"])
                    kb.dve(lambda v: v.scalar_tensor_tensor(out=ysb[:], in0=ysb[:], scalar=ss[:, 1:2], in1=nwr[:], op0=ALU.mult, op1=ALU.mult),
                           reads=["ysbA", "ss1", "nwr"], writes=["ysbA"] + ysk)
                    yt = ynT[0]; ytk = "ynT0"
                    for q in range(4):
                        bank = wbank()
                        for j in range(4):
                            kb.tr(PS[bank][:, j * 128:(j + 1) * 128], ysb[:, (q * 4 + j) * 128:(q * 4 + j + 1) * 128], ident, reads=["ysbA", "cm"], writes=[PK[bank]])
                        o_ap = yt[:, q * 4:q * 4 + 4, (c % 4) * 128:(c % 4 + 1) * 128]
                        i_ap = PS[bank][:, :].rearrange("p (a b) -> p a b", b=128)
                        if q % 2 == 0:
                            kb.act(lambda a: a.copy(out=o_ap, in_=i_ap), reads=[PK[bank]], writes=[ytk])
                        else:
                            kb.dve(lambda v: v.tensor_copy(out=o_ap, in_=i_ap), reads=[PK[bank]], writes=[ytk])
                    if c % 4 == 3:
                        tl = c // 4
                        kb.dma("sp", YN_T.rearrange("(k p) t -> p k t", p=128)[:, :, tl * 512:(tl + 1) * 512], yt[:], reads=[ytk], writes=["D:YN_T"], sem="styn%d" % (tl % 2))
            if S_out_slot is not None and mode != "M":
                kb.dve(lambda v: v.tensor_copy(out=S_out_slot, in_=Sf[:]), reads=["Sf%d" % g for g in range(4)], writes=["S_init"])
            kb.barrier()

    if not SKIP:
        ssd_phase(XBC_C, "D:XBCC", dt_ctx, 2, "F", None, S_init[:, 0, :])
        ssd_phase(XBC_C, "D:XBCC", dt_ctx, 2, "B", None, S_init[:, 1, :])
    if stop == "ctxssd":
        if DBG_A is not None:
            kb.dma("sp", DBG_A[:, 0:4096], S_init[:].rearrange("p a b -> p (a b)"), reads=["S_init"], writes=["D:DBG_A"], sem="st0")
        kb.barrier()
        return nc
    if not SKIP:
        ssd_phase(XBC_T, "D:XBCL", dt_all, NCH, "B", S_init[:, 1, :], None)
    if stop_here("ssdB"):
        return nc
    if not SKIP:
        ssd_phase(XBC_T, "D:XBCL", dt_all, NCH, "M", S_init[:, 0, :], None)
    ssd_es.close()
    if stop_here("ssdM"):
        return nc

    with ExitStack() as pes:
      if not SKIP:
            wsr = kb.sb(pes, "wsr", [128, 8, 128], F32)
            wsTf = kb.sb(pes, "wsTf", [128, 8, 128], F32)
            wsTb = kb.sb(pes, "wsTb", [128, 8, 128], BF16)
            Bbc = kb.sb(pes, "Bbc", [128, D], F32)
            bsr = kb.sb(pes, "bsr", [1, 8, 128], F32)
            Rc = kb.sb(pes, "Rc", [128, 16, 128], F32)
            vgs = [kb.sb(pes, "vg%d" % i, [128, D], F32) for i in range(2)]
            vh = [kb.sb(pes, "vh%d" % i, [128, D], BF16) for i in range(4)]
            uT = [kb.sb(pes, "uT%d" % i, [128, KD, 512], F32) for i in range(1)]
            cmT = [kb.sb(pes, "cmT%d" % i, [128, KD, 512], BF16) for i in range(1)]
            tmpc = [kb.sb(pes, "tmpc%d" % i, [128, 512], F32) for i in range(2)]
            bst = kb.sb(pes, "bstc", [128, 4, 6], F32)
            mv = kb.sb(pes, "mvc", [128, 4], F32)
            col_load(cols[:, C_CMG, :], cmlp_ln_g, 16, "colsCMG")
            kb.dma("sp", wsr[:], cmlp_ws.rearrange("g t s -> t g s"), writes=["wsr"], sem="k_wsr")
            row_load(Bbc[:], cmlp_ln_b, "Bbc")
            kb.dma("sp", bsr[0:1, :, :], cmlp_bs.rearrange("g t -> (g t)").rearrange("(o g t) -> o g t", o=1, g=8), writes=["bsr"], sem="k_bsr")
            for g in range(8):
                kb.tr(PS[0][:, g % 4 * 128:(g % 4 + 1) * 128], wsr[:, g, :], ident, reads=["wsr", "cm"], writes=[PK[0]])
                kb.dve(lambda v: v.tensor_copy(out=wsTf[:, g, :], in_=PS[0][:, g % 4 * 128:(g % 4 + 1) * 128]), reads=[PK[0]], writes=["wsTf"])
            kb.dve(lambda v: v.tensor_copy(out=wsTb[:], in_=wsTf[:]), reads=["wsTf"], writes=["wsTb"])
            for blk in range(16):
                g = blk // 2
                kb.mm(PS[1][:, 0:128], [(Bbc[:, blk * 128:(blk + 1) * 128], wsTf[:, g, :]), (cm[0:1, ONES, :], bsr[0:1, g, :])],
                      reads=["Bbc", "wsTf", "bsr", "cm"], writes=[PK[1]])
                kb.dve(lambda v: v.tensor_copy(out=Rc[:, blk, :], in_=PS[1][:, 0:128]), reads=[PK[1]], writes=["Rc"])
            utv = U_T.rearrange("(k p) t -> p k t", p=128)
            for tl in range(T // 512):
                kb.dma("sp", uT[0][:], utv[:, :, tl * 512:(tl + 1) * 512], reads=["D:u"], writes=["uT0"], sem="ldu0")
                for ci in range(4):
                    c = tl * 4 + ci
                    vg = vgs[c % 2]; vk = "vg%d" % (c % 2)
                    kb.dma("sp", vg[:], VG[c * 128:(c + 1) * 128, :], reads=["D:v"], writes=[vk], sem="ldv%d" % (c % 2))
                    for q in range(4):
                        kb.dve(lambda v: v.bn_stats(out=bst[:, q, :], in_=vg[:, q * 512:(q + 1) * 512]), reads=[vk], writes=["bstc"])
                    kb.dve(lambda v: v.bn_aggr(out=mv[:, 0:2], in_=bst[:].rearrange("p a b -> p (a b)")), reads=["bstc"], writes=["mvc"])
                    kb.act(lambda a: a.activation(out=mv[:, 2:3], in_=mv[:, 1:2], func=AF.Sqrt, bias=EPS), reads=["mvc"], writes=["mvc2"])
                    kb.dve(lambda v: v.reciprocal(out=mv[:, 2:3], in_=mv[:, 2:3]), reads=["mvc2"], writes=["mvc2"])
                    kb.dve(lambda v: v.scalar_tensor_tensor(out=mv[:, 3:4], in0=mv[:, 0:1], scalar=-1.0, in1=mv[:, 2:3], op0=ALU.mult, op1=ALU.mult),
                           reads=["mvc", "mvc2"], writes=["mvc3"])
                    kb.act(lambda a: a.activation(out=vh[ci][:], in_=vg[:], func=AF.Identity, scale=mv[:, 2:3], bias=mv[:, 3:4]),
                           reads=[vk, "mvc2", "mvc3"], writes=["vh%d" % ci])
                ct = cmT[0]; ck = "cmT0"
                for blk in range(16):
                    g = blk // 2
                    bank = 2 + blk % 4
                    for ci in range(4):
                        kb.mm(PS[bank][:, ci * 128:(ci + 1) * 128], [(vh[ci][:, blk * 128:(blk + 1) * 128], wsTb[:, g, :])],
                              reads=["vh%d" % ci, "wsTb"], writes=[PK[bank]])
                    tp = tmpc[blk % 2]; tk = "tmpc%d" % (blk % 2)
                    kb.dve(lambda v: v.scalar_tensor_tensor(out=tp[:].rearrange("p (a b) -> p a b", b=128), in0=PS[bank][:, :].rearrange("p (a b) -> p a b", b=128),
                                                            scalar=cols[:, C_CMG, blk:blk + 1], in1=Rc[:, blk, :].unsqueeze(1).to_broadcast([128, 4, 128]),
                                                            op0=ALU.mult, op1=ALU.add),
                           reads=[PK[bank], "colsCMG", "Rc"], writes=[tk])
                    kb.dve(lambda g_: g_.tensor_tensor(out=ct[:, blk, :], in0=tp[:], in1=uT[0][:, blk, :], op=ALU.mult),
                            reads=[tk, "uT0"], writes=[ck])
                kb.dma("sp", CM_T.rearrange("(k p) t -> p k t", p=128)[:, :, tl * 512:(tl + 1) * 512], ct[:], reads=[ck], writes=["D:CM_T"], sem="stcm%d" % (tl % 2))
            if stop == "cmlp" and DBG_A is not None:
                kb.dma("sp", DBG_A[:, 0:2048], Rc[:].rearrange("p a b -> p (a b)"), reads=["Rc"], writes=["D:DBG_A"], sem="st0")
                kb.dma("sp", DBG_A[:, 2048:3072], wsTf[:].rearrange("p a b -> p (a b)"), reads=["wsTf"], writes=["D:DBG_A"], sem="st0")
                kb.dma("sp", DBG_A[:, 3072:3088], cols[:, C_CMG, :], reads=["colsCMG"], writes=["D:DBG_A"], sem="st0")
            kb.barrier()

    if stop_here("cmlp"):
        return nc
    def load_w2048(wres, wsrc, key):
        wv = wsrc.rearrange("(kc p) n -> p kc n", p=128)
        for i in range(4):
            kb.dma("pool", wres[:, :, i * 512:(i + 1) * 512], wv[:, :, i * 512:(i + 1) * 512], writes=[key], sem="ldwr")

    def gated_proj(wsrc, inT, inkey, gateT, gkey, second):
        with ExitStack() as pes:
            wres = kb.sb(pes, "wres", [128, KD, D], BF16)
            ins = [kb.sb(pes, "gin%d" % i, [128, KD, 512], BF16) for i in range(2)]
            gts = [kb.sb(pes, "ggt%d" % i, [128, 512], F32) for i in range(2)]
            pts_ = [kb.sb(pes, "gpt%d" % i, [128, 512], F32) for i in range(2)]
            so = [kb.sb(pes, "gso%d" % i, [128, 512], F32) for i in range(2)]
            sob = [kb.sb(pes, "gsob%d" % i, [128, 512], BF16) for i in range(2)]
            load_w2048(wres, wsrc, "wres")
            iv = inT.rearrange("(k p) t -> p k t", p=128)
            n = 0
            for tl in range(T // 512):
                ts_ = slice(tl * 512, (tl + 1) * 512)
                kb.dma("sp", ins[tl % 2][:], iv[:, :, ts_], reads=[inkey], writes=["gin%d" % (tl % 2)], sem="ldgi%d" % (tl % 2))
                for cb in range(16):
                    rs = slice(cb * 128, (cb + 1) * 128)
                    j = n % 2
                    kb.dma("sp", gts[j][:], gateT[rs, ts_], reads=[gkey], writes=["ggt%d" % j], sem="ldgg%d" % j)
                    if second:
                        kb.dma("sp", pts_[j][:], PART_T[rs, ts_], reads=["D:PART_T"], writes=["gpt%d" % j], sem="ldgp%d" % j)
                    bank = n % 4
                    kb.mm(PS[bank][:, :], [(wres[:, k, rs], ins[tl % 2][:, k, :]) for k in range(KD)], reads=["wres", "gin%d" % (tl % 2)], writes=[PK[bank]])
                    if not second:
                        kb.dve(lambda v: v.tensor_tensor(out=so[j][:], in0=PS[bank][:, :], in1=gts[j][:], op=ALU.mult), reads=[PK[bank], "ggt%d" % j], writes=["gso%d" % j])
                        kb.dma("sp", PART_T[rs, ts_], so[j][:], reads=["gso%d" % j], writes=["D:PART_T"], sem="stgo%d" % j)
                    else:
                        kb.dve(lambda v: v.tensor_tensor(out=so[j][:], in0=PS[bank][:, :], in1=gts[j][:], op=ALU.mult), reads=[PK[bank], "ggt%d" % j], writes=["gso%d" % j])
                        kb.dve(lambda g_: g_.tensor_tensor(out=sob[j][:], in0=so[j][:], in1=pts_[j][:], op=ALU.add), reads=["gso%d" % j, "gpt%d" % j], writes=["gsob%d" % j])
                        kb.dma("sp", MG_T[rs, ts_], sob[j][:], reads=["gsob%d" % j], writes=["D:MG_T"], sem="stgo%d" % j)
                    n += 1
            kb.barrier()

    if not SKIP:
        gated_proj(w_ssd_br, YN_T, "D:YN_T", GS_T, "D:gs", False)
    if stop_here("gp1"):
        return nc
    if not SKIP:
        gated_proj(w_cmlp_br, CM_T, "D:CM_T", GC_T, "D:gc", True)
    if stop_here("gp"):
        return nc

    kb.dve(lambda v: v.memset(gates[:, :, NE:NE + 1], 1.0), writes=["gates1"])
    with ExitStack() as pes:
        wres = kb.sb(pes, "wres_o", [128, KD, D], BF16)
        g1r = kb.sb(pes, "g1r", [128, D], F32)
        l1g = kb.sb(pes, "l1g", [128, D], F32)
        l1b = kb.sb(pes, "l1b", [128, D], F32)
        mgs = [kb.sb(pes, "mg%d" % i, [128, KD, 512], BF16) for i in range(1)]
        hls = [kb.sb(pes, "hl3%d" % i, [128, D], F32) for i in range(2)]
        rss = [kb.sb(pes, "res%d" % i, [128, D], F32) for i in range(2)]
        a2f = kb.sb(pes, "a2f", [128, KD, 128], F32)
        a2s = [kb.sb(pes, "a2s%d" % i, [128, KD, 512], BF16) for i in range(1)]
        wr = kb.sb(pes, "wr", [128, KD, NE], F32)
        bst = kb.sb(pes, "bst3", [128, 4, 6], F32)
        mv = kb.sb(pes, "mv3", [128, 4], F32)
        rt = kb.sb(pes, "rt", [128, 10, 64], F32)
        load_w2048(wres, w_o, "wres_o")
        row_load(g1r[:], ADAFLAT[2 * D:3 * D], "g1r")
        kb._deps("sp", ["D:ADAROW"], ())
        row_load(l1g[:], ln1_g, "l1g")
        row_load(l1b[:], ln1_b, "l1b")
        kb.dma("sp", wr[:], w_router.rearrange("(k p) e -> p k e", p=128), writes=["wr"], sem="k_wr")
        mv_ = MG_T.rearrange("(k p) t -> p k t", p=128)
        SCR, BIA, M8, GSC, T8, GM, M1, MSK, SEL, WW = range(10)
        NTL3 = dbg.get("p3c_tiles", T // 512)
        a2 = a2s[0]; a2k = "a2s0"
        mg = mgs[0]; mk = "mg0"

        def names(c):
            return hls[c % 2], "hl3%d" % (c % 2), rss[c % 2], "res%d" % (c % 2)

        def part_a(c):
            tl, tc = c // 4, c % 4
            hl, hk, rs_, rk = names(c)
            if tc == 0:
                kb.dma("sp", mg[:], mv_[:, :, tl * 512:(tl + 1) * 512], reads=["D:MG_T"], writes=[mk], sem="ldmg0")
            kb.dma("sp", hl[:], XN[c * 128:(c + 1) * 128, :], reads=["D:XN"], writes=[hk], sem="ldh%d" % (c % 2))
            for db in range(4):
                bank = db
                ds_ = slice(db * 512, (db + 1) * 512)
                kb.mm(PS[bank][:, :], [(mg[:, k, tc * 128:(tc + 1) * 128], wres[:, k, ds_]) for k in range(KD)], reads=[mk, "wres_o"], writes=[PK[bank]])
                kb.dve(lambda v: v.tensor_tensor(out=rs_[:, ds_], in0=PS[bank][:, :], in1=g1r[:, ds_], op=ALU.mult), reads=[PK[bank], "g1r"], writes=[rk + "_%d" % db, rk])

        def part_b(c):
            tl, tc = c // 4, c % 4
            hl, hk, rs_, rk = names(c)
            rks = [rk + "_%d" % db for db in range(4)]
            kb.dve(lambda v: v.scalar_tensor_tensor(out=rs_[:], in0=hl[:], scalar=ALPHA, in1=rs_[:], op0=ALU.mult, op1=ALU.add), reads=rks + [hk], writes=[rk])
            for q in range(4):
                kb.dve(lambda v: v.bn_stats(out=bst[:, q, :], in_=rs_[:, q * 512:(q + 1) * 512]), reads=[rk], writes=["bst3"])
            kb.dve(lambda v: v.bn_aggr(out=mv[:, 0:2], in_=bst[:].rearrange("p a b -> p (a b)")), reads=["bst3"], writes=["mv3"])
            kb.act(lambda a: a.activation(out=mv[:, 2:3], in_=mv[:, 1:2], func=AF.Sqrt, bias=EPS), reads=["mv3"], writes=["mv32"])
            kb.dve(lambda v: v.reciprocal(out=mv[:, 2:3], in_=mv[:, 2:3]), reads=["mv32"], writes=["mv32"])
            kb.dve(lambda v: v.scalar_tensor_tensor(out=mv[:, 3:4], in0=mv[:, 0:1], scalar=-1.0, in1=mv[:, 2:3], op0=ALU.mult, op1=ALU.mult),
                   reads=["mv3", "mv32"], writes=["mv33"])
            kb.act(lambda a: a.activation(out=rs_[:], in_=rs_[:], func=AF.Identity, scale=mv[:, 2:3], bias=mv[:, 3:4]), reads=[rk, "mv32", "mv33"], writes=[rk])
            kb.dve(lambda v: v.tensor_tensor(out=rs_[:], in0=rs_[:], in1=l1g[:], op=ALU.mult), reads=[rk, "l1g"], writes=[rk])
            kb.dve(lambda g_: g_.tensor_tensor(out=rs_[:], in0=rs_[:], in1=l1b[:], op=ALU.add), reads=[rk, "l1b"], writes=[rk] + rks)
            kb.dma("sp", H1[c * 128:(c + 1) * 128, :], rs_[:], reads=[rk], writes=["D:H1"], sem="sth%d" % (c % 2))
            for q in range(4):
                bank = 4 + q
                for j in range(4):
                    dk = q * 4 + j
                    kb.tr(PS[bank][:, j * 128:(j + 1) * 128], rs_[:, dk * 128:(dk + 1) * 128], ident, reads=[rk, "cm"], writes=[PK[bank]])
                for j in range(4):
                    dk = q * 4 + j
                    kb.dve(lambda v: v.tensor_scalar(out=a2f[:, dk, :], in0=PS[bank][:, j * 128:(j + 1) * 128], scalar1=cols[:, C_A2, dk:dk + 1],
                                                     scalar2=cols[:, C_B2, dk:dk + 1], op0=ALU.mult, op1=ALU.add),
                           reads=[PK[bank], "colsA2", "colsB2"], writes=["a2f%d" % q])
                kb.act(lambda a: a.copy(out=a2[:, q * 4:q * 4 + 4, tc * 128:(tc + 1) * 128], in_=a2f[:, q * 4:q * 4 + 4, :]),
                       reads=["a2f%d" % q], writes=[a2k])
            if not dbg.get("norouter"):
                kb.mm(PS[7][:, 0:NE], [(a2f[:, dk, :], wr[:, dk, :]) for dk in range(KD)], reads=["a2f%d" % q for q in range(4)] + ["wr"], writes=[PK[7]])
                kb.act(lambda a: a.activation(out=rt[:, SCR, :], in_=PS[7][:, 0:NE], func=AF.Sigmoid), reads=[PK[7]], writes=["rt"])
                R = lambda fn, **kw: kb.dve(fn, reads=["rt", "rows64"], writes=["rt"])
                R(lambda v: v.tensor_tensor(out=rt[:, BIA, :], in0=rt[:, SCR, :], in1=rows64[:, 2, :], op=ALU.add))
                for g in range(8):
                    R(lambda v: v.max(out=rt[:, M8, g * 8:(g + 1) * 8], in_=rt[:, BIA, g * 8:(g + 1) * 8]))
                m8v = rt[:, M8, :].rearrange("p (g e) -> p g e", e=8)
                R(lambda v: v.tensor_tensor(out=rt[:, GSC, 0:8], in0=m8v[:, :, 0], in1=m8v[:, :, 1], op=ALU.add))
                R(lambda v: v.max(out=rt[:, T8, 0:8], in_=rt[:, GSC, 0:8]))
                R(lambda v: v.tensor_scalar(out=rt[:, GM, 0:8], in0=rt[:, GSC, 0:8], scalar1=rt[:, T8, 3:4], scalar2=None, op0=ALU.is_ge))
                R(lambda v: v.tensor_scalar(out=rt[:, M1, 0:8], in0=rt[:, GM, 0:8], scalar1=4.0, scalar2=-4.0, op0=ALU.mult, op1=ALU.add))
                R(lambda v: v.tensor_tensor(out=rt[:, MSK, :].rearrange("p (g e) -> p g e", e=8), in0=rt[:, BIA, :].rearrange("p (g e) -> p g e", e=8),
                                            in1=rt[:, GM, 0:8].unsqueeze(2).to_broadcast([128, 8, 8]), op=ALU.mult))
                R(lambda v: v.tensor_tensor(out=rt[:, MSK, :].rearrange("p (g e) -> p g e", e=8), in0=rt[:, MSK, :].rearrange("p (g e) -> p g e", e=8),
                                            in1=rt[:, M1, 0:8].unsqueeze(2).to_broadcast([128, 8, 8]), op=ALU.add))
                R(lambda v: v.max(out=rt[:, T8, 8:16], in_=rt[:, MSK, :]))
                R(lambda v: v.tensor_scalar(out=rt[:, SEL, :], in0=rt[:, MSK, :], scalar1=rt[:, T8, 15:16], scalar2=None, op0=ALU.is_ge))
                R(lambda v: v.tensor_tensor(out=rt[:, WW, :], in0=rt[:, SCR, :], in1=rt[:, SEL, :], op=ALU.mult))
                R(lambda v: v.tensor_reduce(out=rt[:, T8, 16:17], in_=rt[:, WW, :], axis=mybir.AxisListType.X, op=ALU.add))
                R(lambda v: v.reciprocal(out=rt[:, T8, 16:17], in_=rt[:, T8, 16:17]))
                kb.dve(lambda v: v.tensor_scalar(out=gates[:, c, 0:NE], in0=rt[:, WW, :], scalar1=rt[:, T8, 16:17], scalar2=2.5, op0=ALU.mult, op1=ALU.mult),
                       reads=["rt"], writes=["gates"])
            if tc == 3:
                kb.dma("sp", A2_T.rearrange("(k p) t -> p k t", p=128)[:, :, tl * 512:(tl + 1) * 512], a2[:], reads=[a2k], writes=["D:A2_T"], sem="sta2%d" % (tl % 2))

        NC3 = NTL3 * 4
        if NC3 > 0:
            part_a(0)
        for c in range(NC3):
            if c + 1 < NC3:
                part_a(c + 1)
            part_b(c)
        kb.barrier()

    if stop == "p3c":
        if DBG_A is not None:
            kb.dma("sp", DBG_A[:, 0:NCH * (NE + 1)], gates[:].rearrange("p a b -> p (a b)"), reads=["gates", "gates1"], writes=["D:DBG_A"], sem="st0")
        kb.barrier()
        return nc
    with ExitStack() as pes:
        acc = kb.sb(pes, "acc", [128, 8, D], F32)
        a2t = [kb.sb(pes, "a2t%d" % i, [128, KD, 512], BF16) for i in range(2)]
        wg = [kb.sb(pes, "wg%d" % i, [128, KD, 256], BF16) for i in range(2)]
        wu = [kb.sb(pes, "wu%d" % i, [128, KD, 256], BF16) for i in range(2)]
        wd = [kb.sb(pes, "wd%d" % i, [128, 2, D], BF16) for i in range(2)]
        sgs = [kb.sb(pes, "sg%d" % i, [128, 512], F32) for i in range(2)]
        hT = [kb.sb(pes, "hT%d" % i, [128, 2, 512], BF16) for i in range(2)]
        a2v = A2_T.rearrange("(k p) t -> p k t", p=128)
        NHE = 2 * (NE + 1)

        def wsrc(he):
            e, hf = he // 2, he % 2
            cs = slice(hf * 256, (hf + 1) * 256)
            if e < NE:
                return (w_e_gate[e].rearrange("(k p) n -> p k n", p=128)[:, :, cs], w_e_up[e].rearrange("(k p) n -> p k n", p=128)[:, :, cs],
                        w_e_down[e].rearrange("(j p) n -> p j n", p=128)[:, hf * 2:hf * 2 + 2, :])
            return (w_sh_gate.rearrange("(k p) n -> p k n", p=128)[:, :, cs], w_sh_up.rearrange("(k p) n -> p k n", p=128)[:, :, cs],
                    w_sh_down.rearrange("(j p) n -> p j n", p=128)[:, hf * 2:hf * 2 + 2, :])

        def load_he(n, he):
            sg_, su_, sd_ = wsrc(he)
            j = n % 2
            kb.dma("pool", wg[j][:], sg_, writes=["wg%d" % j], sem="ldwg%d" % j)
            kb.dma("pool", wu[j][:], su_, writes=["wu%d" % j], sem="ldwu%d" % j)
            kb.dma("pool", wd[j][:], sd_, writes=["wd%d" % j], sem="ldwd%d" % j)

        pbc = [0]

        def gateup_groups(st, he, n, tt):
            j = n % 2
            h = hT[tt]; hk = "hT%d" % tt
            items = []
            for jb in range(2):
                def f(jb=jb):
                    bg, bu = pbc[0] % 4, (pbc[0] + 1) % 4
                    pbc[0] += 2
                    kb.mm(PS[bg][:, :], [(wg[j][:, k, jb * 128:(jb + 1) * 128], a2t[tt][:, k, :]) for k in range(KD)], reads=["wg%d" % j, "a2t%d" % tt], writes=[PK[bg]])
                    kb.mm(PS[bu][:, :], [(wu[j][:, k, jb * 128:(jb + 1) * 128], a2t[tt][:, k, :]) for k in range(KD)], reads=["wu%d" % j, "a2t%d" % tt], writes=[PK[bu]])
                    sg_ = sgs[jb]; sgk = "sg%d" % jb
                    kb.act(lambda a: a.activation(out=sg_[:], in_=PS[bg][:, :], func=AF.Silu), reads=[PK[bg]], writes=[sgk])
                    kb.dve(lambda v: v.tensor_tensor(out=h[:, jb, :], in0=sg_[:], in1=PS[bu][:, :], op=ALU.mult), reads=[sgk, PK[bu]], writes=[hk + "_%d" % jb])
                items.append(f)
            return items

        def down_groups(st, he, n, tt):
            j = n % 2
            e = he // 2
            h = hT[tt]; hk = "hT%d" % tt
            items = []
            for tc in range(4):
                for db in range(4):
                    def f(tc=tc, db=db):
                        ch = tt * 4 + tc
                        c = st * 8 + ch
                        bo = 4 + (db % 4)
                        ds_ = slice(db * 512, (db + 1) * 512)
                        kb.mm(PS[bo][:, :], [(h[:, jb, tc * 128:(tc + 1) * 128], wd[j][:, jb, ds_]) for jb in range(2)],
                              reads=[hk + "_0", hk + "_1", "wd%d" % j], writes=[PK[bo]])
                        ak = "acc%d_%d" % (ch, db)
                        if he == 0:
                            kb.dve(lambda v: v.tensor_scalar(out=acc[:, ch, ds_], in0=PS[bo][:, :], scalar1=gates[:, c, e:e + 1], scalar2=None, op0=ALU.mult),
                                   reads=[PK[bo], "gates", "gates1"], writes=[ak])
                        else:
                            kb.dve(lambda v: v.scalar_tensor_tensor(out=acc[:, ch, ds_], in0=PS[bo][:, :], scalar=gates[:, c, e:e + 1], in1=acc[:, ch, ds_],
                                                                    op0=ALU.mult, op1=ALU.add),
                                   reads=[PK[bo], "gates", "gates1", ak], writes=[ak])
                    items.append(f)
            return items

        n = 0
        for st in range(4):
            for tt in range(2):
                kb.dma("sp", a2t[tt][:], a2v[:, :, st * 1024 + tt * 512:st * 1024 + (tt + 1) * 512], reads=["D:A2_T"], writes=["a2t%d" % tt], sem="lda2%d" % tt)
            units = [(he, tt) for he in range(NHE) for tt in range(2)]
            load_he(n, 0)
            for it in gateup_groups(st, 0, n, 0):
                it()
            for ui, (he, tt) in enumerate(units):
                if tt == 0 and he + 1 < NHE:
                    load_he(n + 1, he + 1)
                dn = down_groups(st, he, n, tt)
                if ui + 1 < len(units):
                    he2, tt2 = units[ui + 1]
                    gu = gateup_groups(st, he2, n + (1 if he2 != he else 0), tt2)
                else:
                    gu = []
                for di, d_ in enumerate(dn):
                    d_()
                    if di == 3 and len(gu) > 0:
                        gu[0]()
                    if di == 11 and len(gu) > 1:
                        gu[1]()
                if tt == 1:
                    n += 1
            for ch in range(8):
                c = st * 8 + ch
                kb.dma("sp", FOUT[c * 128:(c + 1) * 128, :], acc[:, ch, :], reads=["acc%d_%d" % (ch, db) for db in range(4)], writes=["D:FOUT"], sem="stfo%d" % ch)
        kb.barrier()

    if stop_here("moe"):
        return nc
    with ExitStack() as pes:
        g2r = kb.sb(pes, "g2r", [128, D], F32)
        l2g = kb.sb(pes, "l2g", [128, D], F32)
        l2b = kb.sb(pes, "l2b", [128, D], F32)
        fs = [kb.sb(pes, "ff%d" % i, [128, D], F32) for i in range(2)]
        hs = [kb.sb(pes, "fh%d" % i, [128, D], F32) for i in range(2)]
        bst = kb.sb(pes, "bstf", [128, 4, 6], F32)
        mv = kb.sb(pes, "mvf", [128, 4], F32)
        row_load(g2r[:], ADAFLAT[5 * D:6 * D], "g2r")
        row_load(l2g[:], ln2_g, "l2g")
        row_load(l2b[:], ln2_b, "l2b")
        for c in range(NCH):
            f = fs[c % 2]; fk = "ff%d" % (c % 2)
            h = hs[c % 2]; hk = "fh%d" % (c % 2)
            kb.dma("sp", f[:], FOUT[c * 128:(c + 1) * 128, :], reads=["D:FOUT"], writes=[fk], sem="ldf%d" % (c % 2))
            kb.dma("sp", h[:], H1[c * 128:(c + 1) * 128, :], reads=["D:H1"], writes=[hk], sem="ldfh%d" % (c % 2))
            kb.dve(lambda g_: g_.tensor_tensor(out=f[:], in0=f[:], in1=g2r[:], op=ALU.mult), reads=[fk, "g2r"], writes=[fk])
            kb.dve(lambda v: v.scalar_tensor_tensor(out=f[:], in0=h[:], scalar=ALPHA, in1=f[:], op0=ALU.mult, op1=ALU.add), reads=[fk, hk], writes=[fk])
            for q in range(4):
                kb.dve(lambda v: v.bn_stats(out=bst[:, q, :], in_=f[:, q * 512:(q + 1) * 512]), reads=[fk], writes=["bstf"])
            kb.dve(lambda v: v.bn_aggr(out=mv[:, 0:2], in_=bst[:].rearrange("p a b -> p (a b)")), reads=["bstf"], writes=["mvf"])
            kb.act(lambda a: a.activation(out=mv[:, 2:3], in_=mv[:, 1:2], func=AF.Sqrt, bias=EPS), reads=["mvf"], writes=["mvf2"])
            kb.dve(lambda v: v.reciprocal(out=mv[:, 2:3], in_=mv[:, 2:3]), reads=["mvf2"], writes=["mvf2"])
            kb.dve(lambda v: v.scalar_tensor_tensor(out=mv[:, 3:4], in0=mv[:, 0:1], scalar=-1.0, in1=mv[:, 2:3], op0=ALU.mult, op1=ALU.mult),
                   reads=["mvf", "mvf2"], writes=["mvf3"])
            kb.act(lambda a: a.activation(out=f[:], in_=f[:], func=AF.Identity, scale=mv[:, 2:3], bias=mv[:, 3:4]), reads=[fk, "mvf2", "mvf3"], writes=[fk])
            kb.dve(lambda v: v.tensor_tensor(out=f[:], in0=f[:], in1=l2g[:], op=ALU.mult), reads=[fk, "l2g"], writes=[fk])
            kb.dve(lambda g_: g_.tensor_tensor(out=f[:], in0=f[:], in1=l2b[:], op=ALU.add), reads=[fk, "l2b"], writes=[fk])
            kb.dma("sp", y[c * 128:(c + 1) * 128, :], f[:], reads=[fk], writes=["D:y"], sem="sty%d" % (c % 2))
    kb.finish(["D:y"])
    kb.barrier()
    return nc


def _prep_inputs(inputs):
    f = lambda a: np.ascontiguousarray(np.asarray(a, dtype=np.float32))
    sq = lambda a: f(a)[0]
    shared = {
        "c_ctx": f(inputs["c_ctx"]), "ln_in_g": f(inputs["ln_in_g"]), "ln_in_b": f(inputs["ln_in_b"]),
        "w_ada": sq(inputs["w_ada"]), "b_ada": sq(inputs["b_ada"]), "w_in": sq(inputs["w_in"]),
        "conv_w": sq(inputs["conv_w"]), "conv_b": sq(inputs["conv_b"]),
        "dt_bias": sq(inputs["dt_bias"]).reshape(64), "a_log": sq(inputs["a_log"]).reshape(64),
        "d_skip": sq(inputs["d_skip"]), "ssd_norm_w": sq(inputs["ssd_norm_w"]), "w_ssd_br": sq(inputs["w_ssd_br"]),
        "cmlp_ln_g": sq(inputs["cmlp_ln_g"]), "cmlp_ln_b": sq(inputs["cmlp_ln_b"]), "cmlp_ws": sq(inputs["cmlp_ws"]),
        "cmlp_bs": sq(inputs["cmlp_bs"]), "w_cmlp_br": sq(inputs["w_cmlp_br"]), "w_o": sq(inputs["w_o"]),
        "ln1_g": sq(inputs["ln1_g"]), "ln1_b": sq(inputs["ln1_b"]), "w_router": sq(inputs["w_router"]),
        "router_bias": sq(inputs["router_bias"]), "w_e_gate": sq(inputs["w_e_gate"]), "w_e_up": sq(inputs["w_e_up"]),
        "w_e_down": sq(inputs["w_e_down"]), "w_sh_gate": sq(inputs["w_sh_gate"]), "w_sh_up": sq(inputs["w_sh_up"]),
        "w_sh_down": sq(inputs["w_sh_down"]), "ln2_g": sq(inputs["ln2_g"]), "ln2_b": sq(inputs["ln2_b"]),
    }
    i = np.arange(128)
    lp, l = i[:, None], i[None, :]
    masks = np.stack([(lp == l), (lp <= l), (lp > l), (lp >= l), (lp < l), np.ones((128, 128), bool)], axis=1)
    shared["cmask"] = np.ascontiguousarray(masks.astype(np.float32).reshape(128, 6 * 128))
    xs, cs, cx = f(inputs["x"]), f(inputs["c"]), f(inputs["ctx"])
    in_maps = []
    for b in range(8):
        m = dict(shared)
        m["x"] = xs[b]
        m["c"] = cs[b]
        m["ctx"] = cx[b]
        in_maps.append(m)
    return in_maps


def kernel(**inputs):
    nc = build_program(DEBUG)
    in_maps = _prep_inputs(inputs)
    res = run_bass_kernel_spmd(nc, in_maps, core_ids=list(range(8)))
    return np.stack([r["y"] for r in res.results], axis=0)
```
